# Optimizing a Trainium2 kernel written in Bass

```python
import jax, jax.numpy as jnp
from jax import lax
import numpy as np

D_MODEL = 1024
BATCH = 32
SEQ = 2048
DEPTH = 4

PLE_DIM = 256
EXPAND = 2
D_MIX = EXPAND * D_MODEL
D_HGRN = D_MIX // 2
D_FOX = D_MIX - D_HGRN
HGRN_HEAD_DIM = 128
HGRN_HEADS = D_HGRN // HGRN_HEAD_DIM
FOX_HEAD_DIM = 64
FOX_HEADS = D_FOX // FOX_HEAD_DIM
HGRN_CHUNK = 32
FOX_BLOCK = 128
NORM_EPS = 1e-6
D_IN = 4 * D_HGRN + 4 * D_FOX + FOX_HEADS
SPLIT_POINTS = tuple(D_HGRN * j for j in range(1, 5)) + tuple(4 * D_HGRN + D_FOX * j for j in range(1, 5))

kernel_name = "hymba_style_hgrn2_fox_hybrid"


def rmsnorm(x, w):
    xf = x.astype(jnp.float32)
    y = xf * lax.rsqrt(jnp.mean(xf * xf, axis=-1, keepdims=True) + NORM_EPS)
    return (y * w.astype(jnp.float32)).astype(x.dtype)


def hgrn_lower_bounds(logits):
    sm = jax.nn.softmax(logits.astype(jnp.float32), axis=0)
    c = jnp.cumsum(sm, axis=0)
    return c - c[0:1]


def hgrn2_branch(q, fz, i, lb, gn_w):
    B, S, _ = q.shape
    H, dk, C = HGRN_HEADS, HGRN_HEAD_DIM, HGRN_CHUNK
    N = S // C
    f32 = jnp.float32
    fz = fz.astype(f32)
    lb = lb.astype(f32)
    log_f = jnp.logaddexp(jnp.log(lb), jnp.log1p(-lb) + jax.nn.log_sigmoid(fz))
    k = (1.0 - lb) * jax.nn.sigmoid(-fz)

    def to_chunks(t):
        return t.astype(f32).reshape(B, N, C, H, -1).transpose(1, 0, 3, 2, 4)

    qc = to_chunks(q) * (dk ** -0.5)
    kc, vc, gc = to_chunks(k), to_chunks(i), to_chunks(log_f)
    causal = jnp.tril(jnp.ones((C, C), dtype=bool))[:, :, None]

    def step(state, xs):
        qb, kb, vb, gb = xs
        b = jnp.cumsum(gb, axis=2)
        diff = b[:, :, :, None, :] - b[:, :, None, :, :]
        decay = jnp.exp(jnp.where(causal, diff, -jnp.inf))
        scores = jnp.einsum('bhtd,bhsd,bhtsd->bhts', qb, kb, decay)
        o = (jnp.einsum('bhts,bhsv->bhtv', scores, vb)
             + jnp.einsum('bhtd,bhdv->bhtv', qb * jnp.exp(b), state))
        b_last = b[:, :, -1:, :]
        new_state = (jnp.exp(b_last[:, :, 0, :])[..., None] * state
                     + jnp.einsum('bhsd,bhsv->bhdv', kb * jnp.exp(b_last - b), vb))
        return new_state, o

    state0 = jnp.zeros((B, H, dk, vc.shape[-1]), f32)
    _, o = lax.scan(step, state0, (qc, kc, vc, gc))
    o = o.transpose(1, 0, 3, 2, 4).reshape(B, S, H, -1)
    o = o * lax.rsqrt(jnp.mean(o * o, axis=-1, keepdims=True) + NORM_EPS)
    o = o * gn_w.astype(f32).reshape(H, -1)
    return o.reshape(B, S, D_HGRN).astype(q.dtype)


def fox_branch(q, k, v, fz, fb):
    B, S, _ = q.shape
    H, d, Q = FOX_HEADS, FOX_HEAD_DIM, FOX_BLOCK
    f32 = jnp.float32

    def heads(t):
        return t.reshape(B, S, H, d).transpose(0, 2, 1, 3)

    q, k, v = heads(q), heads(k), heads(v)
    log_f = jax.nn.log_sigmoid(fz.astype(f32) + fb.astype(f32))
    F = jnp.cumsum(log_f, axis=1).transpose(0, 2, 1)
    scale = d ** -0.5
    outs = []
    for blk in range(S // Q):
        q0, q1 = blk * Q, (blk + 1) * Q
        qb = q[:, :, q0:q1]
        kb, vb = k[:, :, :q1], v[:, :, :q1]
        logits = (jnp.einsum('bhtd,bhsd->bhts', qb, kb).astype(f32) * scale
                  + F[:, :, q0:q1, None] - F[:, :, None, :q1])
        mask = jnp.arange(q0, q1)[:, None] >= jnp.arange(q1)[None, :]
        probs = jax.nn.softmax(jnp.where(mask, logits, -jnp.inf), axis=-1)
        outs.append(jnp.einsum('bhts,bhsd->bhtd', probs.astype(v.dtype), vb))
    o = jnp.concatenate(outs, axis=2)
    return o.transpose(0, 2, 1, 3).reshape(B, S, D_FOX)


def setup_inputs(seed: int = 0) -> dict:
    key = jax.random.key(seed)
    ks = jax.random.split(key, 11)
    f32 = jnp.float32
    x = jax.random.normal(ks[0], (BATCH, SEQ, D_MODEL), f32)
    p = jax.random.normal(ks[1], (DEPTH, BATCH, SEQ, PLE_DIM), f32)
    norm_w = 1.0 + 0.1 * jax.random.normal(ks[2], (DEPTH, D_MODEL), f32)
    w_in = jax.random.normal(ks[3], (DEPTH, D_MODEL, D_IN), f32) * D_MODEL ** -0.5
    fox_fb = jax.random.uniform(ks[4], (DEPTH, FOX_HEADS), f32, 1.0, 4.0)
    hgrn_gn = 1.0 + 0.1 * jax.random.normal(ks[5], (DEPTH, D_HGRN), f32)
    hgrn_lb_logits = 0.5 * jax.random.normal(ks[6], (DEPTH, D_HGRN), f32)
    w_out = jax.random.normal(ks[7], (DEPTH, D_MIX, D_MODEL), f32) * D_MIX ** -0.5
    w_ple = jax.random.normal(ks[8], (DEPTH, PLE_DIM, D_MODEL), f32) * PLE_DIM ** -0.5
    w_ple_gate = jax.random.normal(ks[9], (DEPTH, D_MODEL, D_MODEL), f32) * D_MODEL ** -0.5
    final_norm_w = 1.0 + 0.1 * jax.random.normal(ks[10], (D_MODEL,), f32)
    return {"x": x, "p": p, "norm_w": norm_w, "w_in": w_in, "fox_fb": fox_fb,
            "hgrn_gn": hgrn_gn, "hgrn_lb_logits": hgrn_lb_logits, "w_out": w_out,
            "w_ple": w_ple, "w_ple_gate": w_ple_gate, "final_norm_w": final_norm_w}


def reference(x, p, norm_w, w_in, fox_fb, hgrn_gn, hgrn_lb_logits, w_out, w_ple, w_ple_gate, final_norm_w):
    lb_all = hgrn_lower_bounds(hgrn_lb_logits)
    h = x
    for i in range(DEPTH):
        u = rmsnorm(h, norm_w[i])
        proj = jnp.einsum('bsd,de->bse', u, w_in[i])
        hq, hf, hi, hg, fq, fk, fv, fg, fz = jnp.split(proj, SPLIT_POINTS, axis=-1)
        y_h = hgrn2_branch(hq, hf, hi, lb_all[i], hgrn_gn[i]) * jax.nn.silu(hg)
        y_f = fox_branch(fq, fk, fv, fz, fox_fb[i]) * jax.nn.silu(fg)
        y = jnp.concatenate([y_h, y_f], axis=-1)
        h = h + jnp.einsum('bse,ed->bsd', y, w_out[i])
        ple = jnp.einsum('bsk,kd->bsd', p[i], w_ple[i])
        h = h + jax.nn.sigmoid(jnp.einsum('bsd,de->bse', h, w_ple_gate[i])) * ple
    return rmsnorm(h, final_norm_w)
```

```python
import numpy as np
import ml_dtypes
from contextlib import ExitStack
import concourse.bass as bass
import concourse.mybir as mybir
from concourse.bass_utils import run_bass_kernel_spmd

F32 = mybir.dt.float32
BF16 = mybir.dt.bfloat16
AF = mybir.ActivationFunctionType
ALU = mybir.AluOpType

D = 1024
S = 2048
DEPTH = 4
NCH = 8
TW = 512
NT = S // TW
NB = S // 128
DIN = 8208
PLE = 256
EPS = 1e-6
NCORES = 8


class _Rec:
    def __init__(self):
        self.call = None

    def __getattr__(self, name):
        def f(*a, **k):
            self.call = (name, a, k)
            return None
        return f


class Prog:
    ENG = ('pe', 'act', 'dve', 'pool', 'sp')

    def __init__(self, nc, n_dma_sems=40):
        self.nc = nc
        self.ops = []
        self.last_w = {}
        self.readers = {}
        self.n_dma_sems = n_dma_sems

    def add(self, eng, fn, reads=(), writes=(), dma=False):
        i = len(self.ops)
        deps = set()
        for r in reads:
            w = self.last_w.get(r)
            if w is not None:
                deps.add((w, 0))
        for wkey in writes:
            w = self.last_w.get(wkey)
            if w is not None:
                deps.add((w, 1))
            for rd in self.readers.get(wkey, {}).values():
                deps.add((rd, 2))
        for r in reads:
            self.readers.setdefault(r, {})[('d', i) if dma else eng] = i
        for wkey in writes:
            self.last_w[wkey] = i
            self.readers[wkey] = {}
        rec = _Rec()
        fn(rec)
        self.ops.append(dict(eng=eng, call=rec.call, deps=deps, dma=dma, mark=False))
        return i

    def emit(self):
        nc = self.nc
        ops = self.ops
        for i, op in enumerate(ops):
            need = set()
            for (p, kind) in op['deps']:
                po = ops[p]
                if (not po['dma']) and po['eng'] == op['eng']:
                    if po['eng'] == 'pe' and not op['dma']:
                        continue
                    if kind == 2 and not op['dma']:
                        continue
                need.add(p)
            op['need'] = sorted(need)
            for p in op['need']:
                ops[p]['mark'] = True
            op['deps'] = None
        cnt = {e: 0 for e in self.ENG}
        dcnt = [0] * self.n_dma_sems
        nd = 0
        for op in ops:
            if op['dma']:
                k = nd % self.n_dma_sems
                nd += 1
                op['sem'] = ('dma', k)
                op['prev'] = dcnt[k]
                dcnt[k] += 16
                op['val'] = dcnt[k]
            elif op['mark']:
                cnt[op['eng']] += 1
                op['sem'] = ('eng', op['eng'])
                op['val'] = cnt[op['eng']]
        self.cnt = cnt
        with ExitStack() as st:
            sems = {}
            for e in self.ENG:
                sems[('eng', e)] = st.enter_context(nc.semaphore('s_' + e))
            for k in range(self.n_dma_sems):
                sems[('dma', k)] = st.enter_context(nc.semaphore('d_%d' % k))
            block = st.enter_context(nc.Block())
            per = {e: [op for op in ops if op['eng'] == e] for e in self.ENG}

            def run(eng_name, eng):
                waited = {}
                for op in per[eng_name]:
                    ws = [(ops[p]['sem'], ops[p]['val']) for p in op['need']]
                    if op['dma'] and op['prev'] > 0:
                        ws.append((op['sem'], op['prev']))
                    for (s, v) in ws:
                        if waited.get(s, 0) >= v:
                            continue
                        waited[s] = v
                        eng.wait_ge(sems[s], v)
                    nm, a_, k_ = op['call']
                    ins = getattr(eng, nm)(*a_, **k_)
                    if op['dma']:
                        ins.then_inc(sems[op['sem']], 16)
                    elif op['mark']:
                        ins.then_inc(sems[op['sem']], 1)
                if eng_name == 'sp':
                    for k in range(self.n_dma_sems):
                        if dcnt[k] > 0:
                            eng.wait_ge(sems[('dma', k)], dcnt[k])

            @block.tensor
            def _(e):
                run('pe', e)

            @block.scalar
            def _(e):
                run('act', e)

            @block.vector
            def _(e):
                run('dve', e)

            @block.gpsimd
            def _(e):
                run('pool', e)

            @block.sync
            def _(e):
                run('sp', e)


def host_consts():
    c = {}
    i = np.arange(128)
    c['ident32'] = np.eye(128, dtype=np.float32)
    tri = (i[:, None] <= i[None, :]).astype(np.float32)
    c['tri32'] = tri
    c['ones32'] = np.ones((128, 128), np.float32)
    m63 = np.zeros((128, 128), np.float32)
    m63[:64, :] = 1.0
    c['m63'] = m63
    hm = tri * ((i[:, None] // 32) == (i[None, :] // 32))
    c['hmask'] = hm.astype(np.float32)
    rm = np.ones((128, TW), np.float32)
    rm[:, ::32] = 0.0
    c['rmask'] = rm
    return c


CONST_NAMES = ['ident32', 'tri32', 'ones32', 'm63', 'hmask', 'rmask']


def build(nseq, layers, final_norm=True, wq='pool', phases=('norm', 'prep', 'hgrn', 'wo1', 'fox', 'wo2', 'ple'), nheads=8, dbg=False):
    nc = bass.Bass("TRN2", target_bir_lowering=False)
    NL = DEPTH
    x_d = nc.dram_tensor("x", [nseq, S, D], F32, kind="ExternalInput").ap()
    p_d = nc.dram_tensor("p", [NL, nseq, S, PLE], F32, kind="ExternalInput").ap()
    win_d = nc.dram_tensor("w_in", [NL, D, DIN], F32, kind="ExternalInput").ap()
    wout_d = nc.dram_tensor("w_out", [NL, 2 * D, D], F32, kind="ExternalInput").ap()
    wple_d = nc.dram_tensor("w_ple", [NL, PLE, D], F32, kind="ExternalInput").ap()
    wpg_d = nc.dram_tensor("w_ple_gate", [NL, D, D], F32, kind="ExternalInput").ap()
    nw_d = nc.dram_tensor("v_nw", [128, NL * NCH], F32, kind="ExternalInput").ap()
    gn_d = nc.dram_tensor("v_gn", [128, NL * NCH], F32, kind="ExternalInput").ap()
    lbl_d = nc.dram_tensor("v_lbl", [128, NL * NCH], F32, kind="ExternalInput").ap()
    fnw_d = nc.dram_tensor("v_fnw", [128, NCH], F32, kind="ExternalInput").ap()
    fb_d = nc.dram_tensor("v_fb", [128, NL * 16], F32, kind="ExternalInput").ap()
    cd = {n: nc.dram_tensor("c_" + n, [128, TW if n == 'rmask' else 128], F32, kind="ExternalInput").ap() for n in CONST_NAMES}
    out_d = nc.dram_tensor("out", [nseq, S, D], F32, kind="ExternalOutput").ap()

    with ExitStack() as st:
        def sb(name, shape, dt):
            return st.enter_context(nc.sbuf_tensor(name, shape, dt))

        def pst(name, shape, dt):
            return st.enter_context(nc.psum_tensor(name, shape, dt))

        h = sb("h", [128, NCH, S], F32)
        uT = sb("uT", [128, NCH, S], BF16)
        yT = sb("yT", [128, NCH, S], BF16)
        NWB = 8
        wb = sb("wb", [128, NWB, NCH, 128], BF16)
        NF = 10
        ft = sb("ft", [128, NF, TW], F32)
        NBT = 10
        bt = sb("bt", [128, NBT, TW], BF16)
        kAB = sb("kAB", [128, 2, S], BF16)
        VA = sb("VA", [128, NB, 64], BF16)
        VB = sb("VB", [128, NB, 128], BF16)
        W32 = sb("W32", [128, 3, 128], F32)
        W16 = sb("W16", [128, 3, 128], BF16)
        Dall = sb("Dall", [128, 68], F32)
        sm = sb("sm", [128, 6, 256], F32)
        ident32 = sb("ident32", [128, 128], F32)
        identb = sb("identb", [128, 128], BF16)
        tri32 = sb("tri32", [128, 128], F32)
        trib = sb("trib", [128, 128], BF16)
        ones32 = sb("ones32", [128, 128], F32)
        m63 = sb("m63", [128, 128], F32)
        hmask = sb("hmask", [128, 128], F32)
        rmask = sb("rmask", [128, TW], F32)
        onesD = sb("onesD", [128, 128], BF16)
        onesV = sb("onesV", [128, 128], BF16)
        onesb = sb("onesb", [128, 128], BF16)
        nw = sb("nw", [128, NL * NCH], F32)
        gn = sb("gn", [128, NL * NCH], F32)
        lb = sb("lb", [128, NL * NCH], F32)
        oml = sb("oml", [128, NL * NCH], F32)
        lbe = sb("lbe", [128, NL * NCH], F32)
        lbs = sb("lbs", [128, NCH], F32)
        fnw = sb("fnw", [128, NCH], F32)
        fb = sb("fb", [128, NL * 16], F32)
        epsb = sb("epsb", [128, 1], F32)
        wz = sb("wz", [128, NCH, 16], BF16)
        wpl = sb("wpl", [128, 2, 2, 128], BF16)

        PS = [pst("ps%d" % k, [128, TW], F32) for k in range(7)]
        PSB = pst("psb", [128, 2 * TW], BF16)

        P = Prog(nc)
        A = P.add

        cmap = dict(ident32=ident32, tri32=tri32, ones32=ones32, m63=m63, hmask=hmask, rmask=rmask)
        for n in CONST_NAMES:
            A('sp', (lambda t, s_: (lambda e: e.dma_start(out=t[:], in_=s_[:, :])))(cmap[n], cd[n]), writes=[n], dma=True)
        for (t, s_, n) in ((nw, nw_d, 'nw'), (gn, gn_d, 'gn'), (lbe, lbl_d, 'lbe'), (fnw, fnw_d, 'fnw'), (fb, fb_d, 'fb')):
            A('sp', (lambda t, s_: (lambda e: e.dma_start(out=t[:], in_=s_[:, :])))(t, s_), writes=[n], dma=True)
        A('pool', lambda e: e.tensor_copy(out=identb[:], in_=ident32[:]), reads=['ident32'], writes=['identb'])
        A('pool', lambda e: e.tensor_copy(out=trib[:], in_=tri32[:]), reads=['tri32'], writes=['trib'])
        A('pool', lambda e: e.memset(onesD[:], 1.0 / D), writes=['onesD'])
        A('pool', lambda e: e.memset(onesV[:], 1.0 / 128), writes=['onesV'])
        A('pool', lambda e: e.memset(onesb[:], 1.0), writes=['onesb'])
        A('pool', lambda e: e.memset(epsb[:], EPS), writes=['epsb'])
        A('pool', lambda e: e.memset(kAB[:, 1, :], 0.0), writes=['kB'])
        A('pool', lambda e: e.memset(kAB[64:65, 0, :], 1.0), writes=['kA'])
        A('pool', lambda e: e.memset(kAB[0:1, 1, :], 1.0), reads=['kB'], writes=['kB'])
        A('pool', lambda e: e.memset(VA[:], 1.0), writes=['VA'])
        A('pool', lambda e: e.memset(VB[:], 0.0), writes=['VB'])
        A('pool', lambda e: e.memset(VB[:, :, 0:1], 1.0), reads=['VB'], writes=['VB'])
        A('act', lambda e: e.activation(out=lbe[:], in_=lbe[:], func=AF.Exp), reads=['lbe'], writes=['lbe'])
        A('dve', lambda e: e.tensor_tensor(out=lbs[:], in0=lbe[:, 0:NCH], in1=lbe[:, NCH:2 * NCH], op=ALU.add), reads=['lbe'], writes=['lbs'])
        A('dve', lambda e: e.tensor_tensor(out=lbs[:], in0=lbs[:], in1=lbe[:, 2 * NCH:3 * NCH], op=ALU.add), reads=['lbe', 'lbs'], writes=['lbs'])
        A('dve', lambda e: e.tensor_tensor(out=lbs[:], in0=lbs[:], in1=lbe[:, 3 * NCH:4 * NCH], op=ALU.add), reads=['lbe', 'lbs'], writes=['lbs'])
        A('dve', lambda e: e.reciprocal(out=lbs[:], in_=lbs[:]), reads=['lbs'], writes=['lbs'])
        A('dve', lambda e: e.memset(lb[:, 0:NCH], 0.0), writes=['lb'])
        for l in range(1, NL):
            A('dve', (lambda l: (lambda e: e.tensor_tensor(out=lbe[:, l * NCH:(l + 1) * NCH], in0=lbe[:, l * NCH:(l + 1) * NCH], in1=lbs[:], op=ALU.mult)))(l),
              reads=['lbe', 'lbs'], writes=['lbe'])
            A('dve', (lambda l: (lambda e: e.tensor_tensor(out=lb[:, l * NCH:(l + 1) * NCH], in0=lb[:, (l - 1) * NCH:l * NCH], in1=lbe[:, l * NCH:(l + 1) * NCH], op=ALU.add)))(l),
              reads=['lbe', 'lb'], writes=['lb'])
        A('dve', lambda e: e.tensor_scalar(out=oml[:], in0=lb[:], scalar1=-1.0, scalar2=1.0, op0=ALU.mult, op1=ALU.add), reads=['lb'], writes=['oml'])

        wslot = [0]

        def wload(src_ap, nk=NCH, ncols=128):
            k = wslot[0] % NWB
            wslot[0] += 1
            dst = wb[:, k, 0:nk, 0:ncols]
            A(wq, lambda e: e.dma_start(out=dst, in_=src_ap), writes=[('wb', k)], dma=True)
            return k

        def win_chunk(l, col0, ncols=128):
            return wload(win_d[l].rearrange("(c p) e -> p c e", p=128)[:, :, col0:col0 + ncols], NCH, ncols)

        def proj_fm(k, t0, tw, ps_ap, psres, ncols=128):
            for c in range(NCH):
                A('pe', (lambda c: (lambda e: e.matmul(ps_ap, lhsT=wb[:, k, c, 0:ncols], rhs=uT[:, c, t0:t0 + tw], start=(c == 0), stop=(c == NCH - 1))))(c),
                  reads=[('wb', k), ('uT', c, t0 // TW)], writes=psres)

        def proj_tm(k, tb, ps_ap, psres, ncols=128):
            for c in range(NCH):
                A('pe', (lambda c: (lambda e: e.matmul(ps_ap, lhsT=uT[:, c, tb * 128:(tb + 1) * 128], rhs=wb[:, k, c, 0:ncols], start=(c == 0), stop=(c == NCH - 1))))(c),
                  reads=[('wb', k), ('uT', c, tb // 4)], writes=psres)

        def rmsnorm_to_u(l):
            for t in range(NT):
                ts = slice(t * TW, (t + 1) * TW)
                sq_list = []
                for c in range(NCH):
                    slot = c % 4
                    A('act', (lambda c, slot: (lambda e: e.activation(out=bt[:, slot, :], in_=h[:, c, ts], func=AF.Square)))(c, slot),
                      reads=[('h', c, t)], writes=[('bt', slot)])
                    A('pe', (lambda c, slot: (lambda e: e.matmul(PS[0][:], lhsT=onesD[:], rhs=bt[:, slot, :], start=(c == 0), stop=(c == NCH - 1))))(c, slot),
                      reads=['onesD', ('bt', slot)], writes=[('ps', 0)])
                A('act', lambda e: e.activation(out=ft[:, 0, :], in_=PS[0][:], func=AF.Sqrt, bias=epsb[:], scale=1.0),
                  reads=[('ps', 0), 'epsb'], writes=[('ft', 0)])
                A('dve', lambda e: e.reciprocal(out=ft[:, 0, :], in_=ft[:, 0, :]), reads=[('ft', 0)], writes=[('ft', 0)])
                for c in range(NCH):
                    A('dve', (lambda c: (lambda e: e.scalar_tensor_tensor(out=uT[:, c, ts], in0=h[:, c, ts], scalar=nw[:, l * NCH + c:l * NCH + c + 1],
                                                                         in1=ft[:, 0, :], op0=ALU.mult, op1=ALU.mult)))(c),
                      reads=[('h', c, t), ('ft', 0), 'nw'], writes=[('uT', c, t)])

        def load_x(s):
            for tb in range(NB):
                stg = ft[:, 8:10, :]
                A('sp', (lambda tb: (lambda e: e.dma_start(out=stg, in_=x_d[s, tb * 128:(tb + 1) * 128, :].rearrange("p (a b) -> p a b", a=2))))(tb),
                  writes=[('ft', 8), ('ft', 9)], dma=True)
                for half in range(2):
                    pk = 1 + half
                    for cc in range(4):
                        c = half * 4 + cc
                        A('pe', (lambda c, cc, pk, half: (lambda e: e.transpose(out=PS[pk][:, cc * 128:(cc + 1) * 128], in_=ft[:, 8 + half, cc * 128:(cc + 1) * 128], identity=ident32[:])))(c, cc, pk, half),
                          reads=[('ft', 8 + half), 'ident32'], writes=[('ps', pk)])
                    eng = 'act' if half == 0 else 'dve'
                    if eng == 'act':
                        A('act', (lambda pk, half, tb: (lambda e: e.activation(out=h[:, half * 4:half * 4 + 4, tb * 128:(tb + 1) * 128],
                                                                              in_=PS[pk][:].rearrange("p (a b) -> p a b", a=4), func=AF.Copy)))(pk, half, tb),
                          reads=[('ps', pk)], writes=[('h', half * 4 + cc, tb // 4) for cc in range(4)])
                    else:
                        A('dve', (lambda pk, half, tb: (lambda e: e.tensor_copy(out=h[:, half * 4:half * 4 + 4, tb * 128:(tb + 1) * 128],
                                                                               in_=PS[pk][:].rearrange("p (a b) -> p a b", a=4))))(pk, half, tb),
                          reads=[('ps', pk)], writes=[('h', half * 4 + cc, tb // 4) for cc in range(4)])

        def store_out(s, normed):
            for t in range(NT):
                ts = slice(t * TW, (t + 1) * TW)
                if normed:
                    for c in range(NCH):
                        slot = c % 4
                        A('act', (lambda c, slot: (lambda e: e.activation(out=bt[:, slot, :], in_=h[:, c, ts], func=AF.Square)))(c, slot),
                          reads=[('h', c, t)], writes=[('bt', slot)])
                        A('pe', (lambda c, slot: (lambda e: e.matmul(PS[0][:], lhsT=onesD[:], rhs=bt[:, slot, :], start=(c == 0), stop=(c == NCH - 1))))(c, slot),
                          reads=['onesD', ('bt', slot)], writes=[('ps', 0)])
                    A('act', lambda e: e.activation(out=ft[:, 0, :], in_=PS[0][:], func=AF.Sqrt, bias=epsb[:], scale=1.0),
                      reads=[('ps', 0), 'epsb'], writes=[('ft', 0)])
                    A('dve', lambda e: e.reciprocal(out=ft[:, 0, :], in_=ft[:, 0, :]), reads=[('ft', 0)], writes=[('ft', 0)])
                    for c in range(NCH):
                        A('dve', (lambda c: (lambda e: e.scalar_tensor_tensor(out=h[:, c, ts], in0=h[:, c, ts], scalar=fnw[:, c:c + 1],
                                                                             in1=ft[:, 0, :], op0=ALU.mult, op1=ALU.mult)))(c),
                          reads=[('h', c, t), ('ft', 0), 'fnw'], writes=[('h', c, t)])
                for q in range(4):
                    tb = t * 4 + q
                    for half in range(2):
                        pk = 1 + half
                        for cc in range(4):
                            c = half * 4 + cc
                            A('pe', (lambda c, cc, pk, tb: (lambda e: e.transpose(out=PS[pk][:, cc * 128:(cc + 1) * 128], in_=h[:, c, tb * 128:(tb + 1) * 128], identity=ident32[:])))(c, cc, pk, tb),
                              reads=[('h', c, t), 'ident32'], writes=[('ps', pk)])
                        if half == 0:
                            A('act', (lambda pk, half: (lambda e: e.activation(out=ft[:, 8 + half, :], in_=PS[pk][:], func=AF.Copy)))(pk, half),
                              reads=[('ps', pk)], writes=[('ft', 8 + half)])
                        else:
                            A('dve', (lambda pk, half: (lambda e: e.tensor_copy(out=ft[:, 8 + half, :], in_=PS[pk][:])))(pk, half),
                              reads=[('ps', pk)], writes=[('ft', 8 + half)])
                    A('sp', (lambda tb: (lambda e: e.dma_start(out=out_d[s, tb * 128:(tb + 1) * 128, :].rearrange("p (a b) -> p a b", a=2), in_=ft[:, 8:10, :])))(tb),
                      reads=[('ft', 8), ('ft', 9)], dma=True)

        def hgrn_head(l, hh):
            kq = win_chunk(l, 0 * D + hh * 128)
            kf = win_chunk(l, 1 * D + hh * 128)
            ki = win_chunk(l, 2 * D + hh * 128)
            kg = win_chunk(l, 3 * D + hh * 128)
            lc = l * NCH + hh
            lb_ap = lb[:, lc:lc + 1]
            oml_ap = oml[:, lc:lc + 1]
            gn_ap = gn[:, lc:lc + 1]
            A('pool', lambda e: e.memset(W32[:, 2, :], 0.0), writes=[('W32', 2)])
            A('pool', lambda e: e.memset(W16[:, 2, :], 0.0), writes=[('W16', 2)])
            A('pool', lambda e: e.memset(Dall[:, 0:1], 1.0), writes=['Dall'])
            for sg in range(NT):
                t0 = sg * TW
                proj_fm(kf, t0, TW, PS[0][:], [('ps', 0)])
                A('act', lambda e: e.activation(out=ft[:, 1, :], in_=PS[0][:], func=AF.Sigmoid), reads=[('ps', 0)], writes=[('ft', 1)])
                A('act', lambda e: e.activation(out=ft[:, 2, :], in_=PS[0][:], func=AF.Sigmoid, scale=-1.0), reads=[('ps', 0)], writes=[('ft', 2)])
                A('dve', lambda e: e.tensor_scalar(out=ft[:, 1, :], in0=ft[:, 1, :], scalar1=oml_ap, scalar2=lb_ap, op0=ALU.mult, op1=ALU.add),
                  reads=[('ft', 1), 'oml', 'lb'], writes=[('ft', 1)])
                A('act', lambda e: e.activation(out=ft[:, 3, :], in_=ft[:, 1, :], func=AF.Ln), reads=[('ft', 1)], writes=[('ft', 3)])
                A('dve', lambda e: e.tensor_tensor_scan(out=ft[:, 4, :], data0=rmask[:], data1=ft[:, 3, :], initial=0.0, op0=ALU.mult, op1=ALU.add),
                  reads=[('ft', 3), 'rmask'], writes=[('ft', 4)])
                A('act', lambda e: e.activation(out=ft[:, 5, :], in_=ft[:, 4, :], func=AF.Exp), reads=[('ft', 4)], writes=[('ft', 5)])
                A('act', lambda e: e.activation(out=ft[:, 6, :], in_=ft[:, 4, :], func=AF.Exp, scale=-1.0), reads=[('ft', 4)], writes=[('ft', 6)])
                A('dve', lambda e: e.scalar_tensor_tensor(out=bt[:, 4, :], in0=ft[:, 2, :], scalar=oml_ap, in1=ft[:, 6, :], op0=ALU.mult, op1=ALU.mult),
                  reads=[('ft', 2), ('ft', 6), 'oml'], writes=[('bt', 4)])
                A('pool', (lambda sg: (lambda e: e.tensor_copy(out=Dall[:, 1 + 16 * sg:1 + 16 * sg + 16], in_=ft[:, 5, 31::32])))(sg),
                  reads=[('ft', 5), 'Dall'], writes=['Dall'])
                proj_fm(kq, t0, TW, PS[1][:], [('ps', 1)])
                A('dve', lambda e: e.scalar_tensor_tensor(out=bt[:, 5, :], in0=PS[1][:], scalar=float(128 ** -0.5), in1=ft[:, 5, :], op0=ALU.mult, op1=ALU.mult),
                  reads=[('ps', 1), ('ft', 5)], writes=[('bt', 5)])
                A('pool', (lambda sg: (lambda e: e.tensor_tensor(out=bt[:, 6, :].rearrange("p (a b) -> p a b", b=32), in0=bt[:, 5, :].rearrange("p (a b) -> p a b", b=32),
                                                                 in1=bcast_last(Dall[:, 16 * sg:16 * sg + 16], 32), op=ALU.mult)))(sg),
                  reads=[('bt', 5), 'Dall'], writes=[('bt', 6)])
                proj_fm(kg, t0, TW, PS[2][:], [('ps', 2)])
                A('act', lambda e: e.activation(out=ft[:, 7, :], in_=PS[2][:], func=AF.Silu), reads=[('ps', 2)], writes=[('ft', 7)])
                for q in range(4):
                    proj_tm(ki, sg * 4 + q, PS[3][:, q * 128:(q + 1) * 128], [('ps', 3)])
                A('act', lambda e: e.activation(out=bt[:, 9, :], in_=PS[3][:], func=AF.Copy), reads=[('ps', 3)], writes=[('bt', 9)])
                for q in range(4):
                    A('pe', (lambda q: (lambda e: e.transpose(out=PSB[:, q * 128:(q + 1) * 128], in_=bt[:, 4, q * 128:(q + 1) * 128], identity=identb[:])))(q),
                      reads=[('bt', 4), 'identb'], writes=['psb'])
                A('dve', lambda e: e.tensor_copy(out=bt[:, 8, :], in_=PSB[:, 0:TW]), reads=['psb'], writes=[('bt', 8)])
                for q in range(4):
                    A('pe', (lambda q: (lambda e: e.matmul(PS[4][:, q * 128:(q + 1) * 128], lhsT=bt[:, 4, q * 128:(q + 1) * 128], rhs=bt[:, 5, q * 128:(q + 1) * 128], start=True, stop=True)))(q),
                      reads=[('bt', 4), ('bt', 5)], writes=[('ps', 4)])
                A('dve', lambda e: e.tensor_tensor(out=bt[:, 3, :].rearrange("p (a b) -> p a b", a=4), in0=PS[4][:].rearrange("p (a b) -> p a b", a=4),
                                                   in1=bcast_mid(hmask[:], 4), op=ALU.mult),
                  reads=[('ps', 4), 'hmask'], writes=[('bt', 3)])
                for q in range(4):
                    A('pe', (lambda q: (lambda e: e.matmul(PS[5][:, q * 128:(q + 1) * 128], lhsT=bt[:, 9, q * 128:(q + 1) * 128], rhs=bt[:, 3, q * 128:(q + 1) * 128], start=True, stop=False)))(q),
                      reads=[('bt', 9), ('bt', 3)], writes=[('ps', 5)])
                    for cc in range(4):
                        cg = sg * 16 + q * 4 + cc
                        prev = (cg + 2) % 3
                        cur = cg % 3
                        cs = slice(q * 128 + cc * 32, q * 128 + cc * 32 + 32)
                        A('pe', (lambda cs, prev, cc: (lambda e: e.matmul(PS[5][:, cs], lhsT=W16[:, prev, :], rhs=bt[:, 6, cs], start=False, stop=(cc == 3))))(cs, prev, cc),
                          reads=[('W16', prev), ('bt', 6)], writes=[('ps', 5)])
                        us = slice((cg % 4) * 128, (cg % 4) * 128 + 128)
                        rows = slice(cc * 32, cc * 32 + 32)
                        A('pe', (lambda us, rows, q, cc: (lambda e: e.matmul(PS[6][:, us], lhsT=bt[rows, 8, q * 128:(q + 1) * 128], rhs=bt[rows, 9, q * 128:(q + 1) * 128],
                                                                           start=True, stop=True, tile_position=(cc * 32, 0))))(us, rows, q, cc),
                          reads=[('bt', 8), ('bt', 9)], writes=[('psu', cg % 4)])
                        A('dve', (lambda us, prev, cur, cg: (lambda e: e.scalar_tensor_tensor(out=W32[:, cur, :], in0=W32[:, prev, :], scalar=Dall[:, cg:cg + 1], in1=PS[6][:, us],
                                                                                           op0=ALU.mult, op1=ALU.add)))(us, prev, cur, cg),
                          reads=[('W32', prev), 'Dall', ('psu', cg % 4)], writes=[('W32', cur)])
                        A('pool', (lambda cur: (lambda e: e.tensor_copy(out=W16[:, cur, :], in_=W32[:, cur, :])))(cur),
                          reads=[('W32', cur)], writes=[('W16', cur)])
                A('act', lambda e: e.activation(out=bt[:, 7, :], in_=PS[5][:], func=AF.Square), reads=[('ps', 5)], writes=[('bt', 7)])
                A('pe', lambda e: e.matmul(PS[0][:], lhsT=onesV[:], rhs=bt[:, 7, :], start=True, stop=True), reads=['onesV', ('bt', 7)], writes=[('ps', 0)])
                A('act', lambda e: e.activation(out=ft[:, 0, :], in_=PS[0][:], func=AF.Sqrt, bias=epsb[:], scale=1.0), reads=[('ps', 0), 'epsb'], writes=[('ft', 0)])
                A('dve', lambda e: e.reciprocal(out=ft[:, 0, :], in_=ft[:, 0, :]), reads=[('ft', 0)], writes=[('ft', 0)])
                A('dve', lambda e: e.scalar_tensor_tensor(out=ft[:, 0, :], in0=PS[5][:], scalar=gn_ap, in1=ft[:, 0, :], op0=ALU.mult, op1=ALU.mult),
                  reads=[('ps', 5), ('ft', 0), 'gn'], writes=[('ft', 0)])
                A('pool', (lambda t0, sg: (lambda e: e.tensor_tensor(out=yT[:, hh, t0:t0 + TW], in0=ft[:, 0, :], in1=ft[:, 7, :], op=ALU.mult)))(t0, sg),
                  reads=[('ft', 0), ('ft', 7)], writes=[('yT', hh, sg)])

        def bcast_last(ap2, n):
            return ap2.unsqueeze(2).to_broadcast([ap2.shape[0], ap2.shape[1], n])

        def bcast_mid(ap2, n):
            return ap2.unsqueeze(1).to_broadcast([ap2.shape[0], n, ap2.shape[1]])

        PS6 = [('psu', i) for i in range(4)]

        def psr(k):
            return PS6 if k == 6 else [('ps', k)]

        def wout_pass(l, half, swap):
            for dm in range(NCH):
                src = wout_d[l, half * D:(half + 1) * D, dm * 128:(dm + 1) * 128].rearrange("(c p) e -> p c e", p=128)
                if not swap:
                    k = wload(src)
                else:
                    k = wload(src)
                for t in range(NT):
                    pk = 1 + (t % 2)
                    for c in range(NCH):
                        A('pe', (lambda c, t, pk: (lambda e: e.matmul(PS[pk][:], lhsT=wb[:, k, c, :], rhs=yT[:, c, t * TW:(t + 1) * TW], start=(c == 0), stop=(c == NCH - 1))))(c, t, pk),
                          reads=[('wb', k), ('yT', c, t)], writes=[('ps', pk)])
                    A('dve', (lambda t, pk, dm: (lambda e: e.tensor_tensor(out=h[:, dm, t * TW:(t + 1) * TW], in0=h[:, dm, t * TW:(t + 1) * TW], in1=PS[pk][:], op=ALU.add)))(t, pk, dm),
                      reads=[('ps', pk), ('h', dm, t)], writes=[('h', dm, t)])

        def fox_prep(l):
            A(wq, lambda e: e.dma_start(out=wz[:], in_=win_d[l].rearrange("(c p) e -> p c e", p=128)[:, :, 8 * D:8 * D + 16]), writes=['wz'], dma=True)
            for tb in range(NB):
                for c in range(NCH):
                    A('pe', (lambda c, tb: (lambda e: e.matmul(PS[0][:, tb * 16:(tb + 1) * 16], lhsT=uT[:, c, tb * 128:(tb + 1) * 128], rhs=wz[:, c, :], start=(c == 0), stop=(c == NCH - 1))))(c, tb),
                      reads=['wz', ('uT', c, tb // 4)], writes=[('ps', 0)])
            A('dve', lambda e: e.tensor_tensor(out=sm[:, 0, :].rearrange("p (a b) -> p a b", b=16), in0=PS[0][:, 0:256].rearrange("p (a b) -> p a b", b=16),
                                               in1=bcast_mid(fb[:, l * 16:(l + 1) * 16], 16), op=ALU.add),
              reads=[('ps', 0), 'fb'], writes=[('sm', 0)])
            A('act', lambda e: e.activation(out=sm[:, 0, :], in_=sm[:, 0, :], func=AF.Sigmoid), reads=[('sm', 0)], writes=[('sm', 0)])
            A('act', lambda e: e.activation(out=sm[:, 0, :], in_=sm[:, 0, :], func=AF.Ln), reads=[('sm', 0)], writes=[('sm', 0)])
            A('pe', lambda e: e.matmul(PS[1][:, 0:256], lhsT=tri32[:], rhs=sm[:, 0, :], start=True, stop=True), reads=['tri32', ('sm', 0)], writes=[('ps', 1)])
            A('pe', lambda e: e.matmul(PS[2][:, 0:256], lhsT=ones32[:], rhs=sm[:, 0, :], start=True, stop=True), reads=['ones32', ('sm', 0)], writes=[('ps', 2)])
            A('pe', lambda e: e.matmul(PS[3][:, 0:256], lhsT=m63[:], rhs=sm[:, 0, :], start=True, stop=True), reads=['m63', ('sm', 0)], writes=[('ps', 3)])
            A('act', lambda e: e.activation(out=sm[:, 4, :], in_=PS[2][:, 0:256], func=AF.Copy), reads=[('ps', 2)], writes=[('sm', 4)])
            A('dve', lambda e: e.memset(sm[:, 1, 0:16], 0.0), writes=[('sm', 1)])
            for j in range(1, NB):
                A('dve', (lambda j: (lambda e: e.tensor_tensor(out=sm[:, 1, j * 16:(j + 1) * 16], in0=sm[:, 1, (j - 1) * 16:j * 16], in1=sm[:, 4, (j - 1) * 16:j * 16], op=ALU.add)))(j),
                  reads=[('sm', 1), ('sm', 4)], writes=[('sm', 1)])
            A('dve', lambda e: e.scalar_tensor_tensor(out=sm[:, 2, :], in0=PS[1][:, 0:256], scalar=-1.0, in1=sm[:, 1, :], op0=ALU.mult, op1=ALU.subtract),
              reads=[('ps', 1), ('sm', 1)], writes=[('sm', 2)])
            A('dve', lambda e: e.tensor_tensor(out=sm[:, 3, :], in0=PS[3][:, 0:256], in1=sm[:, 1, :], op=ALU.add), reads=[('ps', 3), ('sm', 1)], writes=[('sm', 3)])
            A('dve', lambda e: e.tensor_scalar(out=sm[:, 3, :], in0=sm[:, 3, :], scalar1=8.0, scalar2=None, op0=ALU.mult), reads=[('sm', 3)], writes=[('sm', 3)])

        def fox_pair(l, pp):
            base = 4 * D
            kfq = win_chunk(l, base + 0 * D + pp * 128)
            kfk = win_chunk(l, base + 1 * D + pp * 128)
            kfv = win_chunk(l, base + 2 * D + pp * 128)
            kfg = win_chunk(l, base + 3 * D + pp * 128)
            hA, hB = 2 * pp, 2 * pp + 1
            for t in range(NT):
                ts = slice(t * TW, (t + 1) * TW)
                pk = t % 2
                proj_fm(kfk, t * TW, TW, PS[pk][:], [('ps', pk)])
                if t % 2 == 0:
                    A('act', (lambda pk, ts: (lambda e: e.activation(out=kAB[:, 0, ts], in_=PS[pk][:], func=AF.Copy)))(pk, ts), reads=[('ps', pk)], writes=['kA'])
                else:
                    A('dve', (lambda pk, ts: (lambda e: e.tensor_copy(out=kAB[:, 0, ts], in_=PS[pk][:])))(pk, ts), reads=[('ps', pk)], writes=['kA'])
            for g in range(4):
                pk = g % 2
                for q in range(4):
                    proj_tm(kfv, g * 4 + q, PS[pk][:, q * 128:(q + 1) * 128], [('ps', pk)])
                if g % 2 == 0:
                    A('act', (lambda pk, g: (lambda e: e.activation(out=VB[:, g * 4:(g + 1) * 4, :], in_=PS[pk][:].rearrange("p (a b) -> p a b", a=4), func=AF.Copy)))(pk, g),
                      reads=[('ps', pk)], writes=['VB'])
                else:
                    A('dve', (lambda pk, g: (lambda e: e.tensor_copy(out=VB[:, g * 4:(g + 1) * 4, :], in_=PS[pk][:].rearrange("p (a b) -> p a b", a=4))))(pk, g),
                      reads=[('ps', pk)], writes=['VB'])
            import os as _os
            FST = int(_os.environ.get('FOXSTAGE', '9'))
            for qi in range(NT if FST >= 2 else 0):
                ts = slice(qi * TW, (qi + 1) * TW)
                qa, qb = 0 + (qi % 2) * 2, 1 + (qi % 2) * 2
                pk = 2
                proj_fm(kfq, qi * TW, TW, PS[pk][:], [('ps', pk)])
                A('act', (lambda pk, qa: (lambda e: e.activation(out=bt[:, qa, :], in_=PS[pk][:], func=AF.Copy)))(pk, qa), reads=[('ps', pk)], writes=[('bt', qa)])
                for hd_, hx_ in ((0, hA), (1, hB)):
                    A('pool', (lambda hd_, hx_, qi: (lambda e: e.tensor_copy(out=bt[0:1, 8 + hd_, :].rearrange("p (a b) -> p a b", a=4),
                                                                             in_=bcast_last(sm[0:1, 3, :].rearrange("p (a b) -> p a b", b=16)[:, 4 * qi:4 * qi + 4, hx_], 128))))(hd_, hx_, qi),
                      reads=[('sm', 3)], writes=[('bt', 8 + hd_)])
                pg = 2
                proj_fm(kfg, qi * TW, TW, PS[pg][:], [('ps', pg)])
                A('act', lambda e: e.activation(out=ft[:, 7, :], in_=PS[pg][:], func=AF.Silu), reads=[('ps', pg)], writes=[('ft', 7)])
                for hd in range(2 if FST >= 3 else 0):
                    hidx = hA if hd == 0 else hB
                    qs = qa if hd == 0 else qb
                    rows = slice(0, 64) if hd == 0 else slice(64, 128)
                    po = 5 + hd
                    nkb = 4 * qi + 4
                    for j in range(nkb):
                        r = j - 4 * qi
                        c0 = max(r, 0) * 128
                        psk = 3 + (j % 2)
                        ptk = 4 + (j % 4)
                        A('pe', (lambda j, c0, psk, rows, qa: (lambda e: e.matmul(PS[psk][:, c0:TW], lhsT=kAB[rows, 0, j * 128:(j + 1) * 128], rhs=bt[rows, qa, c0:TW], start=True, stop=False)))(j, c0, psk, rows, qa),
                          reads=['kA', ('bt', qa)], writes=[('ps', psk)])
                        A('pe', (lambda c0, psk, hd: (lambda e: e.matmul(PS[psk][:, c0:TW], lhsT=onesb[0:1, :], rhs=bt[0:1, 8 + hd, c0:TW], start=False, stop=True)))(c0, psk, hd),
                          reads=['onesb', ('bt', 8 + hd)], writes=[('ps', psk)])
                        A('act', (lambda j, c0, psk, ptk, hidx: (lambda e: e.activation(out=bt[:, ptk, c0:TW], in_=PS[psk][:, c0:TW], func=AF.Exp,
                                                                                      bias=sm[:, 2, j * 16 + hidx:j * 16 + hidx + 1], scale=0.125)))(j, c0, psk, ptk, hidx),
                          reads=[('ps', psk), ('sm', 2)], writes=[('bt', ptk)])
                        if r >= 0:
                            A('pool', (lambda c0, ptk: (lambda e: e.tensor_tensor(out=bt[:, ptk, c0:c0 + 128], in0=bt[:, ptk, c0:c0 + 128], in1=trib[:], op=ALU.mult)))(c0, ptk),
                              reads=[('bt', ptk), 'trib'], writes=[('bt', ptk)])
                        A('pe', (lambda j, c0, ptk, hd, nkb: (lambda e: e.matmul(PS[hd][:, c0:TW], lhsT=onesb[:], rhs=bt[:, ptk, c0:TW], start=(j == 0), stop=(j == nkb - 1))))(j, c0, ptk, hd, nkb),
                          reads=['onesb', ('bt', ptk)], writes=[('ps', hd)])
                        if hd == 0:
                            A('pe', (lambda j, c0, ptk, po, nkb: (lambda e: e.matmul(PS[po][0:64, c0:TW], lhsT=VB[:, j, 0:64], rhs=bt[:, ptk, c0:TW], start=(j == 0), stop=(j == nkb - 1))))(j, c0, ptk, po, nkb),
                              reads=['VB', ('bt', ptk)], writes=psr(po))
                        else:
                            A('pe', (lambda j, c0, ptk, po, nkb: (lambda e: e.matmul(PS[po][:, c0:TW], lhsT=VB[:, j, :], rhs=bt[:, ptk, c0:TW], start=(j == 0), stop=(j == nkb - 1))))(j, c0, ptk, po, nkb),
                              reads=['VB', ('bt', ptk)], writes=psr(po))
                    orow = slice(0, 64) if hd == 0 else slice(64, 128)
                    A('dve', (lambda orow, hd: (lambda e: e.reciprocal(out=ft[orow, 1, :], in_=PS[hd][orow, :])))(orow, hd), reads=[('ps', hd)], writes=[('ft', 1)])
                    A('dve', (lambda orow: (lambda e: e.tensor_tensor(out=ft[orow, 2, :], in0=ft[orow, 1, :], in1=ft[orow, 7, :], op=ALU.mult)))(orow),
                      reads=[('ft', 1), ('ft', 7)], writes=[('ft', 2)])
                    A('dve', (lambda orow, po, qi: (lambda e: e.tensor_tensor(out=yT[orow, pp, qi * TW:(qi + 1) * TW], in0=PS[po][orow, :], in1=ft[orow, 2, :], op=ALU.mult)))(orow, po, qi),
                      reads=psr(po) + [('ft', 2)], writes=[('yT', pp, qi)])

        def ple_phase(l, s):
            for c in range(NCH):
                for t in range(NT):
                    eng = 'pool' if (c + t) % 2 == 0 else 'act'
                    if eng == 'pool':
                        A('pool', (lambda c, t: (lambda e: e.tensor_copy(out=uT[:, c, t * TW:(t + 1) * TW], in_=h[:, c, t * TW:(t + 1) * TW])))(c, t),
                          reads=[('h', c, t)], writes=[('uT', c, t)])
                    else:
                        A('act', (lambda c, t: (lambda e: e.activation(out=uT[:, c, t * TW:(t + 1) * TW], in_=h[:, c, t * TW:(t + 1) * TW], func=AF.Copy)))(c, t),
                          reads=[('h', c, t)], writes=[('uT', c, t)])
            for t in range(NT):
                stg = ft[:, 8:10, :]
                A('sp', (lambda t: (lambda e: e.dma_start(out=stg.rearrange("p a (q k) -> p (a q) k", k=PLE), in_=p_d[l, s, t * TW:(t + 1) * TW, :].rearrange("(q p) k -> p q k", p=128))))(t),
                  writes=[('ft', 8), ('ft', 9)], dma=True)
                for kc in range(2):
                    pk = 1 + kc
                    for q in range(4):
                        a, qq = divmod(q, 2)
                        A('pe', (lambda kc, q, a, qq, pk: (lambda e: e.transpose(out=PS[pk][:, q * 128:(q + 1) * 128], in_=ft[:, 8 + a, qq * PLE + kc * 128:qq * PLE + kc * 128 + 128], identity=ident32[:])))(kc, q, a, qq, pk),
                          reads=[('ft', 8 + a), 'ident32'], writes=[('ps', pk)])
                    if kc == 0:
                        A('act', (lambda pk, t: (lambda e: e.activation(out=kAB[:, 0, t * TW:(t + 1) * TW], in_=PS[pk][:], func=AF.Copy)))(pk, t), reads=[('ps', pk)], writes=['kA'])
                    else:
                        A('dve', (lambda pk, t: (lambda e: e.tensor_copy(out=kAB[:, 1, t * TW:(t + 1) * TW], in_=PS[pk][:])))(pk, t), reads=[('ps', pk)], writes=['kB'])
            for dm in range(NCH):
                kg = wload(wpg_d[l, :, dm * 128:(dm + 1) * 128].rearrange("(c p) e -> p c e", p=128))
                kp = wload(wple_d[l, :, dm * 128:(dm + 1) * 128].rearrange("(c p) e -> p c e", p=128), nk=2)
                for t in range(NT):
                    ts = slice(t * TW, (t + 1) * TW)
                    pa, pb = 3 + (t % 2), 5 + (t % 2)
                    for c in range(NCH):
                        A('pe', (lambda c, pa, ts, t: (lambda e: e.matmul(PS[pa][:], lhsT=wb[:, kg, c, :], rhs=uT[:, c, ts], start=(c == 0), stop=(c == NCH - 1))))(c, pa, ts, t),
                          reads=[('wb', kg), ('uT', c, t)], writes=[('ps', pa)])
                    for kc in range(2):
                        A('pe', (lambda kc, pb, ts: (lambda e: e.matmul(PS[pb][:], lhsT=wb[:, kp, kc, :], rhs=kAB[:, kc, ts], start=(kc == 0), stop=(kc == 1))))(kc, pb, ts),
                          reads=[('wb', kp), 'kA', 'kB'], writes=psr(pb))
                    f1 = 1 + (t % 2)
                    A('act', (lambda pa, f1: (lambda e: e.activation(out=ft[:, f1, :], in_=PS[pa][:], func=AF.Sigmoid)))(pa, f1), reads=[('ps', pa)], writes=[('ft', f1)])
                    A('dve', (lambda pb, f1: (lambda e: e.tensor_tensor(out=ft[:, f1, :], in0=PS[pb][:], in1=ft[:, f1, :], op=ALU.mult)))(pb, f1), reads=psr(pb) + [('ft', f1)], writes=[('ft', f1)])
                    A('pool', (lambda dm, ts, f1, t: (lambda e: e.tensor_tensor(out=h[:, dm, ts], in0=h[:, dm, ts], in1=ft[:, f1, :], op=ALU.add)))(dm, ts, f1, t),
                      reads=[('h', dm, t), ('ft', f1)], writes=[('h', dm, t)])

        for s in range(nseq):
            load_x(s)
            for l in layers:
                if 'norm' in phases:
                    rmsnorm_to_u(l)
                if 'prep' in phases:
                    fox_prep(l)
                if 'hgrn' in phases:
                    for hh in range(nheads):
                        hgrn_head(l, hh)
                if 'wo1' in phases:
                    wout_pass(l, 0, False)
                if 'fox' in phases:
                    for pp in range(nheads):
                        fox_pair(l, pp)
                if 'wo2' in phases:
                    wout_pass(l, 1, False)
                if 'ple' in phases:
                    ple_phase(l, s)
            store_out(s, final_norm)
        if dbg:
            dft = nc.dram_tensor("dbg_ft", [128, NF, TW], F32, kind="ExternalOutput").ap()
            dbt = nc.dram_tensor("dbg_bt", [128, NBT, TW], BF16, kind="ExternalOutput").ap()
            dy = nc.dram_tensor("dbg_y", [128, NCH, S], BF16, kind="ExternalOutput").ap()
            du = nc.dram_tensor("dbg_u", [128, NCH, S], BF16, kind="ExternalOutput").ap()
            dD = nc.dram_tensor("dbg_D", [128, 68], F32, kind="ExternalOutput").ap()
            dW = nc.dram_tensor("dbg_W", [128, 3, 128], F32, kind="ExternalOutput").ap()
            dsm = nc.dram_tensor("dbg_sm", [128, 6, 256], F32, kind="ExternalOutput").ap()
            A('sp', lambda e: e.dma_start(out=dft[:, :, :], in_=ft[:]), reads=[('ft', i) for i in range(NF)], dma=True)
            A('sp', lambda e: e.dma_start(out=dbt[:, :, :], in_=bt[:]), reads=[('bt', i) for i in range(NBT)], dma=True)
            A('sp', lambda e: e.dma_start(out=dy[:, :, :], in_=yT[:]), reads=[('yT', c, t) for c in range(NCH) for t in range(NT)], dma=True)
            A('sp', lambda e: e.dma_start(out=du[:, :, :], in_=uT[:]), reads=[('uT', c, t) for c in range(NCH) for t in range(NT)], dma=True)
            A('sp', lambda e: e.dma_start(out=dD[:, :], in_=Dall[:]), reads=['Dall'], dma=True)
            A('sp', lambda e: e.dma_start(out=dW[:, :, :], in_=W32[:]), reads=[('W32', i) for i in range(3)], dma=True)
            A('sp', lambda e: e.dma_start(out=dsm[:, :, :], in_=sm[:]), reads=[('sm', i) for i in range(6)], dma=True)
        P.emit()
        nc._prog_stats = dict(nops=len(P.ops), cnt=P.cnt)
    return nc


def _vec_layout(v):
    L = v.shape[0]
    return np.ascontiguousarray(v.reshape(L, NCH, 128).transpose(2, 0, 1).reshape(128, L * NCH)).astype(np.float32)


def make_in_maps(x, p, norm_w, w_in, fox_fb, hgrn_gn, hgrn_lb_logits, w_out, w_ple, w_ple_gate, final_norm_w, ncores, nseq):
    cst = host_consts()
    common = {
        "w_in": np.ascontiguousarray(w_in, dtype=np.float32),
        "w_out": np.ascontiguousarray(w_out, dtype=np.float32),
        "w_ple": np.ascontiguousarray(w_ple, dtype=np.float32),
        "w_ple_gate": np.ascontiguousarray(w_ple_gate, dtype=np.float32),
        "v_nw": _vec_layout(norm_w),
        "v_gn": _vec_layout(hgrn_gn),
        "v_lbl": _vec_layout(hgrn_lb_logits),
        "v_fnw": _vec_layout(final_norm_w[None, :]),
        "v_fb": np.ascontiguousarray(np.broadcast_to(np.asarray(fox_fb, np.float32).reshape(1, -1), (128, fox_fb.size))),
    }
    common.update({'c_' + k: v for k, v in cst.items()})
    maps = []
    for c in range(ncores):
        m = dict(common)
        m["x"] = np.ascontiguousarray(x[c * nseq:(c + 1) * nseq], dtype=np.float32)
        m["p"] = np.ascontiguousarray(p[:, c * nseq:(c + 1) * nseq], dtype=np.float32)
        maps.append(m)
    return maps


def kernel(x, p, norm_w, w_in, fox_fb, hgrn_gn, hgrn_lb_logits, w_out, w_ple, w_ple_gate, final_norm_w):
    x = np.asarray(x)
    B = x.shape[0]
    nseq = B // NCORES
    nc = build(nseq, list(range(DEPTH)), final_norm=True)
    maps = make_in_maps(x, np.asarray(p), np.asarray(norm_w), np.asarray(w_in), np.asarray(fox_fb), np.asarray(hgrn_gn),
                        np.asarray(hgrn_lb_logits), np.asarray(w_out), np.asarray(w_ple), np.asarray(w_ple_gate),
                        np.asarray(final_norm_w), NCORES, nseq)
    res = run_bass_kernel_spmd(nc, maps, core_ids=list(range(NCORES)))
    return np.concatenate([r["out"] for r in res.results], axis=0).astype(np.float32)
```

```python
import numpy as np
import ml_dtypes
from contextlib import ExitStack
import concourse.bass as bass
import concourse.mybir as mybir
from concourse.bass_utils import run_bass_kernel_spmd

F32 = mybir.dt.float32
BF16 = mybir.dt.bfloat16
AF = mybir.ActivationFunctionType
ALU = mybir.AluOpType

D = 1024
S = 2048
DEPTH = 4
NCH = 8
TW = 512
NT = S // TW
NB = S // 128
DIN = 8208
PLE = 256
EPS = 1e-6
NCORES = 8


class _Rec:
    def __init__(self):
        self.call = None

    def __getattr__(self, name):
        def f(*a, **k):
            self.call = (name, a, k)
            return None
        return f


class Prog:
    ENG = ('pe', 'act', 'dve', 'pool', 'sp')

    def __init__(self, nc, n_dma_sems=40):
        self.nc = nc
        self.ops = []
        self.last_w = {}
        self.readers = {}
        self.n_dma_sems = n_dma_sems

    def add(self, eng, fn, reads=(), writes=(), dma=False):
        i = len(self.ops)
        deps = set()
        for r in reads:
            w = self.last_w.get(r)
            if w is not None:
                deps.add((w, 0))
        for wkey in writes:
            w = self.last_w.get(wkey)
            if w is not None:
                deps.add((w, 1))
            for rd in self.readers.get(wkey, {}).values():
                deps.add((rd, 2))
        for r in reads:
            self.readers.setdefault(r, {})[('d', i) if dma else eng] = i
        for wkey in writes:
            self.last_w[wkey] = i
            self.readers[wkey] = {}
        rec = _Rec()
        fn(rec)
        self.ops.append(dict(eng=eng, call=rec.call, deps=deps, dma=dma, mark=False))
        return i

    def emit(self):
        nc = self.nc
        ops = self.ops
        for i, op in enumerate(ops):
            need = set()
            for (p, kind) in op['deps']:
                po = ops[p]
                if (not po['dma']) and po['eng'] == op['eng']:
                    if po['eng'] == 'pe' and not op['dma']:
                        continue
                    if kind == 2 and not op['dma']:
                        continue
                need.add(p)
            op['need'] = sorted(need)
            for p in op['need']:
                ops[p]['mark'] = True
            op['deps'] = None
        cnt = {e: 0 for e in self.ENG}
        dcnt = [0] * self.n_dma_sems
        nd = 0
        for op in ops:
            if op['dma']:
                k = nd % self.n_dma_sems
                nd += 1
                op['sem'] = ('dma', k)
                op['prev'] = dcnt[k]
                dcnt[k] += 16
                op['val'] = dcnt[k]
            elif op['mark']:
                cnt[op['eng']] += 1
                op['sem'] = ('eng', op['eng'])
                op['val'] = cnt[op['eng']]
        self.cnt = cnt
        with ExitStack() as st:
            sems = {}
            for e in self.ENG:
                sems[('eng', e)] = st.enter_context(nc.semaphore('s_' + e))
            for k in range(self.n_dma_sems):
                sems[('dma', k)] = st.enter_context(nc.semaphore('d_%d' % k))
            block = st.enter_context(nc.Block())
            per = {e: [op for op in ops if op['eng'] == e] for e in self.ENG}

            def run(eng_name, eng):
                waited = {}
                for op in per[eng_name]:
                    ws = [(ops[p]['sem'], ops[p]['val']) for p in op['need']]
                    if op['dma'] and op['prev'] > 0:
                        ws.append((op['sem'], op['prev']))
                    for (s, v) in ws:
                        if waited.get(s, 0) >= v:
                            continue
                        waited[s] = v
                        eng.wait_ge(sems[s], v)
                    nm, a_, k_ = op['call']
                    ins = getattr(eng, nm)(*a_, **k_)
                    if op['dma']:
                        ins.then_inc(sems[op['sem']], 16)
                    elif op['mark']:
                        ins.then_inc(sems[op['sem']], 1)
                if eng_name == 'sp':
                    for k in range(self.n_dma_sems):
                        if dcnt[k] > 0:
                            eng.wait_ge(sems[('dma', k)], dcnt[k])

            @block.tensor
            def _(e):
                run('pe', e)

            @block.scalar
            def _(e):
                run('act', e)

            @block.vector
            def _(e):
                run('dve', e)

            @block.gpsimd
            def _(e):
                run('pool', e)

            @block.sync
            def _(e):
                run('sp', e)


def host_consts():
    c = {}
    i = np.arange(128)
    c['ident32'] = np.eye(128, dtype=np.float32)
    tri = (i[:, None] <= i[None, :]).astype(np.float32)
    c['tri32'] = tri
    c['ones32'] = np.ones((128, 128), np.float32)
    m63 = np.zeros((128, 128), np.float32)
    m63[:64, :] = 1.0
    c['m63'] = m63
    hm = tri * ((i[:, None] // 32) == (i[None, :] // 32))
    c['hmask'] = hm.astype(np.float32)
    rm = np.ones((128, TW), np.float32)
    rm[:, ::32] = 0.0
    c['rmask'] = rm
    return c


CONST_NAMES = ['ident32', 'tri32', 'ones32', 'm63', 'hmask', 'rmask']


def build(nseq, layers, final_norm=True, wq='pool', phases=('norm', 'prep', 'hgrn', 'wo1', 'fox', 'wo2', 'ple'), nheads=8, dbg=False):
    nc = bass.Bass("TRN2", target_bir_lowering=False)
    NL = DEPTH
    x_d = nc.dram_tensor("x", [nseq, S, D], F32, kind="ExternalInput").ap()
    p_d = nc.dram_tensor("p", [NL, nseq, S, PLE], F32, kind="ExternalInput").ap()
    win_d = nc.dram_tensor("w_in", [NL, D, DIN], F32, kind="ExternalInput").ap()
    wout_d = nc.dram_tensor("w_out", [NL, 2 * D, D], F32, kind="ExternalInput").ap()
    wple_d = nc.dram_tensor("w_ple", [NL, PLE, D], F32, kind="ExternalInput").ap()
    wpg_d = nc.dram_tensor("w_ple_gate", [NL, D, D], F32, kind="ExternalInput").ap()
    nw_d = nc.dram_tensor("v_nw", [128, NL * NCH], F32, kind="ExternalInput").ap()
    gn_d = nc.dram_tensor("v_gn", [128, NL * NCH], F32, kind="ExternalInput").ap()
    lbl_d = nc.dram_tensor("v_lbl", [128, NL * NCH], F32, kind="ExternalInput").ap()
    fnw_d = nc.dram_tensor("v_fnw", [128, NCH], F32, kind="ExternalInput").ap()
    fb_d = nc.dram_tensor("v_fb", [128, NL * 16], F32, kind="ExternalInput").ap()
    cd = {n: nc.dram_tensor("c_" + n, [128, TW if n == 'rmask' else 128], F32, kind="ExternalInput").ap() for n in CONST_NAMES}
    out_d = nc.dram_tensor("out", [nseq, S, D], F32, kind="ExternalOutput").ap()

    with ExitStack() as st:
        def sb(name, shape, dt):
            return st.enter_context(nc.sbuf_tensor(name, shape, dt))

        def pst(name, shape, dt):
            return st.enter_context(nc.psum_tensor(name, shape, dt))

        h = sb("h", [128, NCH, S], F32)
        uT = sb("uT", [128, NCH, S], BF16)
        yT = sb("yT", [128, NCH, S], BF16)
        NWB = 8
        wb = sb("wb", [128, NWB, NCH, 128], BF16)
        NF = 10
        ft = sb("ft", [128, NF, TW], F32)
        NBT = 10
        bt = sb("bt", [128, NBT, TW], BF16)
        kAB = sb("kAB", [128, 2, S], BF16)
        VB = sb("VB", [128, NB, 128], BF16)
        W32 = sb("W32", [128, 3, 128], F32)
        W16 = sb("W16", [128, 3, 128], BF16)
        Dall = sb("Dall", [128, 68], F32)
        sm = sb("sm", [128, 6, 256], F32)
        ident32 = sb("ident32", [128, 128], F32)
        identb = sb("identb", [128, 128], BF16)
        tri32 = sb("tri32", [128, 128], F32)
        trib = sb("trib", [128, 128], BF16)
        ones32 = sb("ones32", [128, 128], F32)
        m63 = sb("m63", [128, 128], F32)
        hmask = sb("hmask", [128, 128], F32)
        rmask = sb("rmask", [128, TW], F32)
        onesD = sb("onesD", [128, 128], BF16)
        onesV = sb("onesV", [128, 128], BF16)
        onesb = sb("onesb", [128, 128], BF16)
        nw = sb("nw", [128, NL * NCH], F32)
        gn = sb("gn", [128, NL * NCH], F32)
        lb = sb("lb", [128, NL * NCH], F32)
        oml = sb("oml", [128, NL * NCH], F32)
        lbe = sb("lbe", [128, NL * NCH], F32)
        lbs = sb("lbs", [128, NCH], F32)
        fnw = sb("fnw", [128, NCH], F32)
        fb = sb("fb", [128, NL * 16], F32)
        epsb = sb("epsb", [128, 1], F32)
        wz = sb("wz", [128, NCH, 16], BF16)
        wpl = sb("wpl", [128, 2, 2, 128], BF16)

        PS = [pst("ps%d" % k, [128, TW], F32) for k in range(7)]
        PSB = pst("psb", [128, 2 * TW], BF16)

        P = Prog(nc)
        A = P.add

        cmap = dict(ident32=ident32, tri32=tri32, ones32=ones32, m63=m63, hmask=hmask, rmask=rmask)
        for n in CONST_NAMES:
            A('sp', (lambda t, s_: (lambda e: e.dma_start(out=t[:], in_=s_[:, :])))(cmap[n], cd[n]), writes=[n], dma=True)
        for (t, s_, n) in ((nw, nw_d, 'nw'), (gn, gn_d, 'gn'), (lbe, lbl_d, 'lbe'), (fnw, fnw_d, 'fnw'), (fb, fb_d, 'fb')):
            A('sp', (lambda t, s_: (lambda e: e.dma_start(out=t[:], in_=s_[:, :])))(t, s_), writes=[n], dma=True)
        A('pool', lambda e: e.tensor_copy(out=identb[:], in_=ident32[:]), reads=['ident32'], writes=['identb'])
        A('pool', lambda e: e.tensor_copy(out=trib[:], in_=tri32[:]), reads=['tri32'], writes=['trib'])
        A('pool', lambda e: e.memset(onesD[:], 1.0 / D), writes=['onesD'])
        A('pool', lambda e: e.memset(onesV[:], 1.0 / 128), writes=['onesV'])
        A('pool', lambda e: e.memset(onesb[:], 1.0), writes=['onesb'])
        A('pool', lambda e: e.memset(epsb[:], EPS), writes=['epsb'])
        A('pool', lambda e: e.memset(kAB[:, 1, :], 0.0), writes=['kB'])
        A('pool', lambda e: e.memset(kAB[64:65, 0, :], 1.0), writes=['kA'])
        A('pool', lambda e: e.memset(kAB[0:1, 1, :], 1.0), reads=['kB'], writes=['kB'])
        A('pool', lambda e: e.memset(VB[:], 0.0), writes=['VB'])
        A('pool', lambda e: e.memset(VB[:, :, 0:1], 1.0), reads=['VB'], writes=['VB'])
        A('act', lambda e: e.activation(out=lbe[:], in_=lbe[:], func=AF.Exp), reads=['lbe'], writes=['lbe'])
        A('dve', lambda e: e.tensor_tensor(out=lbs[:], in0=lbe[:, 0:NCH], in1=lbe[:, NCH:2 * NCH], op=ALU.add), reads=['lbe'], writes=['lbs'])
        A('dve', lambda e: e.tensor_tensor(out=lbs[:], in0=lbs[:], in1=lbe[:, 2 * NCH:3 * NCH], op=ALU.add), reads=['lbe', 'lbs'], writes=['lbs'])
        A('dve', lambda e: e.tensor_tensor(out=lbs[:], in0=lbs[:], in1=lbe[:, 3 * NCH:4 * NCH], op=ALU.add), reads=['lbe', 'lbs'], writes=['lbs'])
        A('dve', lambda e: e.reciprocal(out=lbs[:], in_=lbs[:]), reads=['lbs'], writes=['lbs'])
        A('dve', lambda e: e.memset(lb[:, 0:NCH], 0.0), writes=['lb'])
        for l in range(1, NL):
            A('dve', (lambda l: (lambda e: e.tensor_tensor(out=lbe[:, l * NCH:(l + 1) * NCH], in0=lbe[:, l * NCH:(l + 1) * NCH], in1=lbs[:], op=ALU.mult)))(l),
              reads=['lbe', 'lbs'], writes=['lbe'])
            A('dve', (lambda l: (lambda e: e.tensor_tensor(out=lb[:, l * NCH:(l + 1) * NCH], in0=lb[:, (l - 1) * NCH:l * NCH], in1=lbe[:, l * NCH:(l + 1) * NCH], op=ALU.add)))(l),
              reads=['lbe', 'lb'], writes=['lb'])
        A('dve', lambda e: e.tensor_scalar(out=oml[:], in0=lb[:], scalar1=-1.0, scalar2=1.0, op0=ALU.mult, op1=ALU.add), reads=['lb'], writes=['oml'])

        wslot = [0]

        def wload(src_ap, nk=NCH, ncols=128):
            k = wslot[0] % NWB
            wslot[0] += 1
            dst = wb[:, k, 0:nk, 0:ncols]
            A(wq, lambda e: e.dma_start(out=dst, in_=src_ap), writes=[('wb', k)], dma=True)
            return k

        def win_chunk(l, col0, ncols=128):
            return wload(win_d[l].rearrange("(c p) e -> p c e", p=128)[:, :, col0:col0 + ncols], NCH, ncols)

        def proj_fm(k, t0, tw, ps_ap, psres, ncols=128):
            for c in range(NCH):
                A('pe', (lambda c: (lambda e: e.matmul(ps_ap, lhsT=wb[:, k, c, 0:ncols], rhs=uT[:, c, t0:t0 + tw], start=(c == 0), stop=(c == NCH - 1))))(c),
                  reads=[('wb', k), ('uT', c, t0 // TW)], writes=psres)

        def proj_tm(k, tb, ps_ap, psres, ncols=128):
            for c in range(NCH):
                A('pe', (lambda c: (lambda e: e.matmul(ps_ap, lhsT=uT[:, c, tb * 128:(tb + 1) * 128], rhs=wb[:, k, c, 0:ncols], start=(c == 0), stop=(c == NCH - 1))))(c),
                  reads=[('wb', k), ('uT', c, tb // 4)], writes=psres)

        def rmsnorm_to_u(l):
            for t in range(NT):
                ts = slice(t * TW, (t + 1) * TW)
                sq_list = []
                for c in range(NCH):
                    slot = c % 4
                    A('act', (lambda c, slot: (lambda e: e.activation(out=bt[:, slot, :], in_=h[:, c, ts], func=AF.Square)))(c, slot),
                      reads=[('h', c, t)], writes=[('bt', slot)])
                    A('pe', (lambda c, slot: (lambda e: e.matmul(PS[0][:], lhsT=onesD[:], rhs=bt[:, slot, :], start=(c == 0), stop=(c == NCH - 1))))(c, slot),
                      reads=['onesD', ('bt', slot)], writes=[('ps', 0)])
                A('act', lambda e: e.activation(out=ft[:, 0, :], in_=PS[0][:], func=AF.Sqrt, bias=epsb[:], scale=1.0),
                  reads=[('ps', 0), 'epsb'], writes=[('ft', 0)])
                A('dve', lambda e: e.reciprocal(out=ft[:, 0, :], in_=ft[:, 0, :]), reads=[('ft', 0)], writes=[('ft', 0)])
                for c in range(NCH):
                    A('dve', (lambda c: (lambda e: e.scalar_tensor_tensor(out=uT[:, c, ts], in0=h[:, c, ts], scalar=nw[:, l * NCH + c:l * NCH + c + 1],
                                                                         in1=ft[:, 0, :], op0=ALU.mult, op1=ALU.mult)))(c),
                      reads=[('h', c, t), ('ft', 0), 'nw'], writes=[('uT', c, t)])

        def load_x(s):
            for tb in range(NB):
                stg = ft[:, 8:10, :]
                A('sp', (lambda tb: (lambda e: e.dma_start(out=stg, in_=x_d[s, tb * 128:(tb + 1) * 128, :].rearrange("p (a b) -> p a b", a=2))))(tb),
                  writes=[('ft', 8), ('ft', 9)], dma=True)
                for half in range(2):
                    pk = 1 + half
                    for cc in range(4):
                        c = half * 4 + cc
                        A('pe', (lambda c, cc, pk, half: (lambda e: e.transpose(out=PS[pk][:, cc * 128:(cc + 1) * 128], in_=ft[:, 8 + half, cc * 128:(cc + 1) * 128], identity=ident32[:])))(c, cc, pk, half),
                          reads=[('ft', 8 + half), 'ident32'], writes=[('ps', pk)])
                    eng = 'act' if half == 0 else 'dve'
                    if eng == 'act':
                        A('act', (lambda pk, half, tb: (lambda e: e.activation(out=h[:, half * 4:half * 4 + 4, tb * 128:(tb + 1) * 128],
                                                                              in_=PS[pk][:].rearrange("p (a b) -> p a b", a=4), func=AF.Copy)))(pk, half, tb),
                          reads=[('ps', pk)], writes=[('h', half * 4 + cc, tb // 4) for cc in range(4)])
                    else:
                        A('dve', (lambda pk, half, tb: (lambda e: e.tensor_copy(out=h[:, half * 4:half * 4 + 4, tb * 128:(tb + 1) * 128],
                                                                               in_=PS[pk][:].rearrange("p (a b) -> p a b", a=4))))(pk, half, tb),
                          reads=[('ps', pk)], writes=[('h', half * 4 + cc, tb // 4) for cc in range(4)])

        def store_out(s, normed):
            for t in range(NT):
                ts = slice(t * TW, (t + 1) * TW)
                if normed:
                    for c in range(NCH):
                        slot = c % 4
                        A('act', (lambda c, slot: (lambda e: e.activation(out=bt[:, slot, :], in_=h[:, c, ts], func=AF.Square)))(c, slot),
                          reads=[('h', c, t)], writes=[('bt', slot)])
                        A('pe', (lambda c, slot: (lambda e: e.matmul(PS[0][:], lhsT=onesD[:], rhs=bt[:, slot, :], start=(c == 0), stop=(c == NCH - 1))))(c, slot),
                          reads=['onesD', ('bt', slot)], writes=[('ps', 0)])
                    A('act', lambda e: e.activation(out=ft[:, 0, :], in_=PS[0][:], func=AF.Sqrt, bias=epsb[:], scale=1.0),
                      reads=[('ps', 0), 'epsb'], writes=[('ft', 0)])
                    A('dve', lambda e: e.reciprocal(out=ft[:, 0, :], in_=ft[:, 0, :]), reads=[('ft', 0)], writes=[('ft', 0)])
                    for c in range(NCH):
                        A('dve', (lambda c: (lambda e: e.scalar_tensor_tensor(out=h[:, c, ts], in0=h[:, c, ts], scalar=fnw[:, c:c + 1],
                                                                             in1=ft[:, 0, :], op0=ALU.mult, op1=ALU.mult)))(c),
                          reads=[('h', c, t), ('ft', 0), 'fnw'], writes=[('h', c, t)])
                for q in range(4):
                    tb = t * 4 + q
                    for half in range(2):
                        pk = 1 + half
                        for cc in range(4):
                            c = half * 4 + cc
                            A('pe', (lambda c, cc, pk, tb: (lambda e: e.transpose(out=PS[pk][:, cc * 128:(cc + 1) * 128], in_=h[:, c, tb * 128:(tb + 1) * 128], identity=ident32[:])))(c, cc, pk, tb),
                              reads=[('h', c, t), 'ident32'], writes=[('ps', pk)])
                        if half == 0:
                            A('act', (lambda pk, half: (lambda e: e.activation(out=ft[:, 8 + half, :], in_=PS[pk][:], func=AF.Copy)))(pk, half),
                              reads=[('ps', pk)], writes=[('ft', 8 + half)])
                        else:
                            A('dve', (lambda pk, half: (lambda e: e.tensor_copy(out=ft[:, 8 + half, :], in_=PS[pk][:])))(pk, half),
                              reads=[('ps', pk)], writes=[('ft', 8 + half)])
                    A('sp', (lambda tb: (lambda e: e.dma_start(out=out_d[s, tb * 128:(tb + 1) * 128, :].rearrange("p (a b) -> p a b", a=2), in_=ft[:, 8:10, :])))(tb),
                      reads=[('ft', 8), ('ft', 9)], dma=True)

        def hgrn_head(l, hh):
            kq = win_chunk(l, 0 * D + hh * 128)
            kf = win_chunk(l, 1 * D + hh * 128)
            ki = win_chunk(l, 2 * D + hh * 128)
            kg = win_chunk(l, 3 * D + hh * 128)
            lc = l * NCH + hh
            lb_ap = lb[:, lc:lc + 1]
            oml_ap = oml[:, lc:lc + 1]
            gn_ap = gn[:, lc:lc + 1]
            A('pool', lambda e: e.memset(W32[:, 2, :], 0.0), writes=[('W32', 2)])
            A('pool', lambda e: e.memset(W16[:, 2, :], 0.0), writes=[('W16', 2)])
            A('pool', lambda e: e.memset(Dall[:, 0:1], 1.0), writes=['Dall'])
            for sg in range(NT):
                t0 = sg * TW
                proj_fm(kf, t0, TW, PS[0][:], [('ps', 0)])
                A('act', lambda e: e.activation(out=ft[:, 1, :], in_=PS[0][:], func=AF.Sigmoid), reads=[('ps', 0)], writes=[('ft', 1)])
                A('act', lambda e: e.activation(out=ft[:, 2, :], in_=PS[0][:], func=AF.Sigmoid, scale=-1.0), reads=[('ps', 0)], writes=[('ft', 2)])
                A('dve', lambda e: e.tensor_scalar(out=ft[:, 1, :], in0=ft[:, 1, :], scalar1=oml_ap, scalar2=lb_ap, op0=ALU.mult, op1=ALU.add),
                  reads=[('ft', 1), 'oml', 'lb'], writes=[('ft', 1)])
                A('act', lambda e: e.activation(out=ft[:, 3, :], in_=ft[:, 1, :], func=AF.Ln), reads=[('ft', 1)], writes=[('ft', 3)])
                A('dve', lambda e: e.tensor_tensor_scan(out=ft[:, 4, :], data0=rmask[:], data1=ft[:, 3, :], initial=0.0, op0=ALU.mult, op1=ALU.add),
                  reads=[('ft', 3), 'rmask'], writes=[('ft', 4)])
                A('act', lambda e: e.activation(out=ft[:, 5, :], in_=ft[:, 4, :], func=AF.Exp), reads=[('ft', 4)], writes=[('ft', 5)])
                A('act', lambda e: e.activation(out=ft[:, 6, :], in_=ft[:, 4, :], func=AF.Exp, scale=-1.0), reads=[('ft', 4)], writes=[('ft', 6)])
                A('dve', lambda e: e.scalar_tensor_tensor(out=bt[:, 4, :], in0=ft[:, 2, :], scalar=oml_ap, in1=ft[:, 6, :], op0=ALU.mult, op1=ALU.mult),
                  reads=[('ft', 2), ('ft', 6), 'oml'], writes=[('bt', 4)])
                A('pool', (lambda sg: (lambda e: e.tensor_copy(out=Dall[:, 1 + 16 * sg:1 + 16 * sg + 16], in_=ft[:, 5, 31::32])))(sg),
                  reads=[('ft', 5), 'Dall'], writes=['Dall'])
                proj_fm(kq, t0, TW, PS[1][:], [('ps', 1)])
                A('dve', lambda e: e.scalar_tensor_tensor(out=bt[:, 5, :], in0=PS[1][:], scalar=float(128 ** -0.5), in1=ft[:, 5, :], op0=ALU.mult, op1=ALU.mult),
                  reads=[('ps', 1), ('ft', 5)], writes=[('bt', 5)])
                A('pool', (lambda sg: (lambda e: e.tensor_tensor(out=bt[:, 6, :].rearrange("p (a b) -> p a b", b=32), in0=bt[:, 5, :].rearrange("p (a b) -> p a b", b=32),
                                                                 in1=bcast_last(Dall[:, 16 * sg:16 * sg + 16], 32), op=ALU.mult)))(sg),
                  reads=[('bt', 5), 'Dall'], writes=[('bt', 6)])
                proj_fm(kg, t0, TW, PS[2][:], [('ps', 2)])
                A('act', lambda e: e.activation(out=ft[:, 7, :], in_=PS[2][:], func=AF.Silu), reads=[('ps', 2)], writes=[('ft', 7)])
                for q in range(4):
                    proj_tm(ki, sg * 4 + q, PS[3][:, q * 128:(q + 1) * 128], [('ps', 3)])
                A('act', lambda e: e.activation(out=bt[:, 9, :], in_=PS[3][:], func=AF.Copy), reads=[('ps', 3)], writes=[('bt', 9)])
                for q in range(4):
                    A('pe', (lambda q: (lambda e: e.transpose(out=PSB[:, q * 128:(q + 1) * 128], in_=bt[:, 4, q * 128:(q + 1) * 128], identity=identb[:])))(q),
                      reads=[('bt', 4), 'identb'], writes=['psb'])
                A('dve', lambda e: e.tensor_copy(out=bt[:, 8, :], in_=PSB[:, 0:TW]), reads=['psb'], writes=[('bt', 8)])
                def emit_U(idx):
                    q, cc = divmod(idx, 4)
                    cg = sg * 16 + idx
                    us = slice((cg % 4) * 128, (cg % 4) * 128 + 128)
                    rows = slice(cc * 32, cc * 32 + 32)
                    A('pe', lambda e: e.matmul(PS[6][:, us], lhsT=bt[rows, 8, q * 128:(q + 1) * 128], rhs=bt[rows, 9, q * 128:(q + 1) * 128],
                                               start=True, stop=True, tile_position=(cc * 32, 0)),
                      reads=[('bt', 8), ('bt', 9)], writes=[('psu', cg % 4)])
                LA = 0
                for q in range(4):
                    A('pe', (lambda q: (lambda e: e.matmul(PS[4][:, q * 128:(q + 1) * 128], lhsT=bt[:, 4, q * 128:(q + 1) * 128], rhs=bt[:, 5, q * 128:(q + 1) * 128], start=True, stop=True)))(q),
                      reads=[('bt', 4), ('bt', 5)], writes=[('ps', 4)])
                    if q < LA:
                        emit_U(q)
                A('dve', lambda e: e.tensor_tensor(out=bt[:, 3, :].rearrange("p (a b) -> p a b", a=4), in0=PS[4][:].rearrange("p (a b) -> p a b", a=4),
                                                   in1=bcast_mid(hmask[:], 4), op=ALU.mult),
                  reads=[('ps', 4), 'hmask'], writes=[('bt', 3)])
                for idx in range(16):
                    q, cc = divmod(idx, 4)
                    cg = sg * 16 + idx
                    prev = (cg + 2) % 3
                    cur = cg % 3
                    cs = slice(q * 128 + cc * 32, q * 128 + cc * 32 + 32)
                    us = slice((cg % 4) * 128, (cg % 4) * 128 + 128)
                    if cc == 0:
                        A('pe', lambda e: e.matmul(PS[5][:, q * 128:(q + 1) * 128], lhsT=bt[:, 9, q * 128:(q + 1) * 128], rhs=bt[:, 3, q * 128:(q + 1) * 128], start=True, stop=False),
                          reads=[('bt', 9), ('bt', 3)], writes=[('ps', 5)])
                    A('pe', lambda e: e.matmul(PS[5][:, cs], lhsT=W16[:, prev, :], rhs=bt[:, 6, cs], start=False, stop=(cc == 3)),
                      reads=[('W16', prev), ('bt', 6)], writes=[('ps', 5)])
                    if idx + LA < 16:
                        emit_U(idx + LA)
                    A('dve', lambda e: e.scalar_tensor_tensor(out=W32[:, cur, :], in0=W32[:, prev, :], scalar=Dall[:, cg:cg + 1], in1=PS[6][:, us], op0=ALU.mult, op1=ALU.add),
                      reads=[('W32', prev), 'Dall', ('psu', cg % 4)], writes=[('W32', cur)])
                    A('pool', lambda e: e.tensor_copy(out=W16[:, cur, :], in_=W32[:, cur, :]), reads=[('W32', cur)], writes=[('W16', cur)])
                A('act', lambda e: e.activation(out=bt[:, 7, :], in_=PS[5][:], func=AF.Square), reads=[('ps', 5)], writes=[('bt', 7)])
                A('pe', lambda e: e.matmul(PS[0][:], lhsT=onesV[:], rhs=bt[:, 7, :], start=True, stop=True), reads=['onesV', ('bt', 7)], writes=[('ps', 0)])
                A('act', lambda e: e.activation(out=ft[:, 0, :], in_=PS[0][:], func=AF.Sqrt, bias=epsb[:], scale=1.0), reads=[('ps', 0), 'epsb'], writes=[('ft', 0)])
                A('dve', lambda e: e.reciprocal(out=ft[:, 0, :], in_=ft[:, 0, :]), reads=[('ft', 0)], writes=[('ft', 0)])
                A('dve', lambda e: e.scalar_tensor_tensor(out=ft[:, 0, :], in0=PS[5][:], scalar=gn_ap, in1=ft[:, 0, :], op0=ALU.mult, op1=ALU.mult),
                  reads=[('ps', 5), ('ft', 0), 'gn'], writes=[('ft', 0)])
                A('pool', (lambda t0, sg: (lambda e: e.tensor_tensor(out=yT[:, hh, t0:t0 + TW], in0=ft[:, 0, :], in1=ft[:, 7, :], op=ALU.mult)))(t0, sg),
                  reads=[('ft', 0), ('ft', 7)], writes=[('yT', hh, sg)])

        def bcast_last(ap2, n):
            return ap2.unsqueeze(2).to_broadcast([ap2.shape[0], ap2.shape[1], n])

        def bcast_mid(ap2, n):
            return ap2.unsqueeze(1).to_broadcast([ap2.shape[0], n, ap2.shape[1]])

        PS6 = [('psu', i) for i in range(4)]

        def psr(k):
            return PS6 if k == 6 else [('ps', k)]

        def wout_pass(l, half, swap):
            for dm in range(NCH):
                src = wout_d[l, half * D:(half + 1) * D, dm * 128:(dm + 1) * 128].rearrange("(c p) e -> p c e", p=128)
                if not swap:
                    k = wload(src)
                else:
                    k = wload(src)
                for t in range(NT):
                    pk = 1 + (t % 2)
                    for c in range(NCH):
                        A('pe', (lambda c, t, pk: (lambda e: e.matmul(PS[pk][:], lhsT=wb[:, k, c, :], rhs=yT[:, c, t * TW:(t + 1) * TW], start=(c == 0), stop=(c == NCH - 1))))(c, t, pk),
                          reads=[('wb', k), ('yT', c, t)], writes=[('ps', pk)])
                    A('dve', (lambda t, pk, dm: (lambda e: e.tensor_tensor(out=h[:, dm, t * TW:(t + 1) * TW], in0=h[:, dm, t * TW:(t + 1) * TW], in1=PS[pk][:], op=ALU.add)))(t, pk, dm),
                      reads=[('ps', pk), ('h', dm, t)], writes=[('h', dm, t)])

        def fox_prep(l):
            A(wq, lambda e: e.dma_start(out=wz[:], in_=win_d[l].rearrange("(c p) e -> p c e", p=128)[:, :, 8 * D:8 * D + 16]), writes=['wz'], dma=True)
            for tb in range(NB):
                for c in range(NCH):
                    A('pe', (lambda c, tb: (lambda e: e.matmul(PS[0][:, tb * 16:(tb + 1) * 16], lhsT=uT[:, c, tb * 128:(tb + 1) * 128], rhs=wz[:, c, :], start=(c == 0), stop=(c == NCH - 1))))(c, tb),
                      reads=['wz', ('uT', c, tb // 4)], writes=[('ps', 0)])
            A('dve', lambda e: e.tensor_tensor(out=sm[:, 0, :].rearrange("p (a b) -> p a b", b=16), in0=PS[0][:, 0:256].rearrange("p (a b) -> p a b", b=16),
                                               in1=bcast_mid(fb[:, l * 16:(l + 1) * 16], 16), op=ALU.add),
              reads=[('ps', 0), 'fb'], writes=[('sm', 0)])
            A('act', lambda e: e.activation(out=sm[:, 0, :], in_=sm[:, 0, :], func=AF.Sigmoid), reads=[('sm', 0)], writes=[('sm', 0)])
            A('act', lambda e: e.activation(out=sm[:, 0, :], in_=sm[:, 0, :], func=AF.Ln), reads=[('sm', 0)], writes=[('sm', 0)])
            A('pe', lambda e: e.matmul(PS[1][:, 0:256], lhsT=tri32[:], rhs=sm[:, 0, :], start=True, stop=True), reads=['tri32', ('sm', 0)], writes=[('ps', 1)])
            A('pe', lambda e: e.matmul(PS[2][:, 0:256], lhsT=ones32[:], rhs=sm[:, 0, :], start=True, stop=True), reads=['ones32', ('sm', 0)], writes=[('ps', 2)])
            A('pe', lambda e: e.matmul(PS[3][:, 0:256], lhsT=m63[:], rhs=sm[:, 0, :], start=True, stop=True), reads=['m63', ('sm', 0)], writes=[('ps', 3)])
            A('act', lambda e: e.activation(out=sm[:, 4, :], in_=PS[2][:, 0:256], func=AF.Copy), reads=[('ps', 2)], writes=[('sm', 4)])
            A('dve', lambda e: e.memset(sm[:, 1, 0:16], 0.0), writes=[('sm', 1)])
            for j in range(1, NB):
                A('dve', (lambda j: (lambda e: e.tensor_tensor(out=sm[:, 1, j * 16:(j + 1) * 16], in0=sm[:, 1, (j - 1) * 16:j * 16], in1=sm[:, 4, (j - 1) * 16:j * 16], op=ALU.add)))(j),
                  reads=[('sm', 1), ('sm', 4)], writes=[('sm', 1)])
            A('dve', lambda e: e.scalar_tensor_tensor(out=sm[:, 2, :], in0=PS[1][:, 0:256], scalar=-1.0, in1=sm[:, 1, :], op0=ALU.mult, op1=ALU.subtract),
              reads=[('ps', 1), ('sm', 1)], writes=[('sm', 2)])
            A('dve', lambda e: e.tensor_tensor(out=sm[:, 3, :], in0=PS[3][:, 0:256], in1=sm[:, 1, :], op=ALU.add), reads=[('ps', 3), ('sm', 1)], writes=[('sm', 3)])
            A('dve', lambda e: e.tensor_scalar(out=sm[:, 3, :], in0=sm[:, 3, :], scalar1=8.0, scalar2=None, op0=ALU.mult), reads=[('sm', 3)], writes=[('sm', 3)])

        def fox_pair(l, pp):
            base = 4 * D
            kfq = win_chunk(l, base + 0 * D + pp * 128)
            kfk = win_chunk(l, base + 1 * D + pp * 128)
            kfv = win_chunk(l, base + 2 * D + pp * 128)
            kfg = win_chunk(l, base + 3 * D + pp * 128)
            hA, hB = 2 * pp, 2 * pp + 1
            for t in range(NT):
                ts = slice(t * TW, (t + 1) * TW)
                pk = t % 2
                proj_fm(kfk, t * TW, TW, PS[pk][:], [('ps', pk)])
                if t % 2 == 0:
                    A('act', (lambda pk, ts: (lambda e: e.activation(out=kAB[:, 0, ts], in_=PS[pk][:], func=AF.Copy)))(pk, ts), reads=[('ps', pk)], writes=['kA'])
                else:
                    A('dve', (lambda pk, ts: (lambda e: e.tensor_copy(out=kAB[:, 0, ts], in_=PS[pk][:])))(pk, ts), reads=[('ps', pk)], writes=['kA'])
            for g in range(4):
                pk = g % 2
                for q in range(4):
                    proj_tm(kfv, g * 4 + q, PS[pk][:, q * 128:(q + 1) * 128], [('ps', pk)])
                if g % 2 == 0:
                    A('act', (lambda pk, g: (lambda e: e.activation(out=VB[:, g * 4:(g + 1) * 4, :], in_=PS[pk][:].rearrange("p (a b) -> p a b", a=4), func=AF.Copy)))(pk, g),
                      reads=[('ps', pk)], writes=['VB'])
                else:
                    A('dve', (lambda pk, g: (lambda e: e.tensor_copy(out=VB[:, g * 4:(g + 1) * 4, :], in_=PS[pk][:].rearrange("p (a b) -> p a b", a=4))))(pk, g),
                      reads=[('ps', pk)], writes=['VB'])
            FST = 9
            for qi in range(NT if FST >= 2 else 0):
                ts = slice(qi * TW, (qi + 1) * TW)
                qa, qb = 0 + (qi % 2) * 2, 1 + (qi % 2) * 2
                pk = 2
                proj_fm(kfq, qi * TW, TW, PS[pk][:], [('ps', pk)])
                A('act', (lambda pk, qa: (lambda e: e.activation(out=bt[:, qa, :], in_=PS[pk][:], func=AF.Copy)))(pk, qa), reads=[('ps', pk)], writes=[('bt', qa)])
                for hd_, hx_ in ((0, hA), (1, hB)):
                    A('pool', (lambda hd_, hx_, qi: (lambda e: e.tensor_copy(out=bt[0:1, 8 + hd_, :].rearrange("p (a b) -> p a b", a=4),
                                                                             in_=bcast_last(sm[0:1, 3, :].rearrange("p (a b) -> p a b", b=16)[:, 4 * qi:4 * qi + 4, hx_], 128))))(hd_, hx_, qi),
                      reads=[('sm', 3)], writes=[('bt', 8 + hd_)])
                pg = 2
                proj_fm(kfg, qi * TW, TW, PS[pg][:], [('ps', pg)])
                A('act', lambda e: e.activation(out=ft[:, 7, :], in_=PS[pg][:], func=AF.Silu), reads=[('ps', pg)], writes=[('ft', 7)])
                for hd in range(2 if FST >= 3 else 0):
                    hidx = hA if hd == 0 else hB
                    qs = qa if hd == 0 else qb
                    rows = slice(0, 64) if hd == 0 else slice(64, 128)
                    po = 5 + hd
                    nkb = 4 * qi + 4
                    def emit_S(j):
                        c0 = max(j - 4 * qi, 0) * 128
                        psk = 3 + (j % 2)
                        A('pe', lambda e: e.matmul(PS[psk][:, c0:TW], lhsT=kAB[rows, 0, j * 128:(j + 1) * 128], rhs=bt[rows, qa, c0:TW], start=True, stop=False),
                          reads=['kA', ('bt', qa)], writes=[('ps', psk)])
                        A('pe', lambda e: e.matmul(PS[psk][:, c0:TW], lhsT=onesb[0:1, :], rhs=bt[0:1, 8 + hd, c0:TW], start=False, stop=True),
                          reads=['onesb', ('bt', 8 + hd)], writes=[('ps', psk)])
                    LS = 1
                    if LS:
                        emit_S(0)
                    for j in range(nkb):
                        r = j - 4 * qi
                        c0 = max(r, 0) * 128
                        psk = 3 + (j % 2)
                        ptk = 4 + (j % 4)
                        if LS and j + 1 < nkb:
                            emit_S(j + 1)
                        if not LS:
                            emit_S(j)
                        A('act', lambda e: e.activation(out=bt[:, ptk, c0:TW], in_=PS[psk][:, c0:TW], func=AF.Exp, bias=sm[:, 2, j * 16 + hidx:j * 16 + hidx + 1], scale=0.125),
                          reads=[('ps', psk), ('sm', 2)], writes=[('bt', ptk)])
                        if r >= 0:
                            A('pool', lambda e: e.tensor_tensor(out=bt[:, ptk, c0:c0 + 128], in0=bt[:, ptk, c0:c0 + 128], in1=trib[:], op=ALU.mult),
                              reads=[('bt', ptk), 'trib'], writes=[('bt', ptk)])
                        A('pe', lambda e: e.matmul(PS[hd][:, c0:TW], lhsT=onesb[:], rhs=bt[:, ptk, c0:TW], start=(j == 0), stop=(j == nkb - 1)),
                          reads=['onesb', ('bt', ptk)], writes=[('ps', hd)])
                        if hd == 0:
                            A('pe', lambda e: e.matmul(PS[po][0:64, c0:TW], lhsT=VB[:, j, 0:64], rhs=bt[:, ptk, c0:TW], start=(j == 0), stop=(j == nkb - 1)),
                              reads=['VB', ('bt', ptk)], writes=psr(po))
                        else:
                            A('pe', lambda e: e.matmul(PS[po][:, c0:TW], lhsT=VB[:, j, :], rhs=bt[:, ptk, c0:TW], start=(j == 0), stop=(j == nkb - 1)),
                              reads=['VB', ('bt', ptk)], writes=psr(po))
                    orow = slice(0, 64) if hd == 0 else slice(64, 128)
                    A('dve', (lambda orow, hd: (lambda e: e.reciprocal(out=ft[orow, 1, :], in_=PS[hd][orow, :])))(orow, hd), reads=[('ps', hd)], writes=[('ft', 1)])
                    A('dve', (lambda orow: (lambda e: e.tensor_tensor(out=ft[orow, 2, :], in0=ft[orow, 1, :], in1=ft[orow, 7, :], op=ALU.mult)))(orow),
                      reads=[('ft', 1), ('ft', 7)], writes=[('ft', 2)])
                    A('dve', (lambda orow, po, qi: (lambda e: e.tensor_tensor(out=yT[orow, pp, qi * TW:(qi + 1) * TW], in0=PS[po][orow, :], in1=ft[orow, 2, :], op=ALU.mult)))(orow, po, qi),
                      reads=psr(po) + [('ft', 2)], writes=[('yT', pp, qi)])

        def ple_phase(l, s):
            for c in range(NCH):
                for t in range(NT):
                    eng = 'pool' if (c + t) % 2 == 0 else 'act'
                    if eng == 'pool':
                        A('pool', (lambda c, t: (lambda e: e.tensor_copy(out=uT[:, c, t * TW:(t + 1) * TW], in_=h[:, c, t * TW:(t + 1) * TW])))(c, t),
                          reads=[('h', c, t)], writes=[('uT', c, t)])
                    else:
                        A('act', (lambda c, t: (lambda e: e.activation(out=uT[:, c, t * TW:(t + 1) * TW], in_=h[:, c, t * TW:(t + 1) * TW], func=AF.Copy)))(c, t),
                          reads=[('h', c, t)], writes=[('uT', c, t)])
            for t in range(NT):
                stg = ft[:, 8:10, :]
                A('sp', (lambda t: (lambda e: e.dma_start(out=stg.rearrange("p a (q k) -> p (a q) k", k=PLE), in_=p_d[l, s, t * TW:(t + 1) * TW, :].rearrange("(q p) k -> p q k", p=128))))(t),
                  writes=[('ft', 8), ('ft', 9)], dma=True)
                for kc in range(2):
                    pk = 1 + kc
                    for q in range(4):
                        a, qq = divmod(q, 2)
                        A('pe', (lambda kc, q, a, qq, pk: (lambda e: e.transpose(out=PS[pk][:, q * 128:(q + 1) * 128], in_=ft[:, 8 + a, qq * PLE + kc * 128:qq * PLE + kc * 128 + 128], identity=ident32[:])))(kc, q, a, qq, pk),
                          reads=[('ft', 8 + a), 'ident32'], writes=[('ps', pk)])
                    if kc == 0:
                        A('act', (lambda pk, t: (lambda e: e.activation(out=kAB[:, 0, t * TW:(t + 1) * TW], in_=PS[pk][:], func=AF.Copy)))(pk, t), reads=[('ps', pk)], writes=['kA'])
                    else:
                        A('dve', (lambda pk, t: (lambda e: e.tensor_copy(out=kAB[:, 1, t * TW:(t + 1) * TW], in_=PS[pk][:])))(pk, t), reads=[('ps', pk)], writes=['kB'])
            for dm in range(NCH):
                kg = wload(wpg_d[l, :, dm * 128:(dm + 1) * 128].rearrange("(c p) e -> p c e", p=128))
                kp = wload(wple_d[l, :, dm * 128:(dm + 1) * 128].rearrange("(c p) e -> p c e", p=128), nk=2)
                for t in range(NT):
                    ts = slice(t * TW, (t + 1) * TW)
                    pa, pb = 3 + (t % 2), 5 + (t % 2)
                    for c in range(NCH):
                        A('pe', (lambda c, pa, ts, t: (lambda e: e.matmul(PS[pa][:], lhsT=wb[:, kg, c, :], rhs=uT[:, c, ts], start=(c == 0), stop=(c == NCH - 1))))(c, pa, ts, t),
                          reads=[('wb', kg), ('uT', c, t)], writes=[('ps', pa)])
                    for kc in range(2):
                        A('pe', (lambda kc, pb, ts: (lambda e: e.matmul(PS[pb][:], lhsT=wb[:, kp, kc, :], rhs=kAB[:, kc, ts], start=(kc == 0), stop=(kc == 1))))(kc, pb, ts),
                          reads=[('wb', kp), 'kA', 'kB'], writes=psr(pb))
                    f1 = 1 + (t % 2)
                    A('act', (lambda pa, f1: (lambda e: e.activation(out=ft[:, f1, :], in_=PS[pa][:], func=AF.Sigmoid)))(pa, f1), reads=[('ps', pa)], writes=[('ft', f1)])
                    A('dve', (lambda pb, f1: (lambda e: e.tensor_tensor(out=ft[:, f1, :], in0=PS[pb][:], in1=ft[:, f1, :], op=ALU.mult)))(pb, f1), reads=psr(pb) + [('ft', f1)], writes=[('ft', f1)])
                    A('pool', (lambda dm, ts, f1, t: (lambda e: e.tensor_tensor(out=h[:, dm, ts], in0=h[:, dm, ts], in1=ft[:, f1, :], op=ALU.add)))(dm, ts, f1, t),
                      reads=[('h', dm, t), ('ft', f1)], writes=[('h', dm, t)])

        for s in range(nseq):
            load_x(s)
            for l in layers:
                if 'norm' in phases:
                    rmsnorm_to_u(l)
                if 'prep' in phases:
                    fox_prep(l)
                if 'hgrn' in phases:
                    for hh in range(nheads):
                        hgrn_head(l, hh)
                if 'wo1' in phases:
                    wout_pass(l, 0, False)
                if 'fox' in phases:
                    for pp in range(nheads):
                        fox_pair(l, pp)
                if 'wo2' in phases:
                    wout_pass(l, 1, False)
                if 'ple' in phases:
                    ple_phase(l, s)
            store_out(s, final_norm)
        if dbg:
            dft = nc.dram_tensor("dbg_ft", [128, NF, TW], F32, kind="ExternalOutput").ap()
            dbt = nc.dram_tensor("dbg_bt", [128, NBT, TW], BF16, kind="ExternalOutput").ap()
            dy = nc.dram_tensor("dbg_y", [128, NCH, S], BF16, kind="ExternalOutput").ap()
            du = nc.dram_tensor("dbg_u", [128, NCH, S], BF16, kind="ExternalOutput").ap()
            dD = nc.dram_tensor("dbg_D", [128, 68], F32, kind="ExternalOutput").ap()
            dW = nc.dram_tensor("dbg_W", [128, 3, 128], F32, kind="ExternalOutput").ap()
            dsm = nc.dram_tensor("dbg_sm", [128, 6, 256], F32, kind="ExternalOutput").ap()
            A('sp', lambda e: e.dma_start(out=dft[:, :, :], in_=ft[:]), reads=[('ft', i) for i in range(NF)], dma=True)
            A('sp', lambda e: e.dma_start(out=dbt[:, :, :], in_=bt[:]), reads=[('bt', i) for i in range(NBT)], dma=True)
            A('sp', lambda e: e.dma_start(out=dy[:, :, :], in_=yT[:]), reads=[('yT', c, t) for c in range(NCH) for t in range(NT)], dma=True)
            A('sp', lambda e: e.dma_start(out=du[:, :, :], in_=uT[:]), reads=[('uT', c, t) for c in range(NCH) for t in range(NT)], dma=True)
            A('sp', lambda e: e.dma_start(out=dD[:, :], in_=Dall[:]), reads=['Dall'], dma=True)
            A('sp', lambda e: e.dma_start(out=dW[:, :, :], in_=W32[:]), reads=[('W32', i) for i in range(3)], dma=True)
            A('sp', lambda e: e.dma_start(out=dsm[:, :, :], in_=sm[:]), reads=[('sm', i) for i in range(6)], dma=True)
        P.emit()
        nc._prog_stats = dict(nops=len(P.ops), cnt=P.cnt)
    return nc


def _vec_layout(v):
    L = v.shape[0]
    return np.ascontiguousarray(v.reshape(L, NCH, 128).transpose(2, 0, 1).reshape(128, L * NCH)).astype(np.float32)


def make_in_maps(x, p, norm_w, w_in, fox_fb, hgrn_gn, hgrn_lb_logits, w_out, w_ple, w_ple_gate, final_norm_w, ncores, nseq):
    cst = host_consts()
    common = {
        "w_in": np.ascontiguousarray(w_in, dtype=np.float32),
        "w_out": np.ascontiguousarray(w_out, dtype=np.float32),
        "w_ple": np.ascontiguousarray(w_ple, dtype=np.float32),
        "w_ple_gate": np.ascontiguousarray(w_ple_gate, dtype=np.float32),
        "v_nw": _vec_layout(norm_w),
        "v_gn": _vec_layout(hgrn_gn),
        "v_lbl": _vec_layout(hgrn_lb_logits),
        "v_fnw": _vec_layout(final_norm_w[None, :]),
        "v_fb": np.ascontiguousarray(np.broadcast_to(np.asarray(fox_fb, np.float32).reshape(1, -1), (128, fox_fb.size))),
    }
    common.update({'c_' + k: v for k, v in cst.items()})
    maps = []
    for c in range(ncores):
        m = dict(common)
        m["x"] = np.ascontiguousarray(x[c * nseq:(c + 1) * nseq], dtype=np.float32)
        m["p"] = np.ascontiguousarray(p[:, c * nseq:(c + 1) * nseq], dtype=np.float32)
        maps.append(m)
    return maps


def kernel(x, p, norm_w, w_in, fox_fb, hgrn_gn, hgrn_lb_logits, w_out, w_ple, w_ple_gate, final_norm_w):
    x = np.asarray(x)
    B = x.shape[0]
    nseq = B // NCORES
    nc = build(nseq, list(range(DEPTH)), final_norm=True)
    maps = make_in_maps(x, np.asarray(p), np.asarray(norm_w), np.asarray(w_in), np.asarray(fox_fb), np.asarray(hgrn_gn),
                        np.asarray(hgrn_lb_logits), np.asarray(w_out), np.asarray(w_ple), np.asarray(w_ple_gate),
                        np.asarray(final_norm_w), NCORES, nseq)
    res = run_bass_kernel_spmd(nc, maps, core_ids=list(range(NCORES)))
    return np.concatenate([r["out"] for r in res.results], axis=0).astype(np.float32)
```

```python
import numpy as np
import ml_dtypes
from contextlib import ExitStack
import concourse.bass as bass
import concourse.mybir as mybir
from concourse.bass_utils import run_bass_kernel_spmd

F32 = mybir.dt.float32
BF16 = mybir.dt.bfloat16
AF = mybir.ActivationFunctionType
ALU = mybir.AluOpType

D = 1024
S = 2048
DEPTH = 4
NCH = 8
TW = 512
NT = S // TW
NB = S // 128
DIN = 8208
PLE = 256
EPS = 1e-6
NCORES = 8


class _Rec:
    def __init__(self):
        self.call = None

    def __getattr__(self, name):
        def f(*a, **k):
            self.call = (name, a, k)
            return None
        return f


class Prog:
    ENG = ('pe', 'act', 'dve', 'pool', 'sp')

    def __init__(self, nc, n_dma_sems=40):
        self.nc = nc
        self.ops = []
        self.last_w = {}
        self.readers = {}
        self.n_dma_sems = n_dma_sems

    def add(self, eng, fn, reads=(), writes=(), dma=False):
        i = len(self.ops)
        deps = set()
        for r in reads:
            w = self.last_w.get(r)
            if w is not None:
                deps.add((w, 0))
        for wkey in writes:
            w = self.last_w.get(wkey)
            if w is not None:
                deps.add((w, 1))
            for rd in self.readers.get(wkey, {}).values():
                deps.add((rd, 2))
        for r in reads:
            self.readers.setdefault(r, {})[('d', i) if dma else eng] = i
        for wkey in writes:
            self.last_w[wkey] = i
            self.readers[wkey] = {}
        rec = _Rec()
        fn(rec)
        self.ops.append(dict(eng=eng, call=rec.call, deps=deps, dma=dma, mark=False))
        return i

    def emit(self):
        nc = self.nc
        ops = self.ops
        for i, op in enumerate(ops):
            need = set()
            for (p, kind) in op['deps']:
                po = ops[p]
                if (not po['dma']) and po['eng'] == op['eng']:
                    if po['eng'] == 'pe' and not op['dma']:
                        continue
                    if kind == 2 and not op['dma']:
                        continue
                need.add(p)
            op['need'] = sorted(need)
            for p in op['need']:
                ops[p]['mark'] = True
            op['deps'] = None
        cnt = {e: 0 for e in self.ENG}
        dcnt = [0] * self.n_dma_sems
        nd = 0
        for op in ops:
            if op['dma']:
                k = nd % self.n_dma_sems
                nd += 1
                op['sem'] = ('dma', k)
                op['prev'] = dcnt[k]
                dcnt[k] += 16
                op['val'] = dcnt[k]
            elif op['mark']:
                cnt[op['eng']] += 1
                op['sem'] = ('eng', op['eng'])
                op['val'] = cnt[op['eng']]
        self.cnt = cnt
        with ExitStack() as st:
            sems = {}
            for e in self.ENG:
                sems[('eng', e)] = st.enter_context(nc.semaphore('s_' + e))
            for k in range(self.n_dma_sems):
                sems[('dma', k)] = st.enter_context(nc.semaphore('d_%d' % k))
            block = st.enter_context(nc.Block())
            per = {e: [op for op in ops if op['eng'] == e] for e in self.ENG}

            def run(eng_name, eng):
                waited = {}
                for op in per[eng_name]:
                    ws = [(ops[p]['sem'], ops[p]['val']) for p in op['need']]
                    if op['dma'] and op['prev'] > 0:
                        ws.append((op['sem'], op['prev']))
                    for (s, v) in ws:
                        if waited.get(s, 0) >= v:
                            continue
                        waited[s] = v
                        eng.wait_ge(sems[s], v)
                    nm, a_, k_ = op['call']
                    ins = getattr(eng, nm)(*a_, **k_)
                    if op['dma']:
                        ins.then_inc(sems[op['sem']], 16)
                    elif op['mark']:
                        ins.then_inc(sems[op['sem']], 1)
                if eng_name == 'sp':
                    for k in range(self.n_dma_sems):
                        if dcnt[k] > 0:
                            eng.wait_ge(sems[('dma', k)], dcnt[k])

            @block.tensor
            def _(e):
                run('pe', e)

            @block.scalar
            def _(e):
                run('act', e)

            @block.vector
            def _(e):
                run('dve', e)

            @block.gpsimd
            def _(e):
                run('pool', e)

            @block.sync
            def _(e):
                run('sp', e)


def host_consts():
    c = {}
    i = np.arange(128)
    c['ident32'] = np.eye(128, dtype=np.float32)
    tri = (i[:, None] <= i[None, :]).astype(np.float32)
    c['tri32'] = tri
    c['ones32'] = np.ones((128, 128), np.float32)
    m63 = np.zeros((128, 128), np.float32)
    m63[:64, :] = 1.0
    c['m63'] = m63
    hm = tri * ((i[:, None] // 32) == (i[None, :] // 32))
    c['hmask'] = hm.astype(np.float32)
    rm = np.ones((128, TW), np.float32)
    rm[:, ::32] = 0.0
    c['rmask'] = rm
    return c


CONST_NAMES = ['ident32', 'tri32', 'ones32', 'm63', 'hmask', 'rmask']


def build(nseq, layers, final_norm=True, wq='pool', phases=('norm', 'prep', 'hgrn', 'wo1', 'fox', 'wo2', 'ple'), nheads=8, dbg=False):
    nc = bass.Bass("TRN2", target_bir_lowering=False)
    NL = DEPTH
    x_d = nc.dram_tensor("x", [nseq, S, D], F32, kind="ExternalInput").ap()
    p_d = nc.dram_tensor("p", [NL, nseq, S, PLE], F32, kind="ExternalInput").ap()
    win_d = nc.dram_tensor("w_in", [NL, D, DIN], F32, kind="ExternalInput").ap()
    wout_d = nc.dram_tensor("w_out", [NL, 2 * D, D], F32, kind="ExternalInput").ap()
    wple_d = nc.dram_tensor("w_ple", [NL, PLE, D], F32, kind="ExternalInput").ap()
    wpg_d = nc.dram_tensor("w_ple_gate", [NL, D, D], F32, kind="ExternalInput").ap()
    nw_d = nc.dram_tensor("v_nw", [128, NL * NCH], F32, kind="ExternalInput").ap()
    gn_d = nc.dram_tensor("v_gn", [128, NL * NCH], F32, kind="ExternalInput").ap()
    lbl_d = nc.dram_tensor("v_lbl", [128, NL * NCH], F32, kind="ExternalInput").ap()
    fnw_d = nc.dram_tensor("v_fnw", [128, NCH], F32, kind="ExternalInput").ap()
    fb_d = nc.dram_tensor("v_fb", [128, NL * 16], F32, kind="ExternalInput").ap()
    cd = {n: nc.dram_tensor("c_" + n, [128, TW if n == 'rmask' else 128], F32, kind="ExternalInput").ap() for n in CONST_NAMES}
    out_d = nc.dram_tensor("out", [nseq, S, D], F32, kind="ExternalOutput").ap()

    with ExitStack() as st:
        def sb(name, shape, dt):
            return st.enter_context(nc.sbuf_tensor(name, shape, dt))

        def pst(name, shape, dt):
            return st.enter_context(nc.psum_tensor(name, shape, dt))

        h = sb("h", [128, NCH, S], F32)
        uT = sb("uT", [128, NCH, S], BF16)
        yT = sb("yT", [128, NCH, S], BF16)
        NWB = 8
        wb = sb("wb", [128, NWB, NCH, 128], BF16)
        NF = 10
        ft = sb("ft", [128, NF, TW], F32)
        NBT = 10
        bt = sb("bt", [128, NBT, TW], BF16)
        kAB = sb("kAB", [128, 2, S], BF16)
        VB = sb("VB", [128, NB, 128], BF16)
        W32 = sb("W32", [128, 3, 128], F32)
        W16 = sb("W16", [128, 3, 128], BF16)
        Dall = sb("Dall", [128, 68], F32)
        sm = sb("sm", [128, 6, 256], F32)
        ident32 = sb("ident32", [128, 128], F32)
        identb = sb("identb", [128, 128], BF16)
        tri32 = sb("tri32", [128, 128], F32)
        trib = sb("trib", [128, 128], BF16)
        ones32 = sb("ones32", [128, 128], F32)
        m63 = sb("m63", [128, 128], F32)
        hmask = sb("hmask", [128, 128], F32)
        rmask = sb("rmask", [128, TW], F32)
        onesD = sb("onesD", [128, 128], BF16)
        onesV = sb("onesV", [128, 128], BF16)
        onesb = sb("onesb", [128, 128], BF16)
        nw = sb("nw", [128, NL * NCH], F32)
        gn = sb("gn", [128, NL * NCH], F32)
        lb = sb("lb", [128, NL * NCH], F32)
        oml = sb("oml", [128, NL * NCH], F32)
        lbe = sb("lbe", [128, NL * NCH], F32)
        lbs = sb("lbs", [128, NCH], F32)
        fnw = sb("fnw", [128, NCH], F32)
        fb = sb("fb", [128, NL * 16], F32)
        epsb = sb("epsb", [128, 1], F32)
        wz = sb("wz", [128, NCH, 16], BF16)
        wpl = sb("wpl", [128, 2, 2, 128], BF16)

        PS = [pst("ps%d" % k, [128, TW], F32) for k in range(7)]
        PSB = pst("psb", [128, 2 * TW], BF16)

        P = Prog(nc)
        A = P.add

        cmap = dict(ident32=ident32, tri32=tri32, ones32=ones32, m63=m63, hmask=hmask, rmask=rmask)
        for n in CONST_NAMES:
            A('sp', (lambda t, s_: (lambda e: e.dma_start(out=t[:], in_=s_[:, :])))(cmap[n], cd[n]), writes=[n], dma=True)
        for (t, s_, n) in ((nw, nw_d, 'nw'), (gn, gn_d, 'gn'), (lbe, lbl_d, 'lbe'), (fnw, fnw_d, 'fnw'), (fb, fb_d, 'fb')):
            A('sp', (lambda t, s_: (lambda e: e.dma_start(out=t[:], in_=s_[:, :])))(t, s_), writes=[n], dma=True)
        A('pool', lambda e: e.tensor_copy(out=identb[:], in_=ident32[:]), reads=['ident32'], writes=['identb'])
        A('pool', lambda e: e.tensor_copy(out=trib[:], in_=tri32[:]), reads=['tri32'], writes=['trib'])
        A('pool', lambda e: e.memset(onesD[:], 1.0 / D), writes=['onesD'])
        A('pool', lambda e: e.memset(onesV[:], 1.0 / 128), writes=['onesV'])
        A('pool', lambda e: e.memset(onesb[:], 1.0), writes=['onesb'])
        A('pool', lambda e: e.memset(epsb[:], EPS), writes=['epsb'])
        A('pool', lambda e: e.memset(kAB[:, 1, :], 0.0), writes=['kB'])
        A('pool', lambda e: e.memset(kAB[64:65, 0, :], 1.0), writes=['kA'])
        A('pool', lambda e: e.memset(kAB[0:1, 1, :], 1.0), reads=['kB'], writes=['kB'])
        A('pool', lambda e: e.memset(VB[:], 0.0), writes=['VB'])
        A('pool', lambda e: e.memset(VB[:, :, 0:1], 1.0), reads=['VB'], writes=['VB'])
        A('act', lambda e: e.activation(out=lbe[:], in_=lbe[:], func=AF.Exp), reads=['lbe'], writes=['lbe'])
        A('dve', lambda e: e.tensor_tensor(out=lbs[:], in0=lbe[:, 0:NCH], in1=lbe[:, NCH:2 * NCH], op=ALU.add), reads=['lbe'], writes=['lbs'])
        A('dve', lambda e: e.tensor_tensor(out=lbs[:], in0=lbs[:], in1=lbe[:, 2 * NCH:3 * NCH], op=ALU.add), reads=['lbe', 'lbs'], writes=['lbs'])
        A('dve', lambda e: e.tensor_tensor(out=lbs[:], in0=lbs[:], in1=lbe[:, 3 * NCH:4 * NCH], op=ALU.add), reads=['lbe', 'lbs'], writes=['lbs'])
        A('dve', lambda e: e.reciprocal(out=lbs[:], in_=lbs[:]), reads=['lbs'], writes=['lbs'])
        A('dve', lambda e: e.memset(lb[:, 0:NCH], 0.0), writes=['lb'])
        for l in range(1, NL):
            A('dve', (lambda l: (lambda e: e.tensor_tensor(out=lbe[:, l * NCH:(l + 1) * NCH], in0=lbe[:, l * NCH:(l + 1) * NCH], in1=lbs[:], op=ALU.mult)))(l),
              reads=['lbe', 'lbs'], writes=['lbe'])
            A('dve', (lambda l: (lambda e: e.tensor_tensor(out=lb[:, l * NCH:(l + 1) * NCH], in0=lb[:, (l - 1) * NCH:l * NCH], in1=lbe[:, l * NCH:(l + 1) * NCH], op=ALU.add)))(l),
              reads=['lbe', 'lb'], writes=['lb'])
        A('dve', lambda e: e.tensor_scalar(out=oml[:], in0=lb[:], scalar1=-1.0, scalar2=1.0, op0=ALU.mult, op1=ALU.add), reads=['lb'], writes=['oml'])

        wslot = [0]

        def wload(src_ap, nk=NCH, ncols=128):
            k = wslot[0] % NWB
            wslot[0] += 1
            dst = wb[:, k, 0:nk, 0:ncols]
            A(wq, lambda e: e.dma_start(out=dst, in_=src_ap), writes=[('wb', k)], dma=True)
            return k

        def win_chunk(l, col0, ncols=128):
            return wload(win_d[l].rearrange("(c p) e -> p c e", p=128)[:, :, col0:col0 + ncols], NCH, ncols)

        def proj_fm(k, t0, tw, ps_ap, psres, ncols=128):
            for c in range(NCH):
                A('pe', (lambda c: (lambda e: e.matmul(ps_ap, lhsT=wb[:, k, c, 0:ncols], rhs=uT[:, c, t0:t0 + tw], start=(c == 0), stop=(c == NCH - 1))))(c),
                  reads=[('wb', k), ('uT', c, t0 // TW)], writes=psres)

        def proj_tm(k, tb, ps_ap, psres, ncols=128):
            for c in range(NCH):
                A('pe', (lambda c: (lambda e: e.matmul(ps_ap, lhsT=uT[:, c, tb * 128:(tb + 1) * 128], rhs=wb[:, k, c, 0:ncols], start=(c == 0), stop=(c == NCH - 1))))(c),
                  reads=[('wb', k), ('uT', c, tb // 4)], writes=psres)

        def rmsnorm_to_u(l):
            for t in range(NT):
                ts = slice(t * TW, (t + 1) * TW)
                sq_list = []
                for c in range(NCH):
                    slot = c % 4
                    A('act', (lambda c, slot: (lambda e: e.activation(out=bt[:, slot, :], in_=h[:, c, ts], func=AF.Square)))(c, slot),
                      reads=[('h', c, t)], writes=[('bt', slot)])
                    A('pe', (lambda c, slot: (lambda e: e.matmul(PS[0][:], lhsT=onesD[:], rhs=bt[:, slot, :], start=(c == 0), stop=(c == NCH - 1))))(c, slot),
                      reads=['onesD', ('bt', slot)], writes=[('ps', 0)])
                A('act', lambda e: e.activation(out=ft[:, 0, :], in_=PS[0][:], func=AF.Sqrt, bias=epsb[:], scale=1.0),
                  reads=[('ps', 0), 'epsb'], writes=[('ft', 0)])
                A('dve', lambda e: e.reciprocal(out=ft[:, 0, :], in_=ft[:, 0, :]), reads=[('ft', 0)], writes=[('ft', 0)])
                for c in range(NCH):
                    A('dve', (lambda c: (lambda e: e.scalar_tensor_tensor(out=uT[:, c, ts], in0=h[:, c, ts], scalar=nw[:, l * NCH + c:l * NCH + c + 1],
                                                                         in1=ft[:, 0, :], op0=ALU.mult, op1=ALU.mult)))(c),
                      reads=[('h', c, t), ('ft', 0), 'nw'], writes=[('uT', c, t)])

        def load_x(s):
            for tb in range(NB):
                stg = ft[:, 8:10, :]
                A('sp', (lambda tb: (lambda e: e.dma_start(out=stg, in_=x_d[s, tb * 128:(tb + 1) * 128, :].rearrange("p (a b) -> p a b", a=2))))(tb),
                  writes=[('ft', 8), ('ft', 9)], dma=True)
                for half in range(2):
                    pk = 1 + half
                    for cc in range(4):
                        c = half * 4 + cc
                        A('pe', (lambda c, cc, pk, half: (lambda e: e.transpose(out=PS[pk][:, cc * 128:(cc + 1) * 128], in_=ft[:, 8 + half, cc * 128:(cc + 1) * 128], identity=ident32[:])))(c, cc, pk, half),
                          reads=[('ft', 8 + half), 'ident32'], writes=[('ps', pk)])
                    eng = 'act' if half == 0 else 'dve'
                    if eng == 'act':
                        A('act', (lambda pk, half, tb: (lambda e: e.activation(out=h[:, half * 4:half * 4 + 4, tb * 128:(tb + 1) * 128],
                                                                              in_=PS[pk][:].rearrange("p (a b) -> p a b", a=4), func=AF.Copy)))(pk, half, tb),
                          reads=[('ps', pk)], writes=[('h', half * 4 + cc, tb // 4) for cc in range(4)])
                    else:
                        A('dve', (lambda pk, half, tb: (lambda e: e.tensor_copy(out=h[:, half * 4:half * 4 + 4, tb * 128:(tb + 1) * 128],
                                                                               in_=PS[pk][:].rearrange("p (a b) -> p a b", a=4))))(pk, half, tb),
                          reads=[('ps', pk)], writes=[('h', half * 4 + cc, tb // 4) for cc in range(4)])

        def store_out(s, normed):
            for t in range(NT):
                ts = slice(t * TW, (t + 1) * TW)
                if normed:
                    for c in range(NCH):
                        slot = c % 4
                        A('act', (lambda c, slot: (lambda e: e.activation(out=bt[:, slot, :], in_=h[:, c, ts], func=AF.Square)))(c, slot),
                          reads=[('h', c, t)], writes=[('bt', slot)])
                        A('pe', (lambda c, slot: (lambda e: e.matmul(PS[0][:], lhsT=onesD[:], rhs=bt[:, slot, :], start=(c == 0), stop=(c == NCH - 1))))(c, slot),
                          reads=['onesD', ('bt', slot)], writes=[('ps', 0)])
                    A('act', lambda e: e.activation(out=ft[:, 0, :], in_=PS[0][:], func=AF.Sqrt, bias=epsb[:], scale=1.0),
                      reads=[('ps', 0), 'epsb'], writes=[('ft', 0)])
                    A('dve', lambda e: e.reciprocal(out=ft[:, 0, :], in_=ft[:, 0, :]), reads=[('ft', 0)], writes=[('ft', 0)])
                    for c in range(NCH):
                        A('dve', (lambda c: (lambda e: e.scalar_tensor_tensor(out=h[:, c, ts], in0=h[:, c, ts], scalar=fnw[:, c:c + 1],
                                                                             in1=ft[:, 0, :], op0=ALU.mult, op1=ALU.mult)))(c),
                          reads=[('h', c, t), ('ft', 0), 'fnw'], writes=[('h', c, t)])
                for q in range(4):
                    tb = t * 4 + q
                    for half in range(2):
                        pk = 1 + half
                        for cc in range(4):
                            c = half * 4 + cc
                            A('pe', (lambda c, cc, pk, tb: (lambda e: e.transpose(out=PS[pk][:, cc * 128:(cc + 1) * 128], in_=h[:, c, tb * 128:(tb + 1) * 128], identity=ident32[:])))(c, cc, pk, tb),
                              reads=[('h', c, t), 'ident32'], writes=[('ps', pk)])
                        if half == 0:
                            A('act', (lambda pk, half: (lambda e: e.activation(out=ft[:, 8 + half, :], in_=PS[pk][:], func=AF.Copy)))(pk, half),
                              reads=[('ps', pk)], writes=[('ft', 8 + half)])
                        else:
                            A('dve', (lambda pk, half: (lambda e: e.tensor_copy(out=ft[:, 8 + half, :], in_=PS[pk][:])))(pk, half),
                              reads=[('ps', pk)], writes=[('ft', 8 + half)])
                    A('sp', (lambda tb: (lambda e: e.dma_start(out=out_d[s, tb * 128:(tb + 1) * 128, :].rearrange("p (a b) -> p a b", a=2), in_=ft[:, 8:10, :])))(tb),
                      reads=[('ft', 8), ('ft', 9)], dma=True)

        def hgrn_head(l, hh):
            kq = win_chunk(l, 0 * D + hh * 128)
            kf = win_chunk(l, 1 * D + hh * 128)
            ki = win_chunk(l, 2 * D + hh * 128)
            kg = win_chunk(l, 3 * D + hh * 128)
            lc = l * NCH + hh
            lb_ap = lb[:, lc:lc + 1]
            oml_ap = oml[:, lc:lc + 1]
            gn_ap = gn[:, lc:lc + 1]
            A('pool', lambda e: e.memset(W32[:, 2, :], 0.0), writes=[('W32', 2)])
            A('pool', lambda e: e.memset(W16[:, 2, :], 0.0), writes=[('W16', 2)])
            A('pool', lambda e: e.memset(Dall[:, 0:1], 1.0), writes=['Dall'])
            for sg in range(NT):
                t0 = sg * TW
                proj_fm(kf, t0, TW, PS[0][:], [('ps', 0)])
                A('act', lambda e: e.activation(out=ft[:, 1, :], in_=PS[0][:], func=AF.Sigmoid), reads=[('ps', 0)], writes=[('ft', 1)])
                A('act', lambda e: e.activation(out=ft[:, 2, :], in_=PS[0][:], func=AF.Sigmoid, scale=-1.0), reads=[('ps', 0)], writes=[('ft', 2)])
                A('dve', lambda e: e.tensor_scalar(out=ft[:, 1, :], in0=ft[:, 1, :], scalar1=oml_ap, scalar2=lb_ap, op0=ALU.mult, op1=ALU.add),
                  reads=[('ft', 1), 'oml', 'lb'], writes=[('ft', 1)])
                A('act', lambda e: e.activation(out=ft[:, 3, :], in_=ft[:, 1, :], func=AF.Ln), reads=[('ft', 1)], writes=[('ft', 3)])
                A('dve', lambda e: e.tensor_tensor_scan(out=ft[:, 4, :], data0=rmask[:], data1=ft[:, 3, :], initial=0.0, op0=ALU.mult, op1=ALU.add),
                  reads=[('ft', 3), 'rmask'], writes=[('ft', 4)])
                A('act', lambda e: e.activation(out=ft[:, 5, :], in_=ft[:, 4, :], func=AF.Exp), reads=[('ft', 4)], writes=[('ft', 5)])
                A('act', lambda e: e.activation(out=ft[:, 6, :], in_=ft[:, 4, :], func=AF.Exp, scale=-1.0), reads=[('ft', 4)], writes=[('ft', 6)])
                A('dve', lambda e: e.scalar_tensor_tensor(out=bt[:, 4, :], in0=ft[:, 2, :], scalar=oml_ap, in1=ft[:, 6, :], op0=ALU.mult, op1=ALU.mult),
                  reads=[('ft', 2), ('ft', 6), 'oml'], writes=[('bt', 4)])
                A('pool', (lambda sg: (lambda e: e.tensor_copy(out=Dall[:, 1 + 16 * sg:1 + 16 * sg + 16], in_=ft[:, 5, 31::32])))(sg),
                  reads=[('ft', 5), 'Dall'], writes=['Dall'])
                proj_fm(kq, t0, TW, PS[1][:], [('ps', 1)])
                A('dve', lambda e: e.scalar_tensor_tensor(out=bt[:, 5, :], in0=PS[1][:], scalar=float(128 ** -0.5), in1=ft[:, 5, :], op0=ALU.mult, op1=ALU.mult),
                  reads=[('ps', 1), ('ft', 5)], writes=[('bt', 5)])
                A('pool', (lambda sg: (lambda e: e.tensor_tensor(out=bt[:, 6, :].rearrange("p (a b) -> p a b", b=32), in0=bt[:, 5, :].rearrange("p (a b) -> p a b", b=32),
                                                                 in1=bcast_last(Dall[:, 16 * sg:16 * sg + 16], 32), op=ALU.mult)))(sg),
                  reads=[('bt', 5), 'Dall'], writes=[('bt', 6)])
                proj_fm(kg, t0, TW, PS[1][:], [('ps', 1)])
                A('act', lambda e: e.activation(out=ft[:, 7, :], in_=PS[1][:], func=AF.Silu), reads=[('ps', 1)], writes=[('ft', 7)])
                for q in range(4):
                    proj_tm(ki, sg * 4 + q, PS[3][:, q * 128:(q + 1) * 128], [('ps', 3)])
                A('act', lambda e: e.activation(out=bt[:, 9, :], in_=PS[3][:], func=AF.Copy), reads=[('ps', 3)], writes=[('bt', 9)])
                for q in range(4):
                    A('pe', (lambda q: (lambda e: e.transpose(out=PSB[:, q * 128:(q + 1) * 128], in_=bt[:, 4, q * 128:(q + 1) * 128], identity=identb[:])))(q),
                      reads=[('bt', 4), 'identb'], writes=['psb'])
                A('dve', lambda e: e.tensor_copy(out=bt[:, 8, :], in_=PSB[:, 0:TW]), reads=['psb'], writes=[('bt', 8)])
                def emit_U(idx):
                    q, cc = divmod(idx, 4)
                    ub = 6 if idx % 2 == 0 else 2
                    rows = slice(cc * 32, cc * 32 + 32)
                    A('pe', lambda e: e.matmul(PS[ub][:, 0:128], lhsT=bt[rows, 8, q * 128:(q + 1) * 128], rhs=bt[rows, 9, q * 128:(q + 1) * 128],
                                               start=True, stop=True, tile_position=(cc * 32, 0)),
                      reads=[('bt', 8), ('bt', 9)], writes=psr(ub))
                for q in range(4):
                    A('pe', (lambda q: (lambda e: e.matmul(PS[4][:, q * 128:(q + 1) * 128], lhsT=bt[:, 4, q * 128:(q + 1) * 128], rhs=bt[:, 5, q * 128:(q + 1) * 128], start=True, stop=True)))(q),
                      reads=[('bt', 4), ('bt', 5)], writes=[('ps', 4)])
                A('dve', lambda e: e.tensor_tensor(out=bt[:, 3, :].rearrange("p (a b) -> p a b", a=4), in0=PS[4][:].rearrange("p (a b) -> p a b", a=4),
                                                   in1=bcast_mid(hmask[:], 4), op=ALU.mult),
                  reads=[('ps', 4), 'hmask'], writes=[('bt', 3)])
                emit_U(0)
                for idx in range(16):
                    q, cc = divmod(idx, 4)
                    cg = sg * 16 + idx
                    prev = (cg + 2) % 3
                    cur = cg % 3
                    ub = 6 if idx % 2 == 0 else 2
                    cs = slice(q * 128 + cc * 32, q * 128 + cc * 32 + 32)
                    if cc == 0:
                        A('pe', lambda e: e.matmul(PS[5][:, q * 128:(q + 1) * 128], lhsT=bt[:, 9, q * 128:(q + 1) * 128], rhs=bt[:, 3, q * 128:(q + 1) * 128], start=True, stop=False),
                          reads=[('bt', 9), ('bt', 3)], writes=[('ps', 5)])
                    if idx + 1 < 16:
                        emit_U(idx + 1)
                    A('pe', lambda e: e.matmul(PS[5][:, cs], lhsT=W16[:, prev, :], rhs=bt[:, 6, cs], start=False, stop=(cc == 3)),
                      reads=[('W16', prev), ('bt', 6)], writes=[('ps', 5)])
                    A('dve', lambda e: e.scalar_tensor_tensor(out=W32[:, cur, :], in0=W32[:, prev, :], scalar=Dall[:, cg:cg + 1], in1=PS[ub][:, 0:128], op0=ALU.mult, op1=ALU.add),
                      reads=[('W32', prev), 'Dall'] + psr(ub), writes=[('W32', cur)])
                    A('pool', lambda e: e.tensor_copy(out=W16[:, cur, :], in_=W32[:, cur, :]), reads=[('W32', cur)], writes=[('W16', cur)])
                A('act', lambda e: e.activation(out=bt[:, 7, :], in_=PS[5][:], func=AF.Square), reads=[('ps', 5)], writes=[('bt', 7)])
                A('pe', lambda e: e.matmul(PS[0][:], lhsT=onesV[:], rhs=bt[:, 7, :], start=True, stop=True), reads=['onesV', ('bt', 7)], writes=[('ps', 0)])
                A('act', lambda e: e.activation(out=ft[:, 0, :], in_=PS[0][:], func=AF.Sqrt, bias=epsb[:], scale=1.0), reads=[('ps', 0), 'epsb'], writes=[('ft', 0)])
                A('dve', lambda e: e.reciprocal(out=ft[:, 0, :], in_=ft[:, 0, :]), reads=[('ft', 0)], writes=[('ft', 0)])
                A('dve', lambda e: e.scalar_tensor_tensor(out=ft[:, 0, :], in0=PS[5][:], scalar=gn_ap, in1=ft[:, 0, :], op0=ALU.mult, op1=ALU.mult),
                  reads=[('ps', 5), ('ft', 0), 'gn'], writes=[('ft', 0)])
                A('pool', (lambda t0, sg: (lambda e: e.tensor_tensor(out=yT[:, hh, t0:t0 + TW], in0=ft[:, 0, :], in1=ft[:, 7, :], op=ALU.mult)))(t0, sg),
                  reads=[('ft', 0), ('ft', 7)], writes=[('yT', hh, sg)])

        def bcast_last(ap2, n):
            return ap2.unsqueeze(2).to_broadcast([ap2.shape[0], ap2.shape[1], n])

        def bcast_mid(ap2, n):
            return ap2.unsqueeze(1).to_broadcast([ap2.shape[0], n, ap2.shape[1]])

        PS6 = [('psu', i) for i in range(4)]

        def psr(k):
            return PS6 if k == 6 else [('ps', k)]

        def wout_pass(l, half, swap):
            for dm in range(NCH):
                src = wout_d[l, half * D:(half + 1) * D, dm * 128:(dm + 1) * 128].rearrange("(c p) e -> p c e", p=128)
                if not swap:
                    k = wload(src)
                else:
                    k = wload(src)
                for t in range(NT):
                    pk = 1 + (t % 2)
                    for c in range(NCH):
                        A('pe', (lambda c, t, pk: (lambda e: e.matmul(PS[pk][:], lhsT=wb[:, k, c, :], rhs=yT[:, c, t * TW:(t + 1) * TW], start=(c == 0), stop=(c == NCH - 1))))(c, t, pk),
                          reads=[('wb', k), ('yT', c, t)], writes=[('ps', pk)])
                    A('dve', (lambda t, pk, dm: (lambda e: e.tensor_tensor(out=h[:, dm, t * TW:(t + 1) * TW], in0=h[:, dm, t * TW:(t + 1) * TW], in1=PS[pk][:], op=ALU.add)))(t, pk, dm),
                      reads=[('ps', pk), ('h', dm, t)], writes=[('h', dm, t)])

        def fox_prep(l):
            A(wq, lambda e: e.dma_start(out=wz[:], in_=win_d[l].rearrange("(c p) e -> p c e", p=128)[:, :, 8 * D:8 * D + 16]), writes=['wz'], dma=True)
            for tb in range(NB):
                for c in range(NCH):
                    A('pe', (lambda c, tb: (lambda e: e.matmul(PS[0][:, tb * 16:(tb + 1) * 16], lhsT=uT[:, c, tb * 128:(tb + 1) * 128], rhs=wz[:, c, :], start=(c == 0), stop=(c == NCH - 1))))(c, tb),
                      reads=['wz', ('uT', c, tb // 4)], writes=[('ps', 0)])
            A('dve', lambda e: e.tensor_tensor(out=sm[:, 0, :].rearrange("p (a b) -> p a b", b=16), in0=PS[0][:, 0:256].rearrange("p (a b) -> p a b", b=16),
                                               in1=bcast_mid(fb[:, l * 16:(l + 1) * 16], 16), op=ALU.add),
              reads=[('ps', 0), 'fb'], writes=[('sm', 0)])
            A('act', lambda e: e.activation(out=sm[:, 0, :], in_=sm[:, 0, :], func=AF.Sigmoid), reads=[('sm', 0)], writes=[('sm', 0)])
            A('act', lambda e: e.activation(out=sm[:, 0, :], in_=sm[:, 0, :], func=AF.Ln), reads=[('sm', 0)], writes=[('sm', 0)])
            A('pe', lambda e: e.matmul(PS[1][:, 0:256], lhsT=tri32[:], rhs=sm[:, 0, :], start=True, stop=True), reads=['tri32', ('sm', 0)], writes=[('ps', 1)])
            A('pe', lambda e: e.matmul(PS[2][:, 0:256], lhsT=ones32[:], rhs=sm[:, 0, :], start=True, stop=True), reads=['ones32', ('sm', 0)], writes=[('ps', 2)])
            A('pe', lambda e: e.matmul(PS[3][:, 0:256], lhsT=m63[:], rhs=sm[:, 0, :], start=True, stop=True), reads=['m63', ('sm', 0)], writes=[('ps', 3)])
            A('act', lambda e: e.activation(out=sm[:, 4, :], in_=PS[2][:, 0:256], func=AF.Copy), reads=[('ps', 2)], writes=[('sm', 4)])
            A('dve', lambda e: e.memset(sm[:, 1, 0:16], 0.0), writes=[('sm', 1)])
            for j in range(1, NB):
                A('dve', (lambda j: (lambda e: e.tensor_tensor(out=sm[:, 1, j * 16:(j + 1) * 16], in0=sm[:, 1, (j - 1) * 16:j * 16], in1=sm[:, 4, (j - 1) * 16:j * 16], op=ALU.add)))(j),
                  reads=[('sm', 1), ('sm', 4)], writes=[('sm', 1)])
            A('dve', lambda e: e.scalar_tensor_tensor(out=sm[:, 2, :], in0=PS[1][:, 0:256], scalar=-1.0, in1=sm[:, 1, :], op0=ALU.mult, op1=ALU.subtract),
              reads=[('ps', 1), ('sm', 1)], writes=[('sm', 2)])
            A('dve', lambda e: e.tensor_tensor(out=sm[:, 3, :], in0=PS[3][:, 0:256], in1=sm[:, 1, :], op=ALU.add), reads=[('ps', 3), ('sm', 1)], writes=[('sm', 3)])
            A('dve', lambda e: e.tensor_scalar(out=sm[:, 3, :], in0=sm[:, 3, :], scalar1=8.0, scalar2=None, op0=ALU.mult), reads=[('sm', 3)], writes=[('sm', 3)])

        def fox_pair(l, pp):
            base = 4 * D
            kfq = win_chunk(l, base + 0 * D + pp * 128)
            kfk = win_chunk(l, base + 1 * D + pp * 128)
            kfv = win_chunk(l, base + 2 * D + pp * 128)
            kfg = win_chunk(l, base + 3 * D + pp * 128)
            hA, hB = 2 * pp, 2 * pp + 1
            for t in range(NT):
                ts = slice(t * TW, (t + 1) * TW)
                pk = t % 2
                proj_fm(kfk, t * TW, TW, PS[pk][:], [('ps', pk)])
                if t % 2 == 0:
                    A('act', (lambda pk, ts: (lambda e: e.activation(out=kAB[:, 0, ts], in_=PS[pk][:], func=AF.Copy)))(pk, ts), reads=[('ps', pk)], writes=['kA'])
                else:
                    A('dve', (lambda pk, ts: (lambda e: e.tensor_copy(out=kAB[:, 0, ts], in_=PS[pk][:])))(pk, ts), reads=[('ps', pk)], writes=['kA'])
            for g in range(4):
                pk = g % 2
                for q in range(4):
                    proj_tm(kfv, g * 4 + q, PS[pk][:, q * 128:(q + 1) * 128], [('ps', pk)])
                if g % 2 == 0:
                    A('act', (lambda pk, g: (lambda e: e.activation(out=VB[:, g * 4:(g + 1) * 4, :], in_=PS[pk][:].rearrange("p (a b) -> p a b", a=4), func=AF.Copy)))(pk, g),
                      reads=[('ps', pk)], writes=['VB'])
                else:
                    A('dve', (lambda pk, g: (lambda e: e.tensor_copy(out=VB[:, g * 4:(g + 1) * 4, :], in_=PS[pk][:].rearrange("p (a b) -> p a b", a=4))))(pk, g),
                      reads=[('ps', pk)], writes=['VB'])
            FST = 9
            for qi in range(NT if FST >= 2 else 0):
                ts = slice(qi * TW, (qi + 1) * TW)
                qa, qb = 0 + (qi % 2) * 2, 1 + (qi % 2) * 2
                pk = 2
                proj_fm(kfq, qi * TW, TW, PS[pk][:], [('ps', pk)])
                A('act', (lambda pk, qa: (lambda e: e.activation(out=bt[:, qa, :], in_=PS[pk][:], func=AF.Copy)))(pk, qa), reads=[('ps', pk)], writes=[('bt', qa)])
                for hd_, hx_ in ((0, hA), (1, hB)):
                    A('pool', (lambda hd_, hx_, qi: (lambda e: e.tensor_copy(out=bt[0:1, 8 + hd_, :].rearrange("p (a b) -> p a b", a=4),
                                                                             in_=bcast_last(sm[0:1, 3, :].rearrange("p (a b) -> p a b", b=16)[:, 4 * qi:4 * qi + 4, hx_], 128))))(hd_, hx_, qi),
                      reads=[('sm', 3)], writes=[('bt', 8 + hd_)])
                pg = 2
                proj_fm(kfg, qi * TW, TW, PS[pg][:], [('ps', pg)])
                A('act', lambda e: e.activation(out=ft[:, 7, :], in_=PS[pg][:], func=AF.Silu), reads=[('ps', pg)], writes=[('ft', 7)])
                for hd in range(2 if FST >= 3 else 0):
                    hidx = hA if hd == 0 else hB
                    qs = qa if hd == 0 else qb
                    rows = slice(0, 64) if hd == 0 else slice(64, 128)
                    po = 5 + hd
                    nkb = 4 * qi + 4
                    def emit_S(j):
                        c0 = max(j - 4 * qi, 0) * 128
                        psk = 3 + (j % 2)
                        A('pe', lambda e: e.matmul(PS[psk][:, c0:TW], lhsT=kAB[rows, 0, j * 128:(j + 1) * 128], rhs=bt[rows, qa, c0:TW], start=True, stop=False),
                          reads=['kA', ('bt', qa)], writes=[('ps', psk)])
                        A('pe', lambda e: e.matmul(PS[psk][:, c0:TW], lhsT=onesb[0:1, :], rhs=bt[0:1, 8 + hd, c0:TW], start=False, stop=True),
                          reads=['onesb', ('bt', 8 + hd)], writes=[('ps', psk)])
                    LS = 1
                    if LS:
                        emit_S(0)
                    for j in range(nkb):
                        r = j - 4 * qi
                        c0 = max(r, 0) * 128
                        psk = 3 + (j % 2)
                        ptk = 4 + (j % 4)
                        if LS and j + 1 < nkb:
                            emit_S(j + 1)
                        if not LS:
                            emit_S(j)
                        A('act', lambda e: e.activation(out=bt[:, ptk, c0:TW], in_=PS[psk][:, c0:TW], func=AF.Exp, bias=sm[:, 2, j * 16 + hidx:j * 16 + hidx + 1], scale=0.125),
                          reads=[('ps', psk), ('sm', 2)], writes=[('bt', ptk)])
                        if r >= 0:
                            A('pool', lambda e: e.tensor_tensor(out=bt[:, ptk, c0:c0 + 128], in0=bt[:, ptk, c0:c0 + 128], in1=trib[:], op=ALU.mult),
                              reads=[('bt', ptk), 'trib'], writes=[('bt', ptk)])
                        A('pe', lambda e: e.matmul(PS[hd][:, c0:TW], lhsT=onesb[:], rhs=bt[:, ptk, c0:TW], start=(j == 0), stop=(j == nkb - 1)),
                          reads=['onesb', ('bt', ptk)], writes=[('ps', hd)])
                        if hd == 0:
                            A('pe', lambda e: e.matmul(PS[po][0:64, c0:TW], lhsT=VB[:, j, 0:64], rhs=bt[:, ptk, c0:TW], start=(j == 0), stop=(j == nkb - 1)),
                              reads=['VB', ('bt', ptk)], writes=psr(po))
                        else:
                            A('pe', lambda e: e.matmul(PS[po][:, c0:TW], lhsT=VB[:, j, :], rhs=bt[:, ptk, c0:TW], start=(j == 0), stop=(j == nkb - 1)),
                              reads=['VB', ('bt', ptk)], writes=psr(po))
                    orow = slice(0, 64) if hd == 0 else slice(64, 128)
                    A('dve', (lambda orow, hd: (lambda e: e.reciprocal(out=ft[orow, 1, :], in_=PS[hd][orow, :])))(orow, hd), reads=[('ps', hd)], writes=[('ft', 1)])
                    A('dve', (lambda orow: (lambda e: e.tensor_tensor(out=ft[orow, 2, :], in0=ft[orow, 1, :], in1=ft[orow, 7, :], op=ALU.mult)))(orow),
                      reads=[('ft', 1), ('ft', 7)], writes=[('ft', 2)])
                    A('dve', (lambda orow, po, qi: (lambda e: e.tensor_tensor(out=yT[orow, pp, qi * TW:(qi + 1) * TW], in0=PS[po][orow, :], in1=ft[orow, 2, :], op=ALU.mult)))(orow, po, qi),
                      reads=psr(po) + [('ft', 2)], writes=[('yT', pp, qi)])

        def ple_phase(l, s):
            for c in range(NCH):
                for t in range(NT):
                    eng = 'pool' if (c + t) % 2 == 0 else 'act'
                    if eng == 'pool':
                        A('pool', (lambda c, t: (lambda e: e.tensor_copy(out=uT[:, c, t * TW:(t + 1) * TW], in_=h[:, c, t * TW:(t + 1) * TW])))(c, t),
                          reads=[('h', c, t)], writes=[('uT', c, t)])
                    else:
                        A('act', (lambda c, t: (lambda e: e.activation(out=uT[:, c, t * TW:(t + 1) * TW], in_=h[:, c, t * TW:(t + 1) * TW], func=AF.Copy)))(c, t),
                          reads=[('h', c, t)], writes=[('uT', c, t)])
            for t in range(NT):
                stg = ft[:, 8:10, :]
                A('sp', (lambda t: (lambda e: e.dma_start(out=stg.rearrange("p a (q k) -> p (a q) k", k=PLE), in_=p_d[l, s, t * TW:(t + 1) * TW, :].rearrange("(q p) k -> p q k", p=128))))(t),
                  writes=[('ft', 8), ('ft', 9)], dma=True)
                for kc in range(2):
                    pk = 1 + kc
                    for q in range(4):
                        a, qq = divmod(q, 2)
                        A('pe', (lambda kc, q, a, qq, pk: (lambda e: e.transpose(out=PS[pk][:, q * 128:(q + 1) * 128], in_=ft[:, 8 + a, qq * PLE + kc * 128:qq * PLE + kc * 128 + 128], identity=ident32[:])))(kc, q, a, qq, pk),
                          reads=[('ft', 8 + a), 'ident32'], writes=[('ps', pk)])
                    if kc == 0:
                        A('act', (lambda pk, t: (lambda e: e.activation(out=kAB[:, 0, t * TW:(t + 1) * TW], in_=PS[pk][:], func=AF.Copy)))(pk, t), reads=[('ps', pk)], writes=['kA'])
                    else:
                        A('dve', (lambda pk, t: (lambda e: e.tensor_copy(out=kAB[:, 1, t * TW:(t + 1) * TW], in_=PS[pk][:])))(pk, t), reads=[('ps', pk)], writes=['kB'])
            for dm in range(NCH):
                kg = wload(wpg_d[l, :, dm * 128:(dm + 1) * 128].rearrange("(c p) e -> p c e", p=128))
                kp = wload(wple_d[l, :, dm * 128:(dm + 1) * 128].rearrange("(c p) e -> p c e", p=128), nk=2)
                for t in range(NT):
                    ts = slice(t * TW, (t + 1) * TW)
                    pa, pb = 3 + (t % 2), 5 + (t % 2)
                    for c in range(NCH):
                        A('pe', (lambda c, pa, ts, t: (lambda e: e.matmul(PS[pa][:], lhsT=wb[:, kg, c, :], rhs=uT[:, c, ts], start=(c == 0), stop=(c == NCH - 1))))(c, pa, ts, t),
                          reads=[('wb', kg), ('uT', c, t)], writes=[('ps', pa)])
                    for kc in range(2):
                        A('pe', (lambda kc, pb, ts: (lambda e: e.matmul(PS[pb][:], lhsT=wb[:, kp, kc, :], rhs=kAB[:, kc, ts], start=(kc == 0), stop=(kc == 1))))(kc, pb, ts),
                          reads=[('wb', kp), 'kA', 'kB'], writes=psr(pb))
                    f1 = 1 + (t % 2)
                    A('act', (lambda pa, f1: (lambda e: e.activation(out=ft[:, f1, :], in_=PS[pa][:], func=AF.Sigmoid)))(pa, f1), reads=[('ps', pa)], writes=[('ft', f1)])
                    A('dve', (lambda pb, f1: (lambda e: e.tensor_tensor(out=ft[:, f1, :], in0=PS[pb][:], in1=ft[:, f1, :], op=ALU.mult)))(pb, f1), reads=psr(pb) + [('ft', f1)], writes=[('ft', f1)])
                    A('pool', (lambda dm, ts, f1, t: (lambda e: e.tensor_tensor(out=h[:, dm, ts], in0=h[:, dm, ts], in1=ft[:, f1, :], op=ALU.add)))(dm, ts, f1, t),
                      reads=[('h', dm, t), ('ft', f1)], writes=[('h', dm, t)])

        for s in range(nseq):
            load_x(s)
            for l in layers:
                if 'norm' in phases:
                    rmsnorm_to_u(l)
                if 'prep' in phases:
                    fox_prep(l)
                if 'hgrn' in phases:
                    for hh in range(nheads):
                        hgrn_head(l, hh)
                if 'wo1' in phases:
                    wout_pass(l, 0, False)
                if 'fox' in phases:
                    for pp in range(nheads):
                        fox_pair(l, pp)
                if 'wo2' in phases:
                    wout_pass(l, 1, False)
                if 'ple' in phases:
                    ple_phase(l, s)
            store_out(s, final_norm)
        if dbg:
            dft = nc.dram_tensor("dbg_ft", [128, NF, TW], F32, kind="ExternalOutput").ap()
            dbt = nc.dram_tensor("dbg_bt", [128, NBT, TW], BF16, kind="ExternalOutput").ap()
            dy = nc.dram_tensor("dbg_y", [128, NCH, S], BF16, kind="ExternalOutput").ap()
            du = nc.dram_tensor("dbg_u", [128, NCH, S], BF16, kind="ExternalOutput").ap()
            dD = nc.dram_tensor("dbg_D", [128, 68], F32, kind="ExternalOutput").ap()
            dW = nc.dram_tensor("dbg_W", [128, 3, 128], F32, kind="ExternalOutput").ap()
            dsm = nc.dram_tensor("dbg_sm", [128, 6, 256], F32, kind="ExternalOutput").ap()
            A('sp', lambda e: e.dma_start(out=dft[:, :, :], in_=ft[:]), reads=[('ft', i) for i in range(NF)], dma=True)
            A('sp', lambda e: e.dma_start(out=dbt[:, :, :], in_=bt[:]), reads=[('bt', i) for i in range(NBT)], dma=True)
            A('sp', lambda e: e.dma_start(out=dy[:, :, :], in_=yT[:]), reads=[('yT', c, t) for c in range(NCH) for t in range(NT)], dma=True)
            A('sp', lambda e: e.dma_start(out=du[:, :, :], in_=uT[:]), reads=[('uT', c, t) for c in range(NCH) for t in range(NT)], dma=True)
            A('sp', lambda e: e.dma_start(out=dD[:, :], in_=Dall[:]), reads=['Dall'], dma=True)
            A('sp', lambda e: e.dma_start(out=dW[:, :, :], in_=W32[:]), reads=[('W32', i) for i in range(3)], dma=True)
            A('sp', lambda e: e.dma_start(out=dsm[:, :, :], in_=sm[:]), reads=[('sm', i) for i in range(6)], dma=True)
        P.emit()
        nc._prog_stats = dict(nops=len(P.ops), cnt=P.cnt)
    return nc


def _vec_layout(v):
    L = v.shape[0]
    return np.ascontiguousarray(v.reshape(L, NCH, 128).transpose(2, 0, 1).reshape(128, L * NCH)).astype(np.float32)


def make_in_maps(x, p, norm_w, w_in, fox_fb, hgrn_gn, hgrn_lb_logits, w_out, w_ple, w_ple_gate, final_norm_w, ncores, nseq):
    cst = host_consts()
    common = {
        "w_in": np.ascontiguousarray(w_in, dtype=np.float32),
        "w_out": np.ascontiguousarray(w_out, dtype=np.float32),
        "w_ple": np.ascontiguousarray(w_ple, dtype=np.float32),
        "w_ple_gate": np.ascontiguousarray(w_ple_gate, dtype=np.float32),
        "v_nw": _vec_layout(norm_w),
        "v_gn": _vec_layout(hgrn_gn),
        "v_lbl": _vec_layout(hgrn_lb_logits),
        "v_fnw": _vec_layout(final_norm_w[None, :]),
        "v_fb": np.ascontiguousarray(np.broadcast_to(np.asarray(fox_fb, np.float32).reshape(1, -1), (128, fox_fb.size))),
    }
    common.update({'c_' + k: v for k, v in cst.items()})
    maps = []
    for c in range(ncores):
        m = dict(common)
        m["x"] = np.ascontiguousarray(x[c * nseq:(c + 1) * nseq], dtype=np.float32)
        m["p"] = np.ascontiguousarray(p[:, c * nseq:(c + 1) * nseq], dtype=np.float32)
        maps.append(m)
    return maps


def kernel(x, p, norm_w, w_in, fox_fb, hgrn_gn, hgrn_lb_logits, w_out, w_ple, w_ple_gate, final_norm_w):
    x = np.asarray(x)
    B = x.shape[0]
    nseq = B // NCORES
    nc = build(nseq, list(range(DEPTH)), final_norm=True)
    maps = make_in_maps(x, np.asarray(p), np.asarray(norm_w), np.asarray(w_in), np.asarray(fox_fb), np.asarray(hgrn_gn),
                        np.asarray(hgrn_lb_logits), np.asarray(w_out), np.asarray(w_ple), np.asarray(w_ple_gate),
                        np.asarray(final_norm_w), NCORES, nseq)
    res = run_bass_kernel_spmd(nc, maps, core_ids=list(range(NCORES)))
    return np.concatenate([r["out"] for r in res.results], axis=0).astype(np.float32)
```

```python
import numpy as np
import ml_dtypes
from contextlib import ExitStack
import concourse.bass as bass
import concourse.mybir as mybir
from concourse.bass_utils import run_bass_kernel_spmd

F32 = mybir.dt.float32
BF16 = mybir.dt.bfloat16
AF = mybir.ActivationFunctionType
ALU = mybir.AluOpType

D = 1024
S = 2048
DEPTH = 4
NCH = 8
TW = 512
NT = S // TW
NB = S // 128
DIN = 8208
PLE = 256
EPS = 1e-6
NCORES = 8


class _Rec:
    def __init__(self):
        self.call = None

    def __getattr__(self, name):
        def f(*a, **k):
            self.call = (name, a, k)
            return None
        return f


class Prog:
    ENG = ('pe', 'act', 'dve', 'pool', 'sp')

    def __init__(self, nc, n_dma_sems=40):
        self.nc = nc
        self.ops = []
        self.last_w = {}
        self.readers = {}
        self.n_dma_sems = n_dma_sems

    def add(self, eng, fn, reads=(), writes=(), dma=False):
        i = len(self.ops)
        deps = set()
        for r in reads:
            w = self.last_w.get(r)
            if w is not None:
                deps.add((w, 0))
        for wkey in writes:
            w = self.last_w.get(wkey)
            if w is not None:
                deps.add((w, 1))
            for rd in self.readers.get(wkey, {}).values():
                deps.add((rd, 2))
        for r in reads:
            self.readers.setdefault(r, {})[('d', i) if dma else eng] = i
        for wkey in writes:
            self.last_w[wkey] = i
            self.readers[wkey] = {}
        rec = _Rec()
        fn(rec)
        self.ops.append(dict(eng=eng, call=rec.call, deps=deps, dma=dma, mark=False))
        return i

    def emit(self):
        nc = self.nc
        ops = self.ops
        for i, op in enumerate(ops):
            need = set()
            for (p, kind) in op['deps']:
                po = ops[p]
                if (not po['dma']) and po['eng'] == op['eng']:
                    if po['eng'] == 'pe' and not op['dma']:
                        continue
                    if kind == 2 and not op['dma']:
                        continue
                need.add(p)
            op['need'] = sorted(need)
            for p in op['need']:
                ops[p]['mark'] = True
            op['deps'] = None
        cnt = {e: 0 for e in self.ENG}
        dcnt = [0] * self.n_dma_sems
        nd = 0
        for op in ops:
            if op['dma']:
                k = nd % self.n_dma_sems
                nd += 1
                op['sem'] = ('dma', k)
                op['prev'] = dcnt[k]
                dcnt[k] += 16
                op['val'] = dcnt[k]
            elif op['mark']:
                cnt[op['eng']] += 1
                op['sem'] = ('eng', op['eng'])
                op['val'] = cnt[op['eng']]
        self.cnt = cnt
        with ExitStack() as st:
            sems = {}
            for e in self.ENG:
                sems[('eng', e)] = st.enter_context(nc.semaphore('s_' + e))
            for k in range(self.n_dma_sems):
                sems[('dma', k)] = st.enter_context(nc.semaphore('d_%d' % k))
            block = st.enter_context(nc.Block())
            per = {e: [op for op in ops if op['eng'] == e] for e in self.ENG}

            def run(eng_name, eng):
                waited = {}
                for op in per[eng_name]:
                    ws = [(ops[p]['sem'], ops[p]['val']) for p in op['need']]
                    if op['dma'] and op['prev'] > 0:
                        ws.append((op['sem'], op['prev']))
                    for (s, v) in ws:
                        if waited.get(s, 0) >= v:
                            continue
                        waited[s] = v
                        eng.wait_ge(sems[s], v)
                    nm, a_, k_ = op['call']
                    ins = getattr(eng, nm)(*a_, **k_)
                    if op['dma']:
                        ins.then_inc(sems[op['sem']], 16)
                    elif op['mark']:
                        ins.then_inc(sems[op['sem']], 1)
                if eng_name == 'sp':
                    for k in range(self.n_dma_sems):
                        if dcnt[k] > 0:
                            eng.wait_ge(sems[('dma', k)], dcnt[k])

            @block.tensor
            def _(e):
                run('pe', e)

            @block.scalar
            def _(e):
                run('act', e)

            @block.vector
            def _(e):
                run('dve', e)

            @block.gpsimd
            def _(e):
                run('pool', e)

            @block.sync
            def _(e):
                run('sp', e)


def host_consts():
    c = {}
    i = np.arange(128)
    c['ident32'] = np.eye(128, dtype=np.float32)
    tri = (i[:, None] <= i[None, :]).astype(np.float32)
    c['tri32'] = tri
    c['ones32'] = np.ones((128, 128), np.float32)
    m63 = np.zeros((128, 128), np.float32)
    m63[:64, :] = 1.0
    c['m63'] = m63
    hm = tri * ((i[:, None] // 32) == (i[None, :] // 32))
    c['hmask'] = hm.astype(np.float32)
    rm = np.ones((128, TW), np.float32)
    rm[:, ::32] = 0.0
    c['rmask'] = rm
    return c


CONST_NAMES = ['ident32', 'tri32', 'ones32', 'm63', 'hmask', 'rmask']


def build(nseq, layers, final_norm=True, wq='pool', phases=('norm', 'prep', 'hgrn', 'wo1', 'fox', 'wo2', 'ple'), nheads=8, dbg=False):
    nc = bass.Bass("TRN2", target_bir_lowering=False)
    NL = DEPTH
    x_d = nc.dram_tensor("x", [nseq, S, D], F32, kind="ExternalInput").ap()
    p_d = nc.dram_tensor("p", [NL, nseq, S, PLE], F32, kind="ExternalInput").ap()
    win_d = nc.dram_tensor("w_in", [NL, D, DIN], F32, kind="ExternalInput").ap()
    wout_d = nc.dram_tensor("w_out", [NL, 2 * D, D], F32, kind="ExternalInput").ap()
    wple_d = nc.dram_tensor("w_ple", [NL, PLE, D], F32, kind="ExternalInput").ap()
    wpg_d = nc.dram_tensor("w_ple_gate", [NL, D, D], F32, kind="ExternalInput").ap()
    nw_d = nc.dram_tensor("v_nw", [128, NL * NCH], F32, kind="ExternalInput").ap()
    gn_d = nc.dram_tensor("v_gn", [128, NL * NCH], F32, kind="ExternalInput").ap()
    lbl_d = nc.dram_tensor("v_lbl", [128, NL * NCH], F32, kind="ExternalInput").ap()
    fnw_d = nc.dram_tensor("v_fnw", [128, NCH], F32, kind="ExternalInput").ap()
    fb_d = nc.dram_tensor("v_fb", [128, NL * 16], F32, kind="ExternalInput").ap()
    cd = {n: nc.dram_tensor("c_" + n, [128, TW if n == 'rmask' else 128], F32, kind="ExternalInput").ap() for n in CONST_NAMES}
    out_d = nc.dram_tensor("out", [nseq, S, D], F32, kind="ExternalOutput").ap()

    with ExitStack() as st:
        def sb(name, shape, dt):
            return st.enter_context(nc.sbuf_tensor(name, shape, dt))

        def pst(name, shape, dt):
            return st.enter_context(nc.psum_tensor(name, shape, dt))

        h = sb("h", [128, NCH, S], F32)
        uT = sb("uT", [128, NCH, S], BF16)
        yT = sb("yT", [128, NCH, S], BF16)
        NWB = 8
        wb = sb("wb", [128, NWB, NCH, 128], BF16)
        NF = 10
        ft = sb("ft", [128, NF, TW], F32)
        NBT = 10
        bt = sb("bt", [128, NBT, TW], BF16)
        kAB = sb("kAB", [128, 2, S], BF16)
        VB = sb("VB", [128, NB, 128], BF16)
        W32 = sb("W32", [128, 3, 128], F32)
        W16 = sb("W16", [128, 3, 128], BF16)
        Dall = sb("Dall", [128, 68], F32)
        sm = sb("sm", [128, 6, 256], F32)
        ident32 = sb("ident32", [128, 128], F32)
        identb = sb("identb", [128, 128], BF16)
        tri32 = sb("tri32", [128, 128], F32)
        trib = sb("trib", [128, 128], BF16)
        ones32 = sb("ones32", [128, 128], F32)
        m63 = sb("m63", [128, 128], F32)
        hmask = sb("hmask", [128, 128], F32)
        rmask = sb("rmask", [128, TW], F32)
        onesD = sb("onesD", [128, 128], BF16)
        onesV = sb("onesV", [128, 128], BF16)
        onesb = sb("onesb", [128, 128], BF16)
        nw = sb("nw", [128, NL * NCH], F32)
        gn = sb("gn", [128, NL * NCH], F32)
        lb = sb("lb", [128, NL * NCH], F32)
        oml = sb("oml", [128, NL * NCH], F32)
        lbe = sb("lbe", [128, NL * NCH], F32)
        lbs = sb("lbs", [128, NCH], F32)
        fnw = sb("fnw", [128, NCH], F32)
        fb = sb("fb", [128, NL * 16], F32)
        epsb = sb("epsb", [128, 1], F32)
        wz = sb("wz", [128, NCH, 16], BF16)
        wpl = sb("wpl", [128, 2, 2, 128], BF16)

        PS = [pst("ps%d" % k, [128, TW], F32) for k in range(7)]
        PSB = pst("psb", [128, 2 * TW], BF16)

        P = Prog(nc)
        A = P.add

        cmap = dict(ident32=ident32, tri32=tri32, ones32=ones32, m63=m63, hmask=hmask, rmask=rmask)
        for n in CONST_NAMES:
            A('sp', (lambda t, s_: (lambda e: e.dma_start(out=t[:], in_=s_[:, :])))(cmap[n], cd[n]), writes=[n], dma=True)
        for (t, s_, n) in ((nw, nw_d, 'nw'), (gn, gn_d, 'gn'), (lbe, lbl_d, 'lbe'), (fnw, fnw_d, 'fnw'), (fb, fb_d, 'fb')):
            A('sp', (lambda t, s_: (lambda e: e.dma_start(out=t[:], in_=s_[:, :])))(t, s_), writes=[n], dma=True)
        A('pool', lambda e: e.tensor_copy(out=identb[:], in_=ident32[:]), reads=['ident32'], writes=['identb'])
        A('pool', lambda e: e.tensor_copy(out=trib[:], in_=tri32[:]), reads=['tri32'], writes=['trib'])
        A('pool', lambda e: e.memset(onesD[:], 1.0 / D), writes=['onesD'])
        A('pool', lambda e: e.memset(onesV[:], 1.0 / 128), writes=['onesV'])
        A('pool', lambda e: e.memset(onesb[:], 1.0), writes=['onesb'])
        A('pool', lambda e: e.memset(epsb[:], EPS), writes=['epsb'])
        A('pool', lambda e: e.memset(kAB[:, 1, :], 0.0), writes=['kB'])
        A('pool', lambda e: e.memset(kAB[64:65, 0, :], 1.0), writes=['kA'])
        A('pool', lambda e: e.memset(kAB[0:1, 1, :], 1.0), reads=['kB'], writes=['kB'])
        A('pool', lambda e: e.memset(VB[:], 0.0), writes=['VB'])
        A('pool', lambda e: e.memset(VB[:, :, 0:1], 1.0), reads=['VB'], writes=['VB'])
        A('act', lambda e: e.activation(out=lbe[:], in_=lbe[:], func=AF.Exp), reads=['lbe'], writes=['lbe'])
        A('dve', lambda e: e.tensor_tensor(out=lbs[:], in0=lbe[:, 0:NCH], in1=lbe[:, NCH:2 * NCH], op=ALU.add), reads=['lbe'], writes=['lbs'])
        A('dve', lambda e: e.tensor_tensor(out=lbs[:], in0=lbs[:], in1=lbe[:, 2 * NCH:3 * NCH], op=ALU.add), reads=['lbe', 'lbs'], writes=['lbs'])
        A('dve', lambda e: e.tensor_tensor(out=lbs[:], in0=lbs[:], in1=lbe[:, 3 * NCH:4 * NCH], op=ALU.add), reads=['lbe', 'lbs'], writes=['lbs'])
        A('dve', lambda e: e.reciprocal(out=lbs[:], in_=lbs[:]), reads=['lbs'], writes=['lbs'])
        A('dve', lambda e: e.memset(lb[:, 0:NCH], 0.0), writes=['lb'])
        for l in range(1, NL):
            A('dve', (lambda l: (lambda e: e.tensor_tensor(out=lbe[:, l * NCH:(l + 1) * NCH], in0=lbe[:, l * NCH:(l + 1) * NCH], in1=lbs[:], op=ALU.mult)))(l),
              reads=['lbe', 'lbs'], writes=['lbe'])
            A('dve', (lambda l: (lambda e: e.tensor_tensor(out=lb[:, l * NCH:(l + 1) * NCH], in0=lb[:, (l - 1) * NCH:l * NCH], in1=lbe[:, l * NCH:(l + 1) * NCH], op=ALU.add)))(l),
              reads=['lbe', 'lb'], writes=['lb'])
        A('dve', lambda e: e.tensor_scalar(out=oml[:], in0=lb[:], scalar1=-1.0, scalar2=1.0, op0=ALU.mult, op1=ALU.add), reads=['lb'], writes=['oml'])

        wslot = [0]

        def wload(src_ap, nk=NCH, ncols=128):
            k = wslot[0] % NWB
            wslot[0] += 1
            dst = wb[:, k, 0:nk, 0:ncols]
            A(wq, lambda e: e.dma_start(out=dst, in_=src_ap), writes=[('wb', k)], dma=True)
            return k

        def win_chunk(l, col0, ncols=128):
            return wload(win_d[l].rearrange("(c p) e -> p c e", p=128)[:, :, col0:col0 + ncols], NCH, ncols)

        def proj_fm(k, t0, tw, ps_ap, psres, ncols=128):
            for c in range(NCH):
                A('pe', (lambda c: (lambda e: e.matmul(ps_ap, lhsT=wb[:, k, c, 0:ncols], rhs=uT[:, c, t0:t0 + tw], start=(c == 0), stop=(c == NCH - 1))))(c),
                  reads=[('wb', k), ('uT', c, t0 // TW)], writes=psres)

        def proj_tm(k, tb, ps_ap, psres, ncols=128):
            for c in range(NCH):
                A('pe', (lambda c: (lambda e: e.matmul(ps_ap, lhsT=uT[:, c, tb * 128:(tb + 1) * 128], rhs=wb[:, k, c, 0:ncols], start=(c == 0), stop=(c == NCH - 1))))(c),
                  reads=[('wb', k), ('uT', c, tb // 4)], writes=psres)

        def rmsnorm_to_u(l):
            for t in range(NT):
                ts = slice(t * TW, (t + 1) * TW)
                sq_list = []
                for c in range(NCH):
                    slot = c % 4
                    A('act', (lambda c, slot: (lambda e: e.activation(out=bt[:, slot, :], in_=h[:, c, ts], func=AF.Square)))(c, slot),
                      reads=[('h', c, t)], writes=[('bt', slot)])
                    A('pe', (lambda c, slot: (lambda e: e.matmul(PS[0][:], lhsT=onesD[:], rhs=bt[:, slot, :], start=(c == 0), stop=(c == NCH - 1))))(c, slot),
                      reads=['onesD', ('bt', slot)], writes=[('ps', 0)])
                A('act', lambda e: e.activation(out=ft[:, 0, :], in_=PS[0][:], func=AF.Sqrt, bias=epsb[:], scale=1.0),
                  reads=[('ps', 0), 'epsb'], writes=[('ft', 0)])
                A('dve', lambda e: e.reciprocal(out=ft[:, 0, :], in_=ft[:, 0, :]), reads=[('ft', 0)], writes=[('ft', 0)])
                for c in range(NCH):
                    A('dve', (lambda c: (lambda e: e.scalar_tensor_tensor(out=uT[:, c, ts], in0=h[:, c, ts], scalar=nw[:, l * NCH + c:l * NCH + c + 1],
                                                                         in1=ft[:, 0, :], op0=ALU.mult, op1=ALU.mult)))(c),
                      reads=[('h', c, t), ('ft', 0), 'nw'], writes=[('uT', c, t)])

        def load_x(s):
            for tb in range(NB):
                stg = ft[:, 8:10, :]
                A('sp', (lambda tb: (lambda e: e.dma_start(out=stg, in_=x_d[s, tb * 128:(tb + 1) * 128, :].rearrange("p (a b) -> p a b", a=2))))(tb),
                  writes=[('ft', 8), ('ft', 9)], dma=True)
                for half in range(2):
                    pk = 1 + half
                    for cc in range(4):
                        c = half * 4 + cc
                        A('pe', (lambda c, cc, pk, half: (lambda e: e.transpose(out=PS[pk][:, cc * 128:(cc + 1) * 128], in_=ft[:, 8 + half, cc * 128:(cc + 1) * 128], identity=ident32[:])))(c, cc, pk, half),
                          reads=[('ft', 8 + half), 'ident32'], writes=[('ps', pk)])
                    eng = 'act' if half == 0 else 'dve'
                    if eng == 'act':
                        A('act', (lambda pk, half, tb: (lambda e: e.activation(out=h[:, half * 4:half * 4 + 4, tb * 128:(tb + 1) * 128],
                                                                              in_=PS[pk][:].rearrange("p (a b) -> p a b", a=4), func=AF.Copy)))(pk, half, tb),
                          reads=[('ps', pk)], writes=[('h', half * 4 + cc, tb // 4) for cc in range(4)])
                    else:
                        A('dve', (lambda pk, half, tb: (lambda e: e.tensor_copy(out=h[:, half * 4:half * 4 + 4, tb * 128:(tb + 1) * 128],
                                                                               in_=PS[pk][:].rearrange("p (a b) -> p a b", a=4))))(pk, half, tb),
                          reads=[('ps', pk)], writes=[('h', half * 4 + cc, tb // 4) for cc in range(4)])

        def store_out(s, normed):
            for t in range(NT):
                ts = slice(t * TW, (t + 1) * TW)
                if normed:
                    for c in range(NCH):
                        slot = c % 4
                        A('act', (lambda c, slot: (lambda e: e.activation(out=bt[:, slot, :], in_=h[:, c, ts], func=AF.Square)))(c, slot),
                          reads=[('h', c, t)], writes=[('bt', slot)])
                        A('pe', (lambda c, slot: (lambda e: e.matmul(PS[0][:], lhsT=onesD[:], rhs=bt[:, slot, :], start=(c == 0), stop=(c == NCH - 1))))(c, slot),
                          reads=['onesD', ('bt', slot)], writes=[('ps', 0)])
                    A('act', lambda e: e.activation(out=ft[:, 0, :], in_=PS[0][:], func=AF.Sqrt, bias=epsb[:], scale=1.0),
                      reads=[('ps', 0), 'epsb'], writes=[('ft', 0)])
                    A('dve', lambda e: e.reciprocal(out=ft[:, 0, :], in_=ft[:, 0, :]), reads=[('ft', 0)], writes=[('ft', 0)])
                    for c in range(NCH):
                        A('dve', (lambda c: (lambda e: e.scalar_tensor_tensor(out=h[:, c, ts], in0=h[:, c, ts], scalar=fnw[:, c:c + 1],
                                                                             in1=ft[:, 0, :], op0=ALU.mult, op1=ALU.mult)))(c),
                          reads=[('h', c, t), ('ft', 0), 'fnw'], writes=[('h', c, t)])
                for q in range(4):
                    tb = t * 4 + q
                    for half in range(2):
                        pk = 1 + half
                        for cc in range(4):
                            c = half * 4 + cc
                            A('pe', (lambda c, cc, pk, tb: (lambda e: e.transpose(out=PS[pk][:, cc * 128:(cc + 1) * 128], in_=h[:, c, tb * 128:(tb + 1) * 128], identity=ident32[:])))(c, cc, pk, tb),
                              reads=[('h', c, t), 'ident32'], writes=[('ps', pk)])
                        if half == 0:
                            A('act', (lambda pk, half: (lambda e: e.activation(out=ft[:, 8 + half, :], in_=PS[pk][:], func=AF.Copy)))(pk, half),
                              reads=[('ps', pk)], writes=[('ft', 8 + half)])
                        else:
                            A('dve', (lambda pk, half: (lambda e: e.tensor_copy(out=ft[:, 8 + half, :], in_=PS[pk][:])))(pk, half),
                              reads=[('ps', pk)], writes=[('ft', 8 + half)])
                    A('sp', (lambda tb: (lambda e: e.dma_start(out=out_d[s, tb * 128:(tb + 1) * 128, :].rearrange("p (a b) -> p a b", a=2), in_=ft[:, 8:10, :])))(tb),
                      reads=[('ft', 8), ('ft', 9)], dma=True)

        def hgrn_head(l, hh):
            kq = win_chunk(l, 0 * D + hh * 128)
            kf = win_chunk(l, 1 * D + hh * 128)
            ki = win_chunk(l, 2 * D + hh * 128)
            kg = win_chunk(l, 3 * D + hh * 128)
            lc = l * NCH + hh
            lb_ap = lb[:, lc:lc + 1]
            oml_ap = oml[:, lc:lc + 1]
            gn_ap = gn[:, lc:lc + 1]
            A('pool', lambda e: e.memset(W32[:, 2, :], 0.0), writes=[('W32', 2)])
            A('pool', lambda e: e.memset(W16[:, 2, :], 0.0), writes=[('W16', 2)])
            A('pool', lambda e: e.memset(Dall[:, 0:1], 1.0), writes=['Dall'])
            for sg in range(NT):
                t0 = sg * TW
                proj_fm(kf, t0, TW, PS[0][:], [('ps', 0)])
                A('act', lambda e: e.activation(out=ft[:, 1, :], in_=PS[0][:], func=AF.Sigmoid), reads=[('ps', 0)], writes=[('ft', 1)])
                A('act', lambda e: e.activation(out=ft[:, 2, :], in_=PS[0][:], func=AF.Sigmoid, scale=-1.0), reads=[('ps', 0)], writes=[('ft', 2)])
                A('dve', lambda e: e.tensor_scalar(out=ft[:, 1, :], in0=ft[:, 1, :], scalar1=oml_ap, scalar2=lb_ap, op0=ALU.mult, op1=ALU.add),
                  reads=[('ft', 1), 'oml', 'lb'], writes=[('ft', 1)])
                A('act', lambda e: e.activation(out=ft[:, 3, :], in_=ft[:, 1, :], func=AF.Ln), reads=[('ft', 1)], writes=[('ft', 3)])
                A('dve', lambda e: e.tensor_tensor_scan(out=ft[:, 4, :], data0=rmask[:], data1=ft[:, 3, :], initial=0.0, op0=ALU.mult, op1=ALU.add),
                  reads=[('ft', 3), 'rmask'], writes=[('ft', 4)])
                A('act', lambda e: e.activation(out=ft[:, 5, :], in_=ft[:, 4, :], func=AF.Exp), reads=[('ft', 4)], writes=[('ft', 5)])
                A('act', lambda e: e.activation(out=ft[:, 6, :], in_=ft[:, 4, :], func=AF.Exp, scale=-1.0), reads=[('ft', 4)], writes=[('ft', 6)])
                A('dve', lambda e: e.scalar_tensor_tensor(out=bt[:, 4, :], in0=ft[:, 2, :], scalar=oml_ap, in1=ft[:, 6, :], op0=ALU.mult, op1=ALU.mult),
                  reads=[('ft', 2), ('ft', 6), 'oml'], writes=[('bt', 4)])
                A('pool', (lambda sg: (lambda e: e.tensor_copy(out=Dall[:, 1 + 16 * sg:1 + 16 * sg + 16], in_=ft[:, 5, 31::32])))(sg),
                  reads=[('ft', 5), 'Dall'], writes=['Dall'])
                proj_fm(kq, t0, TW, PS[1][:], [('ps', 1)])
                A('dve', lambda e: e.scalar_tensor_tensor(out=bt[:, 5, :], in0=PS[1][:], scalar=float(128 ** -0.5), in1=ft[:, 5, :], op0=ALU.mult, op1=ALU.mult),
                  reads=[('ps', 1), ('ft', 5)], writes=[('bt', 5)])
                A('pool', (lambda sg: (lambda e: e.tensor_tensor(out=bt[:, 6, :].rearrange("p (a b) -> p a b", b=32), in0=bt[:, 5, :].rearrange("p (a b) -> p a b", b=32),
                                                                 in1=bcast_last(Dall[:, 16 * sg:16 * sg + 16], 32), op=ALU.mult)))(sg),
                  reads=[('bt', 5), 'Dall'], writes=[('bt', 6)])
                proj_fm(kg, t0, TW, PS[1][:], [('ps', 1)])
                A('act', lambda e: e.activation(out=ft[:, 7, :], in_=PS[1][:], func=AF.Silu), reads=[('ps', 1)], writes=[('ft', 7)])
                for q in range(4):
                    proj_tm(ki, sg * 4 + q, PS[3][:, q * 128:(q + 1) * 128], [('ps', 3)])
                A('act', lambda e: e.activation(out=bt[:, 9, :], in_=PS[3][:], func=AF.Copy), reads=[('ps', 3)], writes=[('bt', 9)])
                for q in range(4):
                    A('pe', (lambda q: (lambda e: e.transpose(out=PSB[:, q * 128:(q + 1) * 128], in_=bt[:, 4, q * 128:(q + 1) * 128], identity=identb[:])))(q),
                      reads=[('bt', 4), 'identb'], writes=['psb'])
                A('dve', lambda e: e.tensor_copy(out=bt[:, 8, :], in_=PSB[:, 0:TW]), reads=['psb'], writes=[('bt', 8)])
                def emit_U(idx):
                    q, cc = divmod(idx, 4)
                    ub = 6 if idx % 2 == 0 else 2
                    rows = slice(cc * 32, cc * 32 + 32)
                    A('pe', lambda e: e.matmul(PS[ub][:, 0:128], lhsT=bt[rows, 8, q * 128:(q + 1) * 128], rhs=bt[rows, 9, q * 128:(q + 1) * 128],
                                               start=True, stop=True, tile_position=(cc * 32, 0)),
                      reads=[('bt', 8), ('bt', 9)], writes=psr(ub))
                for q in range(4):
                    A('pe', (lambda q: (lambda e: e.matmul(PS[4][:, q * 128:(q + 1) * 128], lhsT=bt[:, 4, q * 128:(q + 1) * 128], rhs=bt[:, 5, q * 128:(q + 1) * 128], start=True, stop=True)))(q),
                      reads=[('bt', 4), ('bt', 5)], writes=[('ps', 4)])
                A('dve', lambda e: e.tensor_tensor(out=bt[:, 3, :].rearrange("p (a b) -> p a b", a=4), in0=PS[4][:].rearrange("p (a b) -> p a b", a=4),
                                                   in1=bcast_mid(hmask[:], 4), op=ALU.mult),
                  reads=[('ps', 4), 'hmask'], writes=[('bt', 3)])
                emit_U(0)
                for idx in range(16):
                    q, cc = divmod(idx, 4)
                    cg = sg * 16 + idx
                    prev = (cg + 2) % 3
                    cur = cg % 3
                    ub = 6 if idx % 2 == 0 else 2
                    cs = slice(q * 128 + cc * 32, q * 128 + cc * 32 + 32)
                    if cc == 0:
                        A('pe', lambda e: e.matmul(PS[5][:, q * 128:(q + 1) * 128], lhsT=bt[:, 9, q * 128:(q + 1) * 128], rhs=bt[:, 3, q * 128:(q + 1) * 128], start=True, stop=False),
                          reads=[('bt', 9), ('bt', 3)], writes=[('ps', 5)])
                    if idx + 1 < 16:
                        emit_U(idx + 1)
                    A('pe', lambda e: e.matmul(PS[5][:, cs], lhsT=W16[:, prev, :], rhs=bt[:, 6, cs], start=False, stop=(cc == 3)),
                      reads=[('W16', prev), ('bt', 6)], writes=[('ps', 5)])
                    A('dve', lambda e: e.scalar_tensor_tensor(out=W32[:, cur, :], in0=W32[:, prev, :], scalar=Dall[:, cg:cg + 1], in1=PS[ub][:, 0:128], op0=ALU.mult, op1=ALU.add),
                      reads=[('W32', prev), 'Dall'] + psr(ub), writes=[('W32', cur)])
                    A('pool', lambda e: e.tensor_copy(out=W16[:, cur, :], in_=W32[:, cur, :]), reads=[('W32', cur)], writes=[('W16', cur)])
                A('act', lambda e: e.activation(out=bt[:, 7, :], in_=PS[5][:], func=AF.Square), reads=[('ps', 5)], writes=[('bt', 7)])
                A('pe', lambda e: e.matmul(PS[0][:], lhsT=onesV[:], rhs=bt[:, 7, :], start=True, stop=True), reads=['onesV', ('bt', 7)], writes=[('ps', 0)])
                A('act', lambda e: e.activation(out=ft[:, 0, :], in_=PS[0][:], func=AF.Sqrt, bias=epsb[:], scale=1.0), reads=[('ps', 0), 'epsb'], writes=[('ft', 0)])
                A('dve', lambda e: e.reciprocal(out=ft[:, 0, :], in_=ft[:, 0, :]), reads=[('ft', 0)], writes=[('ft', 0)])
                A('dve', lambda e: e.scalar_tensor_tensor(out=ft[:, 0, :], in0=PS[5][:], scalar=gn_ap, in1=ft[:, 0, :], op0=ALU.mult, op1=ALU.mult),
                  reads=[('ps', 5), ('ft', 0), 'gn'], writes=[('ft', 0)])
                A('pool', (lambda t0, sg: (lambda e: e.tensor_tensor(out=yT[:, hh, t0:t0 + TW], in0=ft[:, 0, :], in1=ft[:, 7, :], op=ALU.mult)))(t0, sg),
                  reads=[('ft', 0), ('ft', 7)], writes=[('yT', hh, sg)])

        def bcast_last(ap2, n):
            return ap2.unsqueeze(2).to_broadcast([ap2.shape[0], ap2.shape[1], n])

        def bcast_mid(ap2, n):
            return ap2.unsqueeze(1).to_broadcast([ap2.shape[0], n, ap2.shape[1]])

        PS6 = [('psu', i) for i in range(4)]

        def psr(k):
            return PS6 if k == 6 else [('ps', k)]

        def wout_pass(l, half, swap):
            for dm in range(NCH):
                src = wout_d[l, half * D:(half + 1) * D, dm * 128:(dm + 1) * 128].rearrange("(c p) e -> p c e", p=128)
                if not swap:
                    k = wload(src)
                else:
                    k = wload(src)
                for t in range(NT):
                    pk = 1 + (t % 2)
                    for c in range(NCH):
                        A('pe', (lambda c, t, pk: (lambda e: e.matmul(PS[pk][:], lhsT=wb[:, k, c, :], rhs=yT[:, c, t * TW:(t + 1) * TW], start=(c == 0), stop=(c == NCH - 1))))(c, t, pk),
                          reads=[('wb', k), ('yT', c, t)], writes=[('ps', pk)])
                    A('dve', (lambda t, pk, dm: (lambda e: e.tensor_tensor(out=h[:, dm, t * TW:(t + 1) * TW], in0=h[:, dm, t * TW:(t + 1) * TW], in1=PS[pk][:], op=ALU.add)))(t, pk, dm),
                      reads=[('ps', pk), ('h', dm, t)], writes=[('h', dm, t)])

        def fox_prep(l):
            A(wq, lambda e: e.dma_start(out=wz[:], in_=win_d[l].rearrange("(c p) e -> p c e", p=128)[:, :, 8 * D:8 * D + 16]), writes=['wz'], dma=True)
            for tb in range(NB):
                for c in range(NCH):
                    A('pe', (lambda c, tb: (lambda e: e.matmul(PS[0][:, tb * 16:(tb + 1) * 16], lhsT=uT[:, c, tb * 128:(tb + 1) * 128], rhs=wz[:, c, :], start=(c == 0), stop=(c == NCH - 1))))(c, tb),
                      reads=['wz', ('uT', c, tb // 4)], writes=[('ps', 0)])
            A('dve', lambda e: e.tensor_tensor(out=sm[:, 0, :].rearrange("p (a b) -> p a b", b=16), in0=PS[0][:, 0:256].rearrange("p (a b) -> p a b", b=16),
                                               in1=bcast_mid(fb[:, l * 16:(l + 1) * 16], 16), op=ALU.add),
              reads=[('ps', 0), 'fb'], writes=[('sm', 0)])
            A('act', lambda e: e.activation(out=sm[:, 0, :], in_=sm[:, 0, :], func=AF.Sigmoid), reads=[('sm', 0)], writes=[('sm', 0)])
            A('act', lambda e: e.activation(out=sm[:, 0, :], in_=sm[:, 0, :], func=AF.Ln), reads=[('sm', 0)], writes=[('sm', 0)])
            A('pe', lambda e: e.matmul(PS[1][:, 0:256], lhsT=tri32[:], rhs=sm[:, 0, :], start=True, stop=True), reads=['tri32', ('sm', 0)], writes=[('ps', 1)])
            A('pe', lambda e: e.matmul(PS[2][:, 0:256], lhsT=ones32[:], rhs=sm[:, 0, :], start=True, stop=True), reads=['ones32', ('sm', 0)], writes=[('ps', 2)])
            A('pe', lambda e: e.matmul(PS[3][:, 0:256], lhsT=m63[:], rhs=sm[:, 0, :], start=True, stop=True), reads=['m63', ('sm', 0)], writes=[('ps', 3)])
            A('act', lambda e: e.activation(out=sm[:, 4, :], in_=PS[2][:, 0:256], func=AF.Copy), reads=[('ps', 2)], writes=[('sm', 4)])
            A('dve', lambda e: e.memset(sm[:, 1, 0:16], 0.0), writes=[('sm', 1)])
            for j in range(1, NB):
                A('dve', (lambda j: (lambda e: e.tensor_tensor(out=sm[:, 1, j * 16:(j + 1) * 16], in0=sm[:, 1, (j - 1) * 16:j * 16], in1=sm[:, 4, (j - 1) * 16:j * 16], op=ALU.add)))(j),
                  reads=[('sm', 1), ('sm', 4)], writes=[('sm', 1)])
            A('dve', lambda e: e.scalar_tensor_tensor(out=sm[:, 2, :], in0=PS[1][:, 0:256], scalar=-1.0, in1=sm[:, 1, :], op0=ALU.mult, op1=ALU.subtract),
              reads=[('ps', 1), ('sm', 1)], writes=[('sm', 2)])
            A('dve', lambda e: e.tensor_tensor(out=sm[:, 3, :], in0=PS[3][:, 0:256], in1=sm[:, 1, :], op=ALU.add), reads=[('ps', 3), ('sm', 1)], writes=[('sm', 3)])
            A('dve', lambda e: e.tensor_scalar(out=sm[:, 3, :], in0=sm[:, 3, :], scalar1=8.0, scalar2=None, op0=ALU.mult), reads=[('sm', 3)], writes=[('sm', 3)])

        def fox_pair(l, pp):
            base = 4 * D
            kfq = win_chunk(l, base + 0 * D + pp * 128)
            kfk = win_chunk(l, base + 1 * D + pp * 128)
            kfv = win_chunk(l, base + 2 * D + pp * 128)
            kfg = win_chunk(l, base + 3 * D + pp * 128)
            hA, hB = 2 * pp, 2 * pp + 1
            if pp == 0:
                for qb_ in (1, 3):
                    A('pool', (lambda qb_: (lambda e: e.memset(bt[:, qb_, :], 0.0)))(qb_), writes=[('bt', qb_)])
            for t in range(NT):
                ts = slice(t * TW, (t + 1) * TW)
                pk = t % 2
                proj_fm(kfk, t * TW, TW, PS[pk][:], [('ps', pk)])
                if t % 2 == 0:
                    A('act', (lambda pk, ts: (lambda e: e.activation(out=kAB[:, 0, ts], in_=PS[pk][:], func=AF.Copy)))(pk, ts), reads=[('ps', pk)], writes=['kA'])
                else:
                    A('dve', (lambda pk, ts: (lambda e: e.tensor_copy(out=kAB[:, 0, ts], in_=PS[pk][:])))(pk, ts), reads=[('ps', pk)], writes=['kA'])
                A('pool', lambda e: e.tensor_copy(out=kAB[64:128, 1, ts], in_=kAB[64:128, 0, ts]), reads=['kA'], writes=['kB'])
                A('pool', lambda e: e.memset(kAB[64:65, 0, ts], 1.0), reads=['kA'], writes=['kA'])
            for g in range(4):
                pk = g % 2
                for q in range(4):
                    proj_tm(kfv, g * 4 + q, PS[pk][:, q * 128:(q + 1) * 128], [('ps', pk)])
                if g % 2 == 0:
                    A('act', (lambda pk, g: (lambda e: e.activation(out=VB[:, g * 4:(g + 1) * 4, :], in_=PS[pk][:].rearrange("p (a b) -> p a b", a=4), func=AF.Copy)))(pk, g),
                      reads=[('ps', pk)], writes=['VB'])
                else:
                    A('dve', (lambda pk, g: (lambda e: e.tensor_copy(out=VB[:, g * 4:(g + 1) * 4, :], in_=PS[pk][:].rearrange("p (a b) -> p a b", a=4))))(pk, g),
                      reads=[('ps', pk)], writes=['VB'])
            FST = 9
            for qi in range(NT if FST >= 2 else 0):
                ts = slice(qi * TW, (qi + 1) * TW)
                qa, qb = 0 + (qi % 2) * 2, 1 + (qi % 2) * 2
                pk = 2
                proj_fm(kfq, qi * TW, TW, PS[pk][:], [('ps', pk)])
                A('act', (lambda pk, qa: (lambda e: e.activation(out=bt[:, qa, :], in_=PS[pk][:], func=AF.Copy)))(pk, qa), reads=[('ps', pk)], writes=[('bt', qa)])
                A('pool', lambda e: e.tensor_copy(out=bt[64:128, qb, :], in_=bt[64:128, qa, :]), reads=[('bt', qa)], writes=[('bt', qb)])
                A('pool', lambda e: e.tensor_copy(out=bt[64:65, qa, :].rearrange("p (a b) -> p a b", a=4),
                                                  in_=bcast_last(sm[64:65, 3, :].rearrange("p (a b) -> p a b", b=16)[:, 4 * qi:4 * qi + 4, hA], 128)),
                  reads=[('sm', 3), ('bt', qa)], writes=[('bt', qa)])
                A('pool', lambda e: e.tensor_copy(out=bt[0:1, qb, :].rearrange("p (a b) -> p a b", a=4),
                                                  in_=bcast_last(sm[0:1, 3, :].rearrange("p (a b) -> p a b", b=16)[:, 4 * qi:4 * qi + 4, hB], 128)),
                  reads=[('sm', 3), ('bt', qb)], writes=[('bt', qb)])
                pg = 2
                proj_fm(kfg, qi * TW, TW, PS[pg][:], [('ps', pg)])
                A('act', lambda e: e.activation(out=ft[:, 7, :], in_=PS[pg][:], func=AF.Silu), reads=[('ps', pg)], writes=[('ft', 7)])
                for hd in range(2 if FST >= 3 else 0):
                    hidx = hA if hd == 0 else hB
                    qs = qa if hd == 0 else qb
                    rows = slice(0, 65) if hd == 0 else slice(0, 128)
                    po = 5 + hd
                    nkb = 4 * qi + 4
                    def emit_S(j):
                        c0 = max(j - 4 * qi, 0) * 128
                        psk = 3 + (j % 2)
                        A('pe', lambda e: e.matmul(PS[psk][:, c0:TW], lhsT=kAB[rows, hd, j * 128:(j + 1) * 128], rhs=bt[rows, qs, c0:TW], start=True, stop=True),
                          reads=['kA' if hd == 0 else 'kB', ('bt', qs)], writes=[('ps', psk)])
                    LS = 1
                    if LS:
                        emit_S(0)
                    for j in range(nkb):
                        r = j - 4 * qi
                        c0 = max(r, 0) * 128
                        psk = 3 + (j % 2)
                        ptk = 4 + (j % 4)
                        if LS and j + 1 < nkb:
                            emit_S(j + 1)
                        if not LS:
                            emit_S(j)
                        A('act', lambda e: e.activation(out=bt[:, ptk, c0:TW], in_=PS[psk][:, c0:TW], func=AF.Exp, bias=sm[:, 2, j * 16 + hidx:j * 16 + hidx + 1], scale=0.125),
                          reads=[('ps', psk), ('sm', 2)], writes=[('bt', ptk)])
                        if r >= 0:
                            A('pool', lambda e: e.tensor_tensor(out=bt[:, ptk, c0:c0 + 128], in0=bt[:, ptk, c0:c0 + 128], in1=trib[:], op=ALU.mult),
                              reads=[('bt', ptk), 'trib'], writes=[('bt', ptk)])
                        A('pe', lambda e: e.matmul(PS[hd][:, c0:TW], lhsT=onesb[:], rhs=bt[:, ptk, c0:TW], start=(j == 0), stop=(j == nkb - 1)),
                          reads=['onesb', ('bt', ptk)], writes=[('ps', hd)])
                        if hd == 0:
                            A('pe', lambda e: e.matmul(PS[po][0:64, c0:TW], lhsT=VB[:, j, 0:64], rhs=bt[:, ptk, c0:TW], start=(j == 0), stop=(j == nkb - 1)),
                              reads=['VB', ('bt', ptk)], writes=psr(po))
                        else:
                            A('pe', lambda e: e.matmul(PS[po][:, c0:TW], lhsT=VB[:, j, :], rhs=bt[:, ptk, c0:TW], start=(j == 0), stop=(j == nkb - 1)),
                              reads=['VB', ('bt', ptk)], writes=psr(po))
                    orow = slice(0, 64) if hd == 0 else slice(64, 128)
                    A('dve', (lambda orow, hd: (lambda e: e.reciprocal(out=ft[orow, 1, :], in_=PS[hd][orow, :])))(orow, hd), reads=[('ps', hd)], writes=[('ft', 1)])
                    A('dve', (lambda orow: (lambda e: e.tensor_tensor(out=ft[orow, 2, :], in0=ft[orow, 1, :], in1=ft[orow, 7, :], op=ALU.mult)))(orow),
                      reads=[('ft', 1), ('ft', 7)], writes=[('ft', 2)])
                    A('dve', (lambda orow, po, qi: (lambda e: e.tensor_tensor(out=yT[orow, pp, qi * TW:(qi + 1) * TW], in0=PS[po][orow, :], in1=ft[orow, 2, :], op=ALU.mult)))(orow, po, qi),
                      reads=psr(po) + [('ft', 2)], writes=[('yT', pp, qi)])

        def ple_phase(l, s):
            for c in range(NCH):
                for t in range(NT):
                    eng = 'pool' if (c + t) % 2 == 0 else 'act'
                    if eng == 'pool':
                        A('pool', (lambda c, t: (lambda e: e.tensor_copy(out=uT[:, c, t * TW:(t + 1) * TW], in_=h[:, c, t * TW:(t + 1) * TW])))(c, t),
                          reads=[('h', c, t)], writes=[('uT', c, t)])
                    else:
                        A('act', (lambda c, t: (lambda e: e.activation(out=uT[:, c, t * TW:(t + 1) * TW], in_=h[:, c, t * TW:(t + 1) * TW], func=AF.Copy)))(c, t),
                          reads=[('h', c, t)], writes=[('uT', c, t)])
            for t in range(NT):
                stg = ft[:, 8:10, :]
                A('sp', (lambda t: (lambda e: e.dma_start(out=stg.rearrange("p a (q k) -> p (a q) k", k=PLE), in_=p_d[l, s, t * TW:(t + 1) * TW, :].rearrange("(q p) k -> p q k", p=128))))(t),
                  writes=[('ft', 8), ('ft', 9)], dma=True)
                for kc in range(2):
                    pk = 1 + kc
                    for q in range(4):
                        a, qq = divmod(q, 2)
                        A('pe', (lambda kc, q, a, qq, pk: (lambda e: e.transpose(out=PS[pk][:, q * 128:(q + 1) * 128], in_=ft[:, 8 + a, qq * PLE + kc * 128:qq * PLE + kc * 128 + 128], identity=ident32[:])))(kc, q, a, qq, pk),
                          reads=[('ft', 8 + a), 'ident32'], writes=[('ps', pk)])
                    if kc == 0:
                        A('act', (lambda pk, t: (lambda e: e.activation(out=kAB[:, 0, t * TW:(t + 1) * TW], in_=PS[pk][:], func=AF.Copy)))(pk, t), reads=[('ps', pk)], writes=['kA'])
                    else:
                        A('dve', (lambda pk, t: (lambda e: e.tensor_copy(out=kAB[:, 1, t * TW:(t + 1) * TW], in_=PS[pk][:])))(pk, t), reads=[('ps', pk)], writes=['kB'])
            for dm in range(NCH):
                kg = wload(wpg_d[l, :, dm * 128:(dm + 1) * 128].rearrange("(c p) e -> p c e", p=128))
                kp = wload(wple_d[l, :, dm * 128:(dm + 1) * 128].rearrange("(c p) e -> p c e", p=128), nk=2)
                for t in range(NT):
                    ts = slice(t * TW, (t + 1) * TW)
                    pa, pb = 3 + (t % 2), 5 + (t % 2)
                    for c in range(NCH):
                        A('pe', (lambda c, pa, ts, t: (lambda e: e.matmul(PS[pa][:], lhsT=wb[:, kg, c, :], rhs=uT[:, c, ts], start=(c == 0), stop=(c == NCH - 1))))(c, pa, ts, t),
                          reads=[('wb', kg), ('uT', c, t)], writes=[('ps', pa)])
                    for kc in range(2):
                        A('pe', (lambda kc, pb, ts: (lambda e: e.matmul(PS[pb][:], lhsT=wb[:, kp, kc, :], rhs=kAB[:, kc, ts], start=(kc == 0), stop=(kc == 1))))(kc, pb, ts),
                          reads=[('wb', kp), 'kA', 'kB'], writes=psr(pb))
                    f1 = 1 + (t % 2)
                    A('act', (lambda pa, f1: (lambda e: e.activation(out=ft[:, f1, :], in_=PS[pa][:], func=AF.Sigmoid)))(pa, f1), reads=[('ps', pa)], writes=[('ft', f1)])
                    A('dve', (lambda pb, f1: (lambda e: e.tensor_tensor(out=ft[:, f1, :], in0=PS[pb][:], in1=ft[:, f1, :], op=ALU.mult)))(pb, f1), reads=psr(pb) + [('ft', f1)], writes=[('ft', f1)])
                    A('pool', (lambda dm, ts, f1, t: (lambda e: e.tensor_tensor(out=h[:, dm, ts], in0=h[:, dm, ts], in1=ft[:, f1, :], op=ALU.add)))(dm, ts, f1, t),
                      reads=[('h', dm, t), ('ft', f1)], writes=[('h', dm, t)])

        def fox_consts():
            A('pool', lambda e: e.memset(kAB[:, 1, :], 0.0), reads=['kB'], writes=['kB'])
            A('pool', lambda e: e.memset(kAB[0:1, 1, :], 1.0), reads=['kB'], writes=['kB'])

        for s in range(nseq):
            load_x(s)
            for l in layers:
                if 'norm' in phases:
                    rmsnorm_to_u(l)
                if 'prep' in phases:
                    fox_prep(l)
                if 'hgrn' in phases:
                    for hh in range(nheads):
                        hgrn_head(l, hh)
                if 'wo1' in phases:
                    wout_pass(l, 0, False)
                if 'fox' in phases:
                    fox_consts()
                    for pp in range(nheads):
                        fox_pair(l, pp)
                if 'wo2' in phases:
                    wout_pass(l, 1, False)
                if 'ple' in phases:
                    ple_phase(l, s)
            store_out(s, final_norm)
        if dbg:
            dft = nc.dram_tensor("dbg_ft", [128, NF, TW], F32, kind="ExternalOutput").ap()
            dbt = nc.dram_tensor("dbg_bt", [128, NBT, TW], BF16, kind="ExternalOutput").ap()
            dy = nc.dram_tensor("dbg_y", [128, NCH, S], BF16, kind="ExternalOutput").ap()
            du = nc.dram_tensor("dbg_u", [128, NCH, S], BF16, kind="ExternalOutput").ap()
            dD = nc.dram_tensor("dbg_D", [128, 68], F32, kind="ExternalOutput").ap()
            dW = nc.dram_tensor("dbg_W", [128, 3, 128], F32, kind="ExternalOutput").ap()
            dsm = nc.dram_tensor("dbg_sm", [128, 6, 256], F32, kind="ExternalOutput").ap()
            A('sp', lambda e: e.dma_start(out=dft[:, :, :], in_=ft[:]), reads=[('ft', i) for i in range(NF)], dma=True)
            A('sp', lambda e: e.dma_start(out=dbt[:, :, :], in_=bt[:]), reads=[('bt', i) for i in range(NBT)], dma=True)
            A('sp', lambda e: e.dma_start(out=dy[:, :, :], in_=yT[:]), reads=[('yT', c, t) for c in range(NCH) for t in range(NT)], dma=True)
            A('sp', lambda e: e.dma_start(out=du[:, :, :], in_=uT[:]), reads=[('uT', c, t) for c in range(NCH) for t in range(NT)], dma=True)
            A('sp', lambda e: e.dma_start(out=dD[:, :], in_=Dall[:]), reads=['Dall'], dma=True)
            A('sp', lambda e: e.dma_start(out=dW[:, :, :], in_=W32[:]), reads=[('W32', i) for i in range(3)], dma=True)
            A('sp', lambda e: e.dma_start(out=dsm[:, :, :], in_=sm[:]), reads=[('sm', i) for i in range(6)], dma=True)
        P.emit()
        nc._prog_stats = dict(nops=len(P.ops), cnt=P.cnt)
    return nc


def _vec_layout(v):
    L = v.shape[0]
    return np.ascontiguousarray(v.reshape(L, NCH, 128).transpose(2, 0, 1).reshape(128, L * NCH)).astype(np.float32)


def make_in_maps(x, p, norm_w, w_in, fox_fb, hgrn_gn, hgrn_lb_logits, w_out, w_ple, w_ple_gate, final_norm_w, ncores, nseq):
    cst = host_consts()
    common = {
        "w_in": np.ascontiguousarray(w_in, dtype=np.float32),
        "w_out": np.ascontiguousarray(w_out, dtype=np.float32),
        "w_ple": np.ascontiguousarray(w_ple, dtype=np.float32),
        "w_ple_gate": np.ascontiguousarray(w_ple_gate, dtype=np.float32),
        "v_nw": _vec_layout(norm_w),
        "v_gn": _vec_layout(hgrn_gn),
        "v_lbl": _vec_layout(hgrn_lb_logits),
        "v_fnw": _vec_layout(final_norm_w[None, :]),
        "v_fb": np.ascontiguousarray(np.broadcast_to(np.asarray(fox_fb, np.float32).reshape(1, -1), (128, fox_fb.size))),
    }
    common.update({'c_' + k: v for k, v in cst.items()})
    maps = []
    for c in range(ncores):
        m = dict(common)
        m["x"] = np.ascontiguousarray(x[c * nseq:(c + 1) * nseq], dtype=np.float32)
        m["p"] = np.ascontiguousarray(p[:, c * nseq:(c + 1) * nseq], dtype=np.float32)
        maps.append(m)
    return maps


def kernel(x, p, norm_w, w_in, fox_fb, hgrn_gn, hgrn_lb_logits, w_out, w_ple, w_ple_gate, final_norm_w):
    x = np.asarray(x)
    B = x.shape[0]
    nseq = B // NCORES
    nc = build(nseq, list(range(DEPTH)), final_norm=True)
    maps = make_in_maps(x, np.asarray(p), np.asarray(norm_w), np.asarray(w_in), np.asarray(fox_fb), np.asarray(hgrn_gn),
                        np.asarray(hgrn_lb_logits), np.asarray(w_out), np.asarray(w_ple), np.asarray(w_ple_gate),
                        np.asarray(final_norm_w), NCORES, nseq)
    res = run_bass_kernel_spmd(nc, maps, core_ids=list(range(NCORES)))
    return np.concatenate([r["out"] for r in res.results], axis=0).astype(np.float32)
```

```python
import numpy as np
import ml_dtypes
from contextlib import ExitStack
import concourse.bass as bass
import concourse.mybir as mybir
from concourse.bass_utils import run_bass_kernel_spmd

F32 = mybir.dt.float32
BF16 = mybir.dt.bfloat16
AF = mybir.ActivationFunctionType
ALU = mybir.AluOpType

D = 1024
S = 2048
DEPTH = 4
NCH = 8
TW = 512
NT = S // TW
NB = S // 128
DIN = 8208
PLE = 256
EPS = 1e-6
NCORES = 8


class _Rec:
    def __init__(self):
        self.call = None

    def __getattr__(self, name):
        def f(*a, **k):
            self.call = (name, a, k)
            return None
        return f


class Prog:
    ENG = ('pe', 'act', 'dve', 'pool', 'sp')

    def __init__(self, nc, n_dma_sems=40):
        self.nc = nc
        self.ops = []
        self.last_w = {}
        self.readers = {}
        self.n_dma_sems = n_dma_sems

    def add(self, eng, fn, reads=(), writes=(), dma=False):
        i = len(self.ops)
        deps = set()
        for r in reads:
            w = self.last_w.get(r)
            if w is not None:
                deps.add((w, 0))
        for wkey in writes:
            w = self.last_w.get(wkey)
            if w is not None:
                deps.add((w, 1))
            for rd in self.readers.get(wkey, {}).values():
                deps.add((rd, 2))
        for r in reads:
            self.readers.setdefault(r, {})[('d', i) if dma else eng] = i
        for wkey in writes:
            self.last_w[wkey] = i
            self.readers[wkey] = {}
        rec = _Rec()
        fn(rec)
        self.ops.append(dict(eng=eng, call=rec.call, deps=deps, dma=dma, mark=False))
        return i

    def emit(self):
        nc = self.nc
        ops = self.ops
        for i, op in enumerate(ops):
            need = set()
            for (p, kind) in op['deps']:
                po = ops[p]
                if (not po['dma']) and po['eng'] == op['eng']:
                    if po['eng'] == 'pe' and not op['dma']:
                        continue
                    if kind == 2 and not op['dma']:
                        continue
                need.add(p)
            op['need'] = sorted(need)
            for p in op['need']:
                ops[p]['mark'] = True
            op['deps'] = None
        cnt = {e: 0 for e in self.ENG}
        dcnt = [0] * self.n_dma_sems
        nd = 0
        for op in ops:
            if op['dma']:
                k = nd % self.n_dma_sems
                nd += 1
                op['sem'] = ('dma', k)
                op['prev'] = dcnt[k]
                dcnt[k] += 16
                op['val'] = dcnt[k]
            elif op['mark']:
                cnt[op['eng']] += 1
                op['sem'] = ('eng', op['eng'])
                op['val'] = cnt[op['eng']]
        self.cnt = cnt
        with ExitStack() as st:
            sems = {}
            for e in self.ENG:
                sems[('eng', e)] = st.enter_context(nc.semaphore('s_' + e))
            for k in range(self.n_dma_sems):
                sems[('dma', k)] = st.enter_context(nc.semaphore('d_%d' % k))
            block = st.enter_context(nc.Block())
            per = {e: [op for op in ops if op['eng'] == e] for e in self.ENG}

            def run(eng_name, eng):
                waited = {}
                for op in per[eng_name]:
                    ws = [(ops[p]['sem'], ops[p]['val']) for p in op['need']]
                    if op['dma'] and op['prev'] > 0:
                        ws.append((op['sem'], op['prev']))
                    for (s, v) in ws:
                        if waited.get(s, 0) >= v:
                            continue
                        waited[s] = v
                        eng.wait_ge(sems[s], v)
                    nm, a_, k_ = op['call']
                    ins = getattr(eng, nm)(*a_, **k_)
                    if op['dma']:
                        ins.then_inc(sems[op['sem']], 16)
                    elif op['mark']:
                        ins.then_inc(sems[op['sem']], 1)
                if eng_name == 'sp':
                    for k in range(self.n_dma_sems):
                        if dcnt[k] > 0:
                            eng.wait_ge(sems[('dma', k)], dcnt[k])

            @block.tensor
            def _(e):
                run('pe', e)

            @block.scalar
            def _(e):
                run('act', e)

            @block.vector
            def _(e):
                run('dve', e)

            @block.gpsimd
            def _(e):
                run('pool', e)

            @block.sync
            def _(e):
                run('sp', e)


def host_consts():
    c = {}
    i = np.arange(128)
    c['ident32'] = np.eye(128, dtype=np.float32)
    tri = (i[:, None] <= i[None, :]).astype(np.float32)
    c['tri32'] = tri
    c['ones32'] = np.ones((128, 128), np.float32)
    m63 = np.zeros((128, 128), np.float32)
    m63[:64, :] = 1.0
    c['m63'] = m63
    hm = tri * ((i[:, None] // 32) == (i[None, :] // 32))
    c['hmask'] = hm.astype(np.float32)
    rm = np.ones((128, TW), np.float32)
    rm[:, ::32] = 0.0
    c['rmask'] = rm
    return c


CONST_NAMES = ['ident32', 'tri32', 'ones32', 'm63', 'hmask', 'rmask']


def build(nseq, layers, final_norm=True, wq='pool', phases=('norm', 'prep', 'hgrn', 'wo1', 'fox', 'wo2', 'ple'), nheads=8, dbg=False):
    nc = bass.Bass("TRN2", target_bir_lowering=False)
    NL = DEPTH
    x_d = nc.dram_tensor("x", [nseq, S, D], F32, kind="ExternalInput").ap()
    p_d = nc.dram_tensor("p", [NL, nseq, S, PLE], F32, kind="ExternalInput").ap()
    win_d = nc.dram_tensor("w_in", [NL, D, DIN], F32, kind="ExternalInput").ap()
    wout_d = nc.dram_tensor("w_out", [NL, 2 * D, D], F32, kind="ExternalInput").ap()
    wple_d = nc.dram_tensor("w_ple", [NL, PLE, D], F32, kind="ExternalInput").ap()
    wpg_d = nc.dram_tensor("w_ple_gate", [NL, D, D], F32, kind="ExternalInput").ap()
    nw_d = nc.dram_tensor("v_nw", [128, NL * NCH], F32, kind="ExternalInput").ap()
    gn_d = nc.dram_tensor("v_gn", [128, NL * NCH], F32, kind="ExternalInput").ap()
    lbl_d = nc.dram_tensor("v_lbl", [128, NL * NCH], F32, kind="ExternalInput").ap()
    fnw_d = nc.dram_tensor("v_fnw", [128, NCH], F32, kind="ExternalInput").ap()
    fb_d = nc.dram_tensor("v_fb", [128, NL * 16], F32, kind="ExternalInput").ap()
    cd = {n: nc.dram_tensor("c_" + n, [128, TW if n == 'rmask' else 128], F32, kind="ExternalInput").ap() for n in CONST_NAMES}
    out_d = nc.dram_tensor("out", [nseq, S, D], F32, kind="ExternalOutput").ap()

    with ExitStack() as st:
        def sb(name, shape, dt):
            return st.enter_context(nc.sbuf_tensor(name, shape, dt))

        def pst(name, shape, dt):
            return st.enter_context(nc.psum_tensor(name, shape, dt))

        h = sb("h", [128, NCH, S], F32)
        uT = sb("uT", [128, NCH, S], BF16)
        yT = sb("yT", [128, NCH, S], BF16)
        NWB = 8
        wb = sb("wb", [128, NWB, NCH, 128], BF16)
        NF = 10
        ft = sb("ft", [128, NF, TW], F32)
        NBT = 10
        bt = sb("bt", [128, NBT, TW], BF16)
        kAB = sb("kAB", [128, 2, S], BF16)
        VB = sb("VB", [128, NB, 128], BF16)
        W32 = sb("W32", [128, 3, 128], F32)
        W16 = sb("W16", [128, 3, 128], BF16)
        Dall = sb("Dall", [128, 68], F32)
        sm = sb("sm", [128, 6, 256], F32)
        ident32 = sb("ident32", [128, 128], F32)
        identb = sb("identb", [128, 128], BF16)
        tri32 = sb("tri32", [128, 128], F32)
        trib = sb("trib", [128, 128], BF16)
        ones32 = sb("ones32", [128, 128], F32)
        m63 = sb("m63", [128, 128], F32)
        hmask = sb("hmask", [128, 128], F32)
        rmask = sb("rmask", [128, TW], F32)
        onesD = sb("onesD", [128, 128], BF16)
        onesV = sb("onesV", [128, 128], BF16)
        onesb = sb("onesb", [128, 128], BF16)
        nw = sb("nw", [128, NL * NCH], F32)
        gn = sb("gn", [128, NL * NCH], F32)
        lb = sb("lb", [128, NL * NCH], F32)
        oml = sb("oml", [128, NL * NCH], F32)
        lbe = sb("lbe", [128, NL * NCH], F32)
        lbs = sb("lbs", [128, NCH], F32)
        fnw = sb("fnw", [128, NCH], F32)
        fb = sb("fb", [128, NL * 16], F32)
        epsb = sb("epsb", [128, 1], F32)
        wz = sb("wz", [128, NCH, 16], BF16)
        wpl = sb("wpl", [128, 2, 2, 128], BF16)

        PS = [pst("ps%d" % k, [128, TW], F32) for k in range(7)]
        PSB = pst("psb", [128, 2 * TW], BF16)

        P = Prog(nc)
        A = P.add

        cmap = dict(ident32=ident32, tri32=tri32, ones32=ones32, m63=m63, hmask=hmask, rmask=rmask)
        for n in CONST_NAMES:
            A('sp', (lambda t, s_: (lambda e: e.dma_start(out=t[:], in_=s_[:, :])))(cmap[n], cd[n]), writes=[n], dma=True)
        for (t, s_, n) in ((nw, nw_d, 'nw'), (gn, gn_d, 'gn'), (lbe, lbl_d, 'lbe'), (fnw, fnw_d, 'fnw'), (fb, fb_d, 'fb')):
            A('sp', (lambda t, s_: (lambda e: e.dma_start(out=t[:], in_=s_[:, :])))(t, s_), writes=[n], dma=True)
        A('pool', lambda e: e.tensor_copy(out=identb[:], in_=ident32[:]), reads=['ident32'], writes=['identb'])
        A('pool', lambda e: e.tensor_copy(out=trib[:], in_=tri32[:]), reads=['tri32'], writes=['trib'])
        A('pool', lambda e: e.memset(onesD[:], 1.0 / D), writes=['onesD'])
        A('pool', lambda e: e.memset(onesV[:], 1.0 / 128), writes=['onesV'])
        A('pool', lambda e: e.memset(onesb[:], 1.0), writes=['onesb'])
        A('pool', lambda e: e.memset(epsb[:], EPS), writes=['epsb'])
        A('pool', lambda e: e.memset(kAB[:, 1, :], 0.0), writes=['kB'])
        A('pool', lambda e: e.memset(kAB[64:65, 0, :], 1.0), writes=['kA'])
        A('pool', lambda e: e.memset(kAB[0:1, 1, :], 1.0), reads=['kB'], writes=['kB'])
        A('pool', lambda e: e.memset(VB[:], 0.0), writes=['VB'])
        A('pool', lambda e: e.memset(VB[:, :, 0:1], 1.0), reads=['VB'], writes=['VB'])
        A('act', lambda e: e.activation(out=lbe[:], in_=lbe[:], func=AF.Exp), reads=['lbe'], writes=['lbe'])
        A('dve', lambda e: e.tensor_tensor(out=lbs[:], in0=lbe[:, 0:NCH], in1=lbe[:, NCH:2 * NCH], op=ALU.add), reads=['lbe'], writes=['lbs'])
        A('dve', lambda e: e.tensor_tensor(out=lbs[:], in0=lbs[:], in1=lbe[:, 2 * NCH:3 * NCH], op=ALU.add), reads=['lbe', 'lbs'], writes=['lbs'])
        A('dve', lambda e: e.tensor_tensor(out=lbs[:], in0=lbs[:], in1=lbe[:, 3 * NCH:4 * NCH], op=ALU.add), reads=['lbe', 'lbs'], writes=['lbs'])
        A('dve', lambda e: e.reciprocal(out=lbs[:], in_=lbs[:]), reads=['lbs'], writes=['lbs'])
        A('dve', lambda e: e.memset(lb[:, 0:NCH], 0.0), writes=['lb'])
        for l in range(1, NL):
            A('dve', (lambda l: (lambda e: e.tensor_tensor(out=lbe[:, l * NCH:(l + 1) * NCH], in0=lbe[:, l * NCH:(l + 1) * NCH], in1=lbs[:], op=ALU.mult)))(l),
              reads=['lbe', 'lbs'], writes=['lbe'])
            A('dve', (lambda l: (lambda e: e.tensor_tensor(out=lb[:, l * NCH:(l + 1) * NCH], in0=lb[:, (l - 1) * NCH:l * NCH], in1=lbe[:, l * NCH:(l + 1) * NCH], op=ALU.add)))(l),
              reads=['lbe', 'lb'], writes=['lb'])
        A('dve', lambda e: e.tensor_scalar(out=oml[:], in0=lb[:], scalar1=-1.0, scalar2=1.0, op0=ALU.mult, op1=ALU.add), reads=['lb'], writes=['oml'])

        wslot = [0]

        def wload(src_ap, nk=NCH, ncols=128):
            k = wslot[0] % NWB
            wslot[0] += 1
            dst = wb[:, k, 0:nk, 0:ncols]
            A(wq, lambda e: e.dma_start(out=dst, in_=src_ap), writes=[('wb', k)], dma=True)
            return k

        def win_chunk(l, col0, ncols=128):
            return wload(win_d[l].rearrange("(c p) e -> p c e", p=128)[:, :, col0:col0 + ncols], NCH, ncols)

        def proj_fm(k, t0, tw, ps_ap, psres, ncols=128):
            for c in range(NCH):
                A('pe', (lambda c: (lambda e: e.matmul(ps_ap, lhsT=wb[:, k, c, 0:ncols], rhs=uT[:, c, t0:t0 + tw], start=(c == 0), stop=(c == NCH - 1))))(c),
                  reads=[('wb', k), ('uT', c, t0 // TW)], writes=psres)

        def proj_tm(k, tb, ps_ap, psres, ncols=128):
            for c in range(NCH):
                A('pe', (lambda c: (lambda e: e.matmul(ps_ap, lhsT=uT[:, c, tb * 128:(tb + 1) * 128], rhs=wb[:, k, c, 0:ncols], start=(c == 0), stop=(c == NCH - 1))))(c),
                  reads=[('wb', k), ('uT', c, tb // 4)], writes=psres)

        def rmsnorm_to_u(l):
            for t in range(NT):
                ts = slice(t * TW, (t + 1) * TW)
                sq_list = []
                for c in range(NCH):
                    slot = c % 4
                    A('act', (lambda c, slot: (lambda e: e.activation(out=bt[:, slot, :], in_=h[:, c, ts], func=AF.Square)))(c, slot),
                      reads=[('h', c, t)], writes=[('bt', slot)])
                    A('pe', (lambda c, slot: (lambda e: e.matmul(PS[0][:], lhsT=onesD[:], rhs=bt[:, slot, :], start=(c == 0), stop=(c == NCH - 1))))(c, slot),
                      reads=['onesD', ('bt', slot)], writes=[('ps', 0)])
                A('act', lambda e: e.activation(out=ft[:, 0, :], in_=PS[0][:], func=AF.Sqrt, bias=epsb[:], scale=1.0),
                  reads=[('ps', 0), 'epsb'], writes=[('ft', 0)])
                A('dve', lambda e: e.reciprocal(out=ft[:, 0, :], in_=ft[:, 0, :]), reads=[('ft', 0)], writes=[('ft', 0)])
                for c in range(NCH):
                    A('dve', (lambda c: (lambda e: e.scalar_tensor_tensor(out=uT[:, c, ts], in0=h[:, c, ts], scalar=nw[:, l * NCH + c:l * NCH + c + 1],
                                                                         in1=ft[:, 0, :], op0=ALU.mult, op1=ALU.mult)))(c),
                      reads=[('h', c, t), ('ft', 0), 'nw'], writes=[('uT', c, t)])

        def load_x(s):
            for tb in range(NB):
                stg = ft[:, 8:10, :]
                A('sp', (lambda tb: (lambda e: e.dma_start(out=stg, in_=x_d[s, tb * 128:(tb + 1) * 128, :].rearrange("p (a b) -> p a b", a=2))))(tb),
                  writes=[('ft', 8), ('ft', 9)], dma=True)
                for half in range(2):
                    pk = 1 + half
                    for cc in range(4):
                        c = half * 4 + cc
                        A('pe', (lambda c, cc, pk, half: (lambda e: e.transpose(out=PS[pk][:, cc * 128:(cc + 1) * 128], in_=ft[:, 8 + half, cc * 128:(cc + 1) * 128], identity=ident32[:])))(c, cc, pk, half),
                          reads=[('ft', 8 + half), 'ident32'], writes=[('ps', pk)])
                    eng = 'act' if half == 0 else 'dve'
                    if eng == 'act':
                        A('act', (lambda pk, half, tb: (lambda e: e.activation(out=h[:, half * 4:half * 4 + 4, tb * 128:(tb + 1) * 128],
                                                                              in_=PS[pk][:].rearrange("p (a b) -> p a b", a=4), func=AF.Copy)))(pk, half, tb),
                          reads=[('ps', pk)], writes=[('h', half * 4 + cc, tb // 4) for cc in range(4)])
                    else:
                        A('dve', (lambda pk, half, tb: (lambda e: e.tensor_copy(out=h[:, half * 4:half * 4 + 4, tb * 128:(tb + 1) * 128],
                                                                               in_=PS[pk][:].rearrange("p (a b) -> p a b", a=4))))(pk, half, tb),
                          reads=[('ps', pk)], writes=[('h', half * 4 + cc, tb // 4) for cc in range(4)])

        def store_out(s, normed):
            for t in range(NT):
                ts = slice(t * TW, (t + 1) * TW)
                if normed:
                    for c in range(NCH):
                        slot = c % 4
                        A('act', (lambda c, slot: (lambda e: e.activation(out=bt[:, slot, :], in_=h[:, c, ts], func=AF.Square)))(c, slot),
                          reads=[('h', c, t)], writes=[('bt', slot)])
                        A('pe', (lambda c, slot: (lambda e: e.matmul(PS[0][:], lhsT=onesD[:], rhs=bt[:, slot, :], start=(c == 0), stop=(c == NCH - 1))))(c, slot),
                          reads=['onesD', ('bt', slot)], writes=[('ps', 0)])
                    A('act', lambda e: e.activation(out=ft[:, 0, :], in_=PS[0][:], func=AF.Sqrt, bias=epsb[:], scale=1.0),
                      reads=[('ps', 0), 'epsb'], writes=[('ft', 0)])
                    A('dve', lambda e: e.reciprocal(out=ft[:, 0, :], in_=ft[:, 0, :]), reads=[('ft', 0)], writes=[('ft', 0)])
                    for c in range(NCH):
                        A('dve', (lambda c: (lambda e: e.scalar_tensor_tensor(out=h[:, c, ts], in0=h[:, c, ts], scalar=fnw[:, c:c + 1],
                                                                             in1=ft[:, 0, :], op0=ALU.mult, op1=ALU.mult)))(c),
                          reads=[('h', c, t), ('ft', 0), 'fnw'], writes=[('h', c, t)])
                for q in range(4):
                    tb = t * 4 + q
                    for half in range(2):
                        pk = 1 + half
                        for cc in range(4):
                            c = half * 4 + cc
                            A('pe', (lambda c, cc, pk, tb: (lambda e: e.transpose(out=PS[pk][:, cc * 128:(cc + 1) * 128], in_=h[:, c, tb * 128:(tb + 1) * 128], identity=ident32[:])))(c, cc, pk, tb),
                              reads=[('h', c, t), 'ident32'], writes=[('ps', pk)])
                        if half == 0:
                            A('act', (lambda pk, half: (lambda e: e.activation(out=ft[:, 8 + half, :], in_=PS[pk][:], func=AF.Copy)))(pk, half),
                              reads=[('ps', pk)], writes=[('ft', 8 + half)])
                        else:
                            A('dve', (lambda pk, half: (lambda e: e.tensor_copy(out=ft[:, 8 + half, :], in_=PS[pk][:])))(pk, half),
                              reads=[('ps', pk)], writes=[('ft', 8 + half)])
                    A('sp', (lambda tb: (lambda e: e.dma_start(out=out_d[s, tb * 128:(tb + 1) * 128, :].rearrange("p (a b) -> p a b", a=2), in_=ft[:, 8:10, :])))(tb),
                      reads=[('ft', 8), ('ft', 9)], dma=True)

        def hgrn_head(l, hh):
            kq = win_chunk(l, 0 * D + hh * 128)
            kf = win_chunk(l, 1 * D + hh * 128)
            ki = win_chunk(l, 2 * D + hh * 128)
            kg = win_chunk(l, 3 * D + hh * 128)
            lc = l * NCH + hh
            lb_ap = lb[:, lc:lc + 1]
            oml_ap = oml[:, lc:lc + 1]
            gn_ap = gn[:, lc:lc + 1]
            A('pool', lambda e: e.memset(W32[:, 2, :], 0.0), writes=[('W32', 2)])
            A('pool', lambda e: e.memset(W16[:, 2, :], 0.0), writes=[('W16', 2)])
            A('pool', lambda e: e.memset(Dall[:, 0:1], 1.0), writes=['Dall'])
            for sg in range(NT):
                t0 = sg * TW
                proj_fm(kf, t0, TW, PS[0][:], [('ps', 0)])
                A('act', lambda e: e.activation(out=ft[:, 1, :], in_=PS[0][:], func=AF.Sigmoid), reads=[('ps', 0)], writes=[('ft', 1)])
                A('act', lambda e: e.activation(out=ft[:, 2, :], in_=PS[0][:], func=AF.Sigmoid, scale=-1.0), reads=[('ps', 0)], writes=[('ft', 2)])
                A('dve', lambda e: e.tensor_scalar(out=ft[:, 1, :], in0=ft[:, 1, :], scalar1=oml_ap, scalar2=lb_ap, op0=ALU.mult, op1=ALU.add),
                  reads=[('ft', 1), 'oml', 'lb'], writes=[('ft', 1)])
                A('act', lambda e: e.activation(out=ft[:, 3, :], in_=ft[:, 1, :], func=AF.Ln), reads=[('ft', 1)], writes=[('ft', 3)])
                A('dve', lambda e: e.tensor_tensor_scan(out=ft[:, 4, :], data0=rmask[:], data1=ft[:, 3, :], initial=0.0, op0=ALU.mult, op1=ALU.add),
                  reads=[('ft', 3), 'rmask'], writes=[('ft', 4)])
                A('act', lambda e: e.activation(out=ft[:, 5, :], in_=ft[:, 4, :], func=AF.Exp), reads=[('ft', 4)], writes=[('ft', 5)])
                A('act', lambda e: e.activation(out=ft[:, 6, :], in_=ft[:, 4, :], func=AF.Exp, scale=-1.0), reads=[('ft', 4)], writes=[('ft', 6)])
                A('dve', lambda e: e.scalar_tensor_tensor(out=bt[:, 4, :], in0=ft[:, 2, :], scalar=oml_ap, in1=ft[:, 6, :], op0=ALU.mult, op1=ALU.mult),
                  reads=[('ft', 2), ('ft', 6), 'oml'], writes=[('bt', 4)])
                A('pool', (lambda sg: (lambda e: e.tensor_copy(out=Dall[:, 1 + 16 * sg:1 + 16 * sg + 16], in_=ft[:, 5, 31::32])))(sg),
                  reads=[('ft', 5), 'Dall'], writes=['Dall'])
                proj_fm(kq, t0, TW, PS[1][:], [('ps', 1)])
                A('dve', lambda e: e.scalar_tensor_tensor(out=bt[:, 5, :], in0=PS[1][:], scalar=float(128 ** -0.5), in1=ft[:, 5, :], op0=ALU.mult, op1=ALU.mult),
                  reads=[('ps', 1), ('ft', 5)], writes=[('bt', 5)])
                A('pool', (lambda sg: (lambda e: e.tensor_tensor(out=bt[:, 6, :].rearrange("p (a b) -> p a b", b=32), in0=bt[:, 5, :].rearrange("p (a b) -> p a b", b=32),
                                                                 in1=bcast_last(Dall[:, 16 * sg:16 * sg + 16], 32), op=ALU.mult)))(sg),
                  reads=[('bt', 5), 'Dall'], writes=[('bt', 6)])
                proj_fm(kg, t0, TW, PS[1][:], [('ps', 1)])
                A('act', lambda e: e.activation(out=ft[:, 7, :], in_=PS[1][:], func=AF.Silu), reads=[('ps', 1)], writes=[('ft', 7)])
                for q in range(4):
                    proj_tm(ki, sg * 4 + q, PS[3][:, q * 128:(q + 1) * 128], [('ps', 3)])
                A('act', lambda e: e.activation(out=bt[:, 9, :], in_=PS[3][:], func=AF.Copy), reads=[('ps', 3)], writes=[('bt', 9)])
                for q in range(4):
                    A('pe', (lambda q: (lambda e: e.transpose(out=PSB[:, q * 128:(q + 1) * 128], in_=bt[:, 4, q * 128:(q + 1) * 128], identity=identb[:])))(q),
                      reads=[('bt', 4), 'identb'], writes=['psb'])
                A('dve', lambda e: e.tensor_copy(out=bt[:, 8, :], in_=PSB[:, 0:TW]), reads=['psb'], writes=[('bt', 8)])
                def emit_U(idx):
                    q, cc = divmod(idx, 4)
                    ub = 6 if idx % 2 == 0 else 2
                    rows = slice(cc * 32, cc * 32 + 32)
                    A('pe', lambda e: e.matmul(PS[ub][:, 0:128], lhsT=bt[rows, 8, q * 128:(q + 1) * 128], rhs=bt[rows, 9, q * 128:(q + 1) * 128],
                                               start=True, stop=True, tile_position=(cc * 32, 0)),
                      reads=[('bt', 8), ('bt', 9)], writes=psr(ub))
                for q in range(4):
                    A('pe', (lambda q: (lambda e: e.matmul(PS[4][:, q * 128:(q + 1) * 128], lhsT=bt[:, 4, q * 128:(q + 1) * 128], rhs=bt[:, 5, q * 128:(q + 1) * 128], start=True, stop=True)))(q),
                      reads=[('bt', 4), ('bt', 5)], writes=[('ps', 4)])
                A('dve', lambda e: e.tensor_tensor(out=bt[:, 3, :].rearrange("p (a b) -> p a b", a=4), in0=PS[4][:].rearrange("p (a b) -> p a b", a=4),
                                                   in1=bcast_mid(hmask[:], 4), op=ALU.mult),
                  reads=[('ps', 4), 'hmask'], writes=[('bt', 3)])
                emit_U(0)
                for idx in range(16):
                    q, cc = divmod(idx, 4)
                    cg = sg * 16 + idx
                    prev = (cg + 2) % 3
                    cur = cg % 3
                    ub = 6 if idx % 2 == 0 else 2
                    cs = slice(q * 128 + cc * 32, q * 128 + cc * 32 + 32)
                    if cc == 0:
                        A('pe', lambda e: e.matmul(PS[5][:, q * 128:(q + 1) * 128], lhsT=bt[:, 9, q * 128:(q + 1) * 128], rhs=bt[:, 3, q * 128:(q + 1) * 128], start=True, stop=False),
                          reads=[('bt', 9), ('bt', 3)], writes=[('ps', 5)])
                    if idx + 1 < 16:
                        emit_U(idx + 1)
                    A('pe', lambda e: e.matmul(PS[5][:, cs], lhsT=W16[:, prev, :], rhs=bt[:, 6, cs], start=False, stop=(cc == 3)),
                      reads=[('W16', prev), ('bt', 6)], writes=[('ps', 5)])
                    A('dve', lambda e: e.scalar_tensor_tensor(out=W32[:, cur, :], in0=W32[:, prev, :], scalar=Dall[:, cg:cg + 1], in1=PS[ub][:, 0:128], op0=ALU.mult, op1=ALU.add),
                      reads=[('W32', prev), 'Dall'] + psr(ub), writes=[('W32', cur)])
                    A('pool', lambda e: e.tensor_copy(out=W16[:, cur, :], in_=W32[:, cur, :]), reads=[('W32', cur)], writes=[('W16', cur)])
                A('act', lambda e: e.activation(out=bt[:, 7, :], in_=PS[5][:], func=AF.Square), reads=[('ps', 5)], writes=[('bt', 7)])
                A('pe', lambda e: e.matmul(PS[0][:], lhsT=onesV[:], rhs=bt[:, 7, :], start=True, stop=True), reads=['onesV', ('bt', 7)], writes=[('ps', 0)])
                A('act', lambda e: e.activation(out=ft[:, 0, :], in_=PS[0][:], func=AF.Sqrt, bias=epsb[:], scale=1.0), reads=[('ps', 0), 'epsb'], writes=[('ft', 0)])
                A('dve', lambda e: e.reciprocal(out=ft[:, 0, :], in_=ft[:, 0, :]), reads=[('ft', 0)], writes=[('ft', 0)])
                A('dve', lambda e: e.scalar_tensor_tensor(out=ft[:, 0, :], in0=PS[5][:], scalar=gn_ap, in1=ft[:, 0, :], op0=ALU.mult, op1=ALU.mult),
                  reads=[('ps', 5), ('ft', 0), 'gn'], writes=[('ft', 0)])
                A('pool', (lambda t0, sg: (lambda e: e.tensor_tensor(out=yT[:, hh, t0:t0 + TW], in0=ft[:, 0, :], in1=ft[:, 7, :], op=ALU.mult)))(t0, sg),
                  reads=[('ft', 0), ('ft', 7)], writes=[('yT', hh, sg)])

        def bcast_last(ap2, n):
            return ap2.unsqueeze(2).to_broadcast([ap2.shape[0], ap2.shape[1], n])

        def bcast_mid(ap2, n):
            return ap2.unsqueeze(1).to_broadcast([ap2.shape[0], n, ap2.shape[1]])

        PS6 = [('psu', i) for i in range(4)]

        def psr(k):
            return PS6 if k == 6 else [('ps', k)]

        def wout_pass(l, half, swap):
            for dm in range(NCH):
                src = wout_d[l, half * D:(half + 1) * D, dm * 128:(dm + 1) * 128].rearrange("(c p) e -> p c e", p=128)
                if not swap:
                    k = wload(src)
                else:
                    k = wload(src)
                for t in range(NT):
                    pk = 1 + (t % 2)
                    for c in range(NCH):
                        A('pe', (lambda c, t, pk: (lambda e: e.matmul(PS[pk][:], lhsT=wb[:, k, c, :], rhs=yT[:, c, t * TW:(t + 1) * TW], start=(c == 0), stop=(c == NCH - 1))))(c, t, pk),
                          reads=[('wb', k), ('yT', c, t)], writes=[('ps', pk)])
                    A('dve', (lambda t, pk, dm: (lambda e: e.tensor_tensor(out=h[:, dm, t * TW:(t + 1) * TW], in0=h[:, dm, t * TW:(t + 1) * TW], in1=PS[pk][:], op=ALU.add)))(t, pk, dm),
                      reads=[('ps', pk), ('h', dm, t)], writes=[('h', dm, t)])

        def fox_prep(l):
            A(wq, lambda e: e.dma_start(out=wz[:], in_=win_d[l].rearrange("(c p) e -> p c e", p=128)[:, :, 8 * D:8 * D + 16]), writes=['wz'], dma=True)
            for tb in range(NB):
                for c in range(NCH):
                    A('pe', (lambda c, tb: (lambda e: e.matmul(PS[0][:, tb * 16:(tb + 1) * 16], lhsT=uT[:, c, tb * 128:(tb + 1) * 128], rhs=wz[:, c, :], start=(c == 0), stop=(c == NCH - 1))))(c, tb),
                      reads=['wz', ('uT', c, tb // 4)], writes=[('ps', 0)])
            A('dve', lambda e: e.tensor_tensor(out=sm[:, 0, :].rearrange("p (a b) -> p a b", b=16), in0=PS[0][:, 0:256].rearrange("p (a b) -> p a b", b=16),
                                               in1=bcast_mid(fb[:, l * 16:(l + 1) * 16], 16), op=ALU.add),
              reads=[('ps', 0), 'fb'], writes=[('sm', 0)])
            A('act', lambda e: e.activation(out=sm[:, 0, :], in_=sm[:, 0, :], func=AF.Sigmoid), reads=[('sm', 0)], writes=[('sm', 0)])
            A('act', lambda e: e.activation(out=sm[:, 0, :], in_=sm[:, 0, :], func=AF.Ln), reads=[('sm', 0)], writes=[('sm', 0)])
            A('pe', lambda e: e.matmul(PS[1][:, 0:256], lhsT=tri32[:], rhs=sm[:, 0, :], start=True, stop=True), reads=['tri32', ('sm', 0)], writes=[('ps', 1)])
            A('pe', lambda e: e.matmul(PS[2][:, 0:256], lhsT=ones32[:], rhs=sm[:, 0, :], start=True, stop=True), reads=['ones32', ('sm', 0)], writes=[('ps', 2)])
            A('pe', lambda e: e.matmul(PS[3][:, 0:256], lhsT=m63[:], rhs=sm[:, 0, :], start=True, stop=True), reads=['m63', ('sm', 0)], writes=[('ps', 3)])
            A('act', lambda e: e.activation(out=sm[:, 4, :], in_=PS[2][:, 0:256], func=AF.Copy), reads=[('ps', 2)], writes=[('sm', 4)])
            A('dve', lambda e: e.memset(sm[:, 1, 0:16], 0.0), writes=[('sm', 1)])
            for j in range(1, NB):
                A('dve', (lambda j: (lambda e: e.tensor_tensor(out=sm[:, 1, j * 16:(j + 1) * 16], in0=sm[:, 1, (j - 1) * 16:j * 16], in1=sm[:, 4, (j - 1) * 16:j * 16], op=ALU.add)))(j),
                  reads=[('sm', 1), ('sm', 4)], writes=[('sm', 1)])
            A('dve', lambda e: e.scalar_tensor_tensor(out=sm[:, 2, :], in0=PS[1][:, 0:256], scalar=-1.0, in1=sm[:, 1, :], op0=ALU.mult, op1=ALU.subtract),
              reads=[('ps', 1), ('sm', 1)], writes=[('sm', 2)])
            A('dve', lambda e: e.tensor_tensor(out=sm[:, 3, :], in0=PS[3][:, 0:256], in1=sm[:, 1, :], op=ALU.add), reads=[('ps', 3), ('sm', 1)], writes=[('sm', 3)])
            A('dve', lambda e: e.tensor_scalar(out=sm[:, 3, :], in0=sm[:, 3, :], scalar1=8.0, scalar2=None, op0=ALU.mult), reads=[('sm', 3)], writes=[('sm', 3)])

        def fox_pair(l, pp):
            base = 4 * D
            kfq = win_chunk(l, base + 0 * D + pp * 128)
            kfk = win_chunk(l, base + 1 * D + pp * 128)
            kfv = win_chunk(l, base + 2 * D + pp * 128)
            kfg = win_chunk(l, base + 3 * D + pp * 128)
            hA, hB = 2 * pp, 2 * pp + 1
            if pp == 0:
                for qb_ in (1, 3):
                    A('pool', (lambda qb_: (lambda e: e.memset(bt[:, qb_, :], 0.0)))(qb_), writes=[('bt', qb_)])
            for t in range(NT):
                ts = slice(t * TW, (t + 1) * TW)
                pk = t % 2
                proj_fm(kfk, t * TW, TW, PS[pk][:], [('ps', pk)])
                if t % 2 == 0:
                    A('act', (lambda pk, ts: (lambda e: e.activation(out=kAB[:, 0, ts], in_=PS[pk][:], func=AF.Copy)))(pk, ts), reads=[('ps', pk)], writes=['kA'])
                else:
                    A('dve', (lambda pk, ts: (lambda e: e.tensor_copy(out=kAB[:, 0, ts], in_=PS[pk][:])))(pk, ts), reads=[('ps', pk)], writes=['kA'])
                A('pool', lambda e: e.tensor_copy(out=kAB[64:128, 1, ts], in_=kAB[64:128, 0, ts]), reads=['kA'], writes=['kB'])
                A('pool', lambda e: e.memset(kAB[64:65, 0, ts], 1.0), reads=['kA'], writes=['kA'])
            for g in range(4):
                pk = g % 2
                for q in range(4):
                    proj_tm(kfv, g * 4 + q, PS[pk][:, q * 128:(q + 1) * 128], [('ps', pk)])
                if g % 2 == 0:
                    A('act', (lambda pk, g: (lambda e: e.activation(out=VB[:, g * 4:(g + 1) * 4, :], in_=PS[pk][:].rearrange("p (a b) -> p a b", a=4), func=AF.Copy)))(pk, g),
                      reads=[('ps', pk)], writes=['VB'])
                else:
                    A('dve', (lambda pk, g: (lambda e: e.tensor_copy(out=VB[:, g * 4:(g + 1) * 4, :], in_=PS[pk][:].rearrange("p (a b) -> p a b", a=4))))(pk, g),
                      reads=[('ps', pk)], writes=['VB'])
            FST = 9
            for qi in range(NT if FST >= 2 else 0):
                ts = slice(qi * TW, (qi + 1) * TW)
                qa, qb = 0 + (qi % 2) * 2, 1 + (qi % 2) * 2
                pk = 2
                proj_fm(kfq, qi * TW, TW, PS[pk][:], [('ps', pk)])
                A('act', (lambda pk, qa: (lambda e: e.activation(out=bt[:, qa, :], in_=PS[pk][:], func=AF.Copy)))(pk, qa), reads=[('ps', pk)], writes=[('bt', qa)])
                A('pool', lambda e: e.tensor_copy(out=bt[64:128, qb, :], in_=bt[64:128, qa, :]), reads=[('bt', qa)], writes=[('bt', qb)])
                A('pool', lambda e: e.tensor_copy(out=bt[64:65, qa, :].rearrange("p (a b) -> p a b", a=4),
                                                  in_=bcast_last(sm[64:65, 3, :].rearrange("p (a b) -> p a b", b=16)[:, 4 * qi:4 * qi + 4, hA], 128)),
                  reads=[('sm', 3), ('bt', qa)], writes=[('bt', qa)])
                A('pool', lambda e: e.tensor_copy(out=bt[0:1, qb, :].rearrange("p (a b) -> p a b", a=4),
                                                  in_=bcast_last(sm[0:1, 3, :].rearrange("p (a b) -> p a b", b=16)[:, 4 * qi:4 * qi + 4, hB], 128)),
                  reads=[('sm', 3), ('bt', qb)], writes=[('bt', qb)])
                pg = 2
                proj_fm(kfg, qi * TW, TW, PS[pg][:], [('ps', pg)])
                A('act', lambda e: e.activation(out=ft[:, 7, :], in_=PS[pg][:], func=AF.Silu), reads=[('ps', pg)], writes=[('ft', 7)])
                for hd in range(2 if FST >= 3 else 0):
                    hidx = hA if hd == 0 else hB
                    qs = qa if hd == 0 else qb
                    rows = slice(0, 65) if hd == 0 else slice(0, 128)
                    po = 5 + hd
                    nkb = 4 * qi + 4
                    def emit_S(j):
                        c0 = max(j - 4 * qi, 0) * 128
                        psk = 3 + (j % 2)
                        A('pe', lambda e: e.matmul(PS[psk][:, c0:TW], lhsT=kAB[rows, hd, j * 128:(j + 1) * 128], rhs=bt[rows, qs, c0:TW], start=True, stop=True),
                          reads=['kA' if hd == 0 else 'kB', ('bt', qs)], writes=[('ps', psk)])
                    LS = 1
                    if LS:
                        emit_S(0)
                    for j in range(nkb):
                        r = j - 4 * qi
                        c0 = max(r, 0) * 128
                        psk = 3 + (j % 2)
                        ptk = 4 + (j % 4)
                        if LS and j + 1 < nkb:
                            emit_S(j + 1)
                        if not LS:
                            emit_S(j)
                        A('act', lambda e: e.activation(out=bt[:, ptk, c0:TW], in_=PS[psk][:, c0:TW], func=AF.Exp, bias=sm[:, 2, j * 16 + hidx:j * 16 + hidx + 1], scale=0.125),
                          reads=[('ps', psk), ('sm', 2)], writes=[('bt', ptk)])
                        if r >= 0:
                            A('pool', lambda e: e.tensor_tensor(out=bt[:, ptk, c0:c0 + 128], in0=bt[:, ptk, c0:c0 + 128], in1=trib[:], op=ALU.mult),
                              reads=[('bt', ptk), 'trib'], writes=[('bt', ptk)])
                        A('pe', lambda e: e.matmul(PS[hd][:, c0:TW], lhsT=onesb[:], rhs=bt[:, ptk, c0:TW], start=(j == 0), stop=(j == nkb - 1)),
                          reads=['onesb', ('bt', ptk)], writes=[('ps', hd)])
                        if hd == 0:
                            A('pe', lambda e: e.matmul(PS[po][:, c0:TW], lhsT=VB[:, j, :], rhs=bt[:, ptk, c0:TW], start=(j == 0), stop=(j == nkb - 1)),
                              reads=['VB', ('bt', ptk)], writes=psr(po))
                        else:
                            A('pe', lambda e: e.matmul(PS[po][:, c0:TW], lhsT=VB[:, j, :], rhs=bt[:, ptk, c0:TW], start=(j == 0), stop=(j == nkb - 1)),
                              reads=['VB', ('bt', ptk)], writes=psr(po))
                    orow = slice(0, 64) if hd == 0 else slice(64, 128)
                    A('dve', (lambda orow, hd: (lambda e: e.reciprocal(out=ft[orow, 1, :], in_=PS[hd][orow, :])))(orow, hd), reads=[('ps', hd)], writes=[('ft', 1)])
                    A('dve', (lambda orow: (lambda e: e.tensor_tensor(out=ft[orow, 2, :], in0=ft[orow, 1, :], in1=ft[orow, 7, :], op=ALU.mult)))(orow),
                      reads=[('ft', 1), ('ft', 7)], writes=[('ft', 2)])
                    A('dve', (lambda orow, po, qi: (lambda e: e.tensor_tensor(out=yT[orow, pp, qi * TW:(qi + 1) * TW], in0=PS[po][orow, :], in1=ft[orow, 2, :], op=ALU.mult)))(orow, po, qi),
                      reads=psr(po) + [('ft', 2)], writes=[('yT', pp, qi)])

        def ple_phase(l, s):
            for c in range(NCH):
                for t in range(NT):
                    eng = 'pool' if (c + t) % 2 == 0 else 'act'
                    if eng == 'pool':
                        A('pool', (lambda c, t: (lambda e: e.tensor_copy(out=uT[:, c, t * TW:(t + 1) * TW], in_=h[:, c, t * TW:(t + 1) * TW])))(c, t),
                          reads=[('h', c, t)], writes=[('uT', c, t)])
                    else:
                        A('act', (lambda c, t: (lambda e: e.activation(out=uT[:, c, t * TW:(t + 1) * TW], in_=h[:, c, t * TW:(t + 1) * TW], func=AF.Copy)))(c, t),
                          reads=[('h', c, t)], writes=[('uT', c, t)])
            for t in range(NT):
                stg = ft[:, 8:10, :]
                A('sp', (lambda t: (lambda e: e.dma_start(out=stg.rearrange("p a (q k) -> p (a q) k", k=PLE), in_=p_d[l, s, t * TW:(t + 1) * TW, :].rearrange("(q p) k -> p q k", p=128))))(t),
                  writes=[('ft', 8), ('ft', 9)], dma=True)
                for kc in range(2):
                    pk = 1 + kc
                    for q in range(4):
                        a, qq = divmod(q, 2)
                        A('pe', (lambda kc, q, a, qq, pk: (lambda e: e.transpose(out=PS[pk][:, q * 128:(q + 1) * 128], in_=ft[:, 8 + a, qq * PLE + kc * 128:qq * PLE + kc * 128 + 128], identity=ident32[:])))(kc, q, a, qq, pk),
                          reads=[('ft', 8 + a), 'ident32'], writes=[('ps', pk)])
                    if kc == 0:
                        A('act', (lambda pk, t: (lambda e: e.activation(out=kAB[:, 0, t * TW:(t + 1) * TW], in_=PS[pk][:], func=AF.Copy)))(pk, t), reads=[('ps', pk)], writes=['kA'])
                    else:
                        A('dve', (lambda pk, t: (lambda e: e.tensor_copy(out=kAB[:, 1, t * TW:(t + 1) * TW], in_=PS[pk][:])))(pk, t), reads=[('ps', pk)], writes=['kB'])
            for dm in range(NCH):
                kg = wload(wpg_d[l, :, dm * 128:(dm + 1) * 128].rearrange("(c p) e -> p c e", p=128))
                kp = wload(wple_d[l, :, dm * 128:(dm + 1) * 128].rearrange("(c p) e -> p c e", p=128), nk=2)
                for t in range(NT):
                    ts = slice(t * TW, (t + 1) * TW)
                    pa, pb = 3 + (t % 2), 5 + (t % 2)
                    for c in range(NCH):
                        A('pe', (lambda c, pa, ts, t: (lambda e: e.matmul(PS[pa][:], lhsT=wb[:, kg, c, :], rhs=uT[:, c, ts], start=(c == 0), stop=(c == NCH - 1))))(c, pa, ts, t),
                          reads=[('wb', kg), ('uT', c, t)], writes=[('ps', pa)])
                    for kc in range(2):
                        A('pe', (lambda kc, pb, ts: (lambda e: e.matmul(PS[pb][:], lhsT=wb[:, kp, kc, :], rhs=kAB[:, kc, ts], start=(kc == 0), stop=(kc == 1))))(kc, pb, ts),
                          reads=[('wb', kp), 'kA', 'kB'], writes=psr(pb))
                    f1 = 1 + (t % 2)
                    A('act', (lambda pa, f1: (lambda e: e.activation(out=ft[:, f1, :], in_=PS[pa][:], func=AF.Sigmoid)))(pa, f1), reads=[('ps', pa)], writes=[('ft', f1)])
                    A('dve', (lambda pb, f1: (lambda e: e.tensor_tensor(out=ft[:, f1, :], in0=PS[pb][:], in1=ft[:, f1, :], op=ALU.mult)))(pb, f1), reads=psr(pb) + [('ft', f1)], writes=[('ft', f1)])
                    A('pool', (lambda dm, ts, f1, t: (lambda e: e.tensor_tensor(out=h[:, dm, ts], in0=h[:, dm, ts], in1=ft[:, f1, :], op=ALU.add)))(dm, ts, f1, t),
                      reads=[('h', dm, t), ('ft', f1)], writes=[('h', dm, t)])

        def fox_consts():
            A('pool', lambda e: e.memset(kAB[:, 1, :], 0.0), reads=['kB'], writes=['kB'])
            A('pool', lambda e: e.memset(kAB[0:1, 1, :], 1.0), reads=['kB'], writes=['kB'])

        for s in range(nseq):
            load_x(s)
            for l in layers:
                if 'norm' in phases:
                    rmsnorm_to_u(l)
                if 'prep' in phases:
                    fox_prep(l)
                if 'hgrn' in phases:
                    for hh in range(nheads):
                        hgrn_head(l, hh)
                if 'wo1' in phases:
                    wout_pass(l, 0, False)
                if 'fox' in phases:
                    fox_consts()
                    for pp in range(nheads):
                        fox_pair(l, pp)
                if 'wo2' in phases:
                    wout_pass(l, 1, False)
                if 'ple' in phases:
                    ple_phase(l, s)
            store_out(s, final_norm)
        if dbg:
            dft = nc.dram_tensor("dbg_ft", [128, NF, TW], F32, kind="ExternalOutput").ap()
            dbt = nc.dram_tensor("dbg_bt", [128, NBT, TW], BF16, kind="ExternalOutput").ap()
            dy = nc.dram_tensor("dbg_y", [128, NCH, S], BF16, kind="ExternalOutput").ap()
            du = nc.dram_tensor("dbg_u", [128, NCH, S], BF16, kind="ExternalOutput").ap()
            dD = nc.dram_tensor("dbg_D", [128, 68], F32, kind="ExternalOutput").ap()
            dW = nc.dram_tensor("dbg_W", [128, 3, 128], F32, kind="ExternalOutput").ap()
            dsm = nc.dram_tensor("dbg_sm", [128, 6, 256], F32, kind="ExternalOutput").ap()
            A('sp', lambda e: e.dma_start(out=dft[:, :, :], in_=ft[:]), reads=[('ft', i) for i in range(NF)], dma=True)
            A('sp', lambda e: e.dma_start(out=dbt[:, :, :], in_=bt[:]), reads=[('bt', i) for i in range(NBT)], dma=True)
            A('sp', lambda e: e.dma_start(out=dy[:, :, :], in_=yT[:]), reads=[('yT', c, t) for c in range(NCH) for t in range(NT)], dma=True)
            A('sp', lambda e: e.dma_start(out=du[:, :, :], in_=uT[:]), reads=[('uT', c, t) for c in range(NCH) for t in range(NT)], dma=True)
            A('sp', lambda e: e.dma_start(out=dD[:, :], in_=Dall[:]), reads=['Dall'], dma=True)
            A('sp', lambda e: e.dma_start(out=dW[:, :, :], in_=W32[:]), reads=[('W32', i) for i in range(3)], dma=True)
            A('sp', lambda e: e.dma_start(out=dsm[:, :, :], in_=sm[:]), reads=[('sm', i) for i in range(6)], dma=True)
        P.emit()
        nc._prog_stats = dict(nops=len(P.ops), cnt=P.cnt)
    return nc


def _vec_layout(v):
    L = v.shape[0]
    return np.ascontiguousarray(v.reshape(L, NCH, 128).transpose(2, 0, 1).reshape(128, L * NCH)).astype(np.float32)


def make_in_maps(x, p, norm_w, w_in, fox_fb, hgrn_gn, hgrn_lb_logits, w_out, w_ple, w_ple_gate, final_norm_w, ncores, nseq):
    cst = host_consts()
    common = {
        "w_in": np.ascontiguousarray(w_in, dtype=np.float32),
        "w_out": np.ascontiguousarray(w_out, dtype=np.float32),
        "w_ple": np.ascontiguousarray(w_ple, dtype=np.float32),
        "w_ple_gate": np.ascontiguousarray(w_ple_gate, dtype=np.float32),
        "v_nw": _vec_layout(norm_w),
        "v_gn": _vec_layout(hgrn_gn),
        "v_lbl": _vec_layout(hgrn_lb_logits),
        "v_fnw": _vec_layout(final_norm_w[None, :]),
        "v_fb": np.ascontiguousarray(np.broadcast_to(np.asarray(fox_fb, np.float32).reshape(1, -1), (128, fox_fb.size))),
    }
    common.update({'c_' + k: v for k, v in cst.items()})
    maps = []
    for c in range(ncores):
        m = dict(common)
        m["x"] = np.ascontiguousarray(x[c * nseq:(c + 1) * nseq], dtype=np.float32)
        m["p"] = np.ascontiguousarray(p[:, c * nseq:(c + 1) * nseq], dtype=np.float32)
        maps.append(m)
    return maps


def kernel(x, p, norm_w, w_in, fox_fb, hgrn_gn, hgrn_lb_logits, w_out, w_ple, w_ple_gate, final_norm_w):
    x = np.asarray(x)
    B = x.shape[0]
    nseq = B // NCORES
    nc = build(nseq, list(range(DEPTH)), final_norm=True)
    maps = make_in_maps(x, np.asarray(p), np.asarray(norm_w), np.asarray(w_in), np.asarray(fox_fb), np.asarray(hgrn_gn),
                        np.asarray(hgrn_lb_logits), np.asarray(w_out), np.asarray(w_ple), np.asarray(w_ple_gate),
                        np.asarray(final_norm_w), NCORES, nseq)
    res = run_bass_kernel_spmd(nc, maps, core_ids=list(range(NCORES)))
    return np.concatenate([r["out"] for r in res.results], axis=0).astype(np.float32)
```
